# Optimizing a Trainium2 kernel written in Bass

```python
import math
import jax, jax.numpy as jnp
from jax import lax
import numpy as np


D_MODEL = 1024
BATCH = 2
SEQ = 16384
DEPTH = 2

N_MIXERS = 2
ROPE_THETA = 500000.0
ROT_DIM = 16
Q_BLOCK = 128
NORM_EPS = 1e-6
NEG_INF = -1e30

DA_HEAD_DIM = 64
DA_V_DIM = 2 * DA_HEAD_DIM
DA_HEADS = D_MODEL // DA_V_DIM

NSA_HEAD_DIM = 64
NSA_HEADS = D_MODEL // NSA_HEAD_DIM
NSA_KV_GROUPS = 2
NSA_HEADS_PER_GROUP = NSA_HEADS // NSA_KV_GROUPS
NSA_CMP_BLOCK = 32
NSA_CMP_STRIDE = 16
NSA_CMP_HIDDEN = 4 * NSA_HEAD_DIM
NSA_SEL_BLOCK = 64
NSA_TOP_N = 16
NSA_WINDOW = 512
NSA_FORCE = 1e4
NSA_IN_DIM = NSA_HEADS * NSA_HEAD_DIM + 6 * NSA_KV_GROUPS * NSA_HEAD_DIM + 3 * NSA_HEADS

MOE_GROUPS = 4
MOE_EXPERTS_PER_GROUP = 8
MOE_EXPERTS = MOE_GROUPS * MOE_EXPERTS_PER_GROUP
MOE_TOP_K = 2
MOE_HIDDEN = D_MODEL // 4

kernel_name = 'hybrid_diffattn_nsa_hiermoe_adaln'


def _rmsnorm(x, g):
    xf = x.astype(jnp.float32)
    y = xf * lax.rsqrt(jnp.mean(xf * xf, axis=-1, keepdims=True) + NORM_EPS)
    return (y * g.astype(jnp.float32)).astype(x.dtype)


def _rope_tables(positions):
    inv_freq = ROPE_THETA ** (-jnp.arange(0, ROT_DIM, 2, dtype=jnp.float32) / ROT_DIM)
    ang = positions.astype(jnp.float32)[..., None] * inv_freq
    return jnp.cos(ang)[:, :, None, :], jnp.sin(ang)[:, :, None, :]


def _rotary(x, cos, sin):
    half = ROT_DIM // 2
    xr = x[..., :ROT_DIM].astype(jnp.float32)
    x1, x2 = xr[..., :half], xr[..., half:]
    rot = jnp.concatenate([x1 * cos - x2 * sin, x2 * cos + x1 * sin], axis=-1).astype(x.dtype)
    return jnp.concatenate([rot, x[..., ROT_DIM:]], axis=-1)


def _masked_softmax(s, mask):
    s = jnp.where(mask, s.astype(jnp.float32), NEG_INF)
    p = jnp.where(mask, jnp.exp(s - jnp.max(s, axis=-1, keepdims=True)), 0.0)
    return p / jnp.maximum(jnp.sum(p, axis=-1, keepdims=True), 1e-30)


def diff_attention(h, cos, sin, w_in, w_out, lam, subln_g, lambda_init):
    B, S, D = h.shape
    H, d = DA_HEADS, DA_HEAD_DIM
    q, k, v = jnp.split(h @ w_in, 3, axis=-1)
    q = _rotary(q.reshape(B, S, 2 * H, d), cos, sin).reshape(B, S, H, 2, d).transpose(3, 0, 2, 1, 4)
    k = _rotary(k.reshape(B, S, 2 * H, d), cos, sin).reshape(B, S, H, 2, d).transpose(3, 0, 2, 1, 4)
    v = v.reshape(B, S, H, DA_V_DIM).transpose(0, 2, 1, 3)
    lam = lam.astype(jnp.float32)
    lam_full = jnp.exp(jnp.sum(lam[0] * lam[1])) - jnp.exp(jnp.sum(lam[2] * lam[3])) + lambda_init
    scale = d ** -0.5
    k_pos = jnp.arange(S)

    def block(i):
        q0 = i * Q_BLOCK
        qb = lax.dynamic_slice_in_dim(q, q0, Q_BLOCK, axis=3)
        s = jnp.einsum('cbhqd,cbhkd->cbhqk', qb, k) * scale
        mask = k_pos[None, :] <= (q0 + jnp.arange(Q_BLOCK))[:, None]
        p = _masked_softmax(s, mask)
        a = p[0] - lam_full * p[1]
        return jnp.einsum('bhqk,bhkv->bhqv', a.astype(v.dtype), v)

    o = lax.map(block, jnp.arange(S // Q_BLOCK))
    o = o.transpose(1, 0, 3, 2, 4).reshape(B, S, H, DA_V_DIM)
    o = _rmsnorm(o, subln_g) * (1.0 - lambda_init)
    return o.reshape(B, S, D) @ w_out


def _nsa_compress(t, pos_emb, w1, b1, w2):
    B, G, S, d = t.shape
    ch = t.reshape(B, G, S // NSA_CMP_STRIDE, NSA_CMP_STRIDE, d)
    blk = jnp.concatenate([ch[:, :, :-1], ch[:, :, 1:]], axis=3) + pos_emb
    blk = blk.reshape(B, G, blk.shape[2], NSA_CMP_BLOCK * d)
    return jax.nn.gelu(blk @ w1 + b1) @ w2


def nsa_attention(h, cos, sin, w_in, w_out, cmp_pos, cmp_w1, cmp_b1, cmp_w2):
    B, S, D = h.shape
    H, G, Hg, d = NSA_HEADS, NSA_KV_GROUPS, NSA_HEADS_PER_GROUP, NSA_HEAD_DIM
    kvw = G * d
    proj = h @ w_in
    bounds = [H * d + j * kvw for j in range(7)]
    q, k_c, v_c, k_s, v_s, k_w, v_w, g_raw = jnp.split(proj, bounds, axis=-1)
    q = _rotary(q.reshape(B, S, H, d), cos, sin).reshape(B, S, G, Hg, d).transpose(0, 2, 3, 1, 4)

    def kv(t, rope):
        t = t.reshape(B, S, G, d)
        if rope:
            t = _rotary(t, cos, sin)
        return t.transpose(0, 2, 1, 3)

    k_c, v_c = kv(k_c, False), kv(v_c, False)
    k_s, v_s = kv(k_s, True), kv(v_s, True)
    k_w, v_w = kv(k_w, True), kv(v_w, True)
    gates = jax.nn.sigmoid(g_raw.astype(jnp.float32)).reshape(B, S, G, Hg, 3).transpose(0, 2, 3, 1, 4)

    k_cmp = _nsa_compress(k_c, cmp_pos[0], cmp_w1[0], cmp_b1[0], cmp_w2[0])
    v_cmp = _nsa_compress(v_c, cmp_pos[1], cmp_w1[1], cmp_b1[1], cmp_w2[1])
    n_cmp = k_cmp.shape[2]
    n_sel = S // NSA_SEL_BLOCK
    n_top = min(NSA_TOP_N, n_sel)
    cmp_start = jnp.arange(n_cmp) * NSA_CMP_STRIDE
    cmp_end = cmp_start + NSA_CMP_BLOCK - 1
    sel_start = jnp.arange(n_sel) * NSA_SEL_BLOCK
    cmp_to_sel = ((cmp_start[:, None] < sel_start[None, :] + NSA_SEL_BLOCK)
                  & (cmp_start[:, None] + NSA_CMP_BLOCK > sel_start[None, :])).astype(jnp.float32)
    k_blocks = k_s.reshape(B, G, n_sel, NSA_SEL_BLOCK, d)
    v_blocks = v_s.reshape(B, G, n_sel, NSA_SEL_BLOCK, d)
    pad = ((0, 0), (0, 0), (NSA_WINDOW, 0), (0, 0))
    k_w_pad, v_w_pad = jnp.pad(k_w, pad), jnp.pad(v_w, pad)
    b_ix = jnp.arange(B)[:, None, None, None]
    g_ix = jnp.arange(G)[None, :, None, None]
    blk_off = jnp.arange(NSA_SEL_BLOCK)
    sel_ids = jnp.arange(n_sel)
    scale = d ** -0.5

    def block(i):
        q0 = i * Q_BLOCK
        tq = q0 + jnp.arange(Q_BLOCK)
        qb = lax.dynamic_slice_in_dim(q, q0, Q_BLOCK, axis=3)
        s_c = jnp.einsum('bghqd,bgnd->bghqn', qb, k_cmp) * scale
        p_c = _masked_softmax(s_c, cmp_end[None, :] <= tq[:, None])
        o_c = jnp.einsum('bghqn,bgnd->bghqd', p_c.astype(v_cmp.dtype), v_cmp)
        imp = jnp.einsum('bgqn,ns->bgqs', p_c.sum(axis=2), cmp_to_sel)
        q_blk = tq // NSA_SEL_BLOCK
        forced = ((sel_ids[None, :] == 0) | (sel_ids[None, :] == q_blk[:, None])
                  | (sel_ids[None, :] == q_blk[:, None] - 1))
        causal = sel_ids[None, :] <= q_blk[:, None]
        imp = jnp.where(forced, NSA_FORCE, jnp.where(causal, imp, -NSA_FORCE))
        _, sel = lax.top_k(imp, n_top)
        k_sel = k_blocks[b_ix, g_ix, sel].reshape(B, G, Q_BLOCK, n_top * NSA_SEL_BLOCK, d)
        v_sel = v_blocks[b_ix, g_ix, sel].reshape(B, G, Q_BLOCK, n_top * NSA_SEL_BLOCK, d)
        tok = (sel[..., None] * NSA_SEL_BLOCK + blk_off).reshape(B, G, Q_BLOCK, n_top * NSA_SEL_BLOCK)
        s_s = jnp.einsum('bghqd,bgqkd->bghqk', qb, k_sel) * scale
        p_s = _masked_softmax(s_s, tok[:, :, None] <= tq[:, None])
        o_s = jnp.einsum('bghqk,bgqkd->bghqd', p_s.astype(v_sel.dtype), v_sel)
        k_win = lax.dynamic_slice_in_dim(k_w_pad, q0, Q_BLOCK + NSA_WINDOW, axis=2)
        v_win = lax.dynamic_slice_in_dim(v_w_pad, q0, Q_BLOCK + NSA_WINDOW, axis=2)
        tk = q0 - NSA_WINDOW + jnp.arange(Q_BLOCK + NSA_WINDOW)
        dist = tq[:, None] - tk[None, :]
        mask_w = (tk[None, :] >= 0) & (dist >= 0) & (dist < NSA_WINDOW)
        s_w = jnp.einsum('bghqd,bgkd->bghqk', qb, k_win) * scale
        p_w = _masked_softmax(s_w, mask_w)
        o_w = jnp.einsum('bghqk,bgkd->bghqd', p_w.astype(v_win.dtype), v_win)
        gb = lax.dynamic_slice_in_dim(gates, q0, Q_BLOCK, axis=3)
        o = gb[..., 0:1] * o_c + gb[..., 1:2] * o_s + gb[..., 2:3] * o_w
        return o.astype(h.dtype)

    o = lax.map(block, jnp.arange(S // Q_BLOCK))
    o = o.transpose(1, 0, 4, 2, 3, 5).reshape(B, S, D)
    return o @ w_out


def hier_moe(h, w_grp, b_grp, w_exp, b_exp, w_gate, w_up, w_down):
    B, S, D = h.shape
    t = h.reshape(B * S, D)
    T = t.shape[0]
    pg = jax.nn.softmax((t @ w_grp).astype(jnp.float32) + b_grp, axis=-1)
    p_top, grp = lax.top_k(pg, 1)
    le = jnp.einsum('td,gde->tge', t, w_exp).astype(jnp.float32) + b_exp
    le_sel = le[jnp.arange(T), grp[:, 0]]
    val, idx = lax.top_k(le_sel, MOE_TOP_K)
    wts = jax.nn.softmax(val, axis=-1) * p_top
    eid = grp * MOE_EXPERTS_PER_GROUP + idx
    combine = jnp.sum(jax.nn.one_hot(eid, MOE_EXPERTS, dtype=jnp.float32) * wts[..., None], axis=1)
    y = jnp.zeros_like(t)
    for gi in range(MOE_GROUPS):
        sl = slice(gi * MOE_EXPERTS_PER_GROUP, (gi + 1) * MOE_EXPERTS_PER_GROUP)
        a = jax.nn.silu(jnp.einsum('td,edh->teh', t, w_gate[sl])) * jnp.einsum('td,edh->teh', t, w_up[sl])
        a = a * combine[:, sl, None].astype(a.dtype)
        y = y + jnp.einsum('teh,ehd->td', a, w_down[sl]).astype(t.dtype)
    return y.reshape(B, S, D)


def setup_inputs(seed: int = 0) -> dict:
    key = jax.random.key(seed)
    ks = iter(jax.random.split(key, 32))
    D = D_MODEL
    n_a = (DEPTH + N_MIXERS - 1) // N_MIXERS
    n_b = DEPTH // N_MIXERS

    def nrm(shape, scale):
        return jax.random.normal(next(ks), shape, jnp.float32) * scale

    return {
        'x': nrm((BATCH, SEQ, D), 1.0),
        'c': nrm((BATCH, D), 1.0),
        'positions': (jax.random.randint(next(ks), (BATCH, 1), 0, 4096) + jnp.arange(SEQ)[None, :]).astype(jnp.int32),
        'ada_w': nrm((DEPTH, D, 6 * D), 0.5 * D ** -0.5),
        'ada_b': nrm((DEPTH, 6 * D), 0.02),
        'norm_g': 1.0 + nrm((DEPTH, 2, D), 0.02),
        'final_g': 1.0 + nrm((D,), 0.02),
        'diff_w_in': nrm((n_a, D, 3 * D), D ** -0.5),
        'diff_w_out': nrm((n_a, D, D), D ** -0.5),
        'diff_lambda': nrm((n_a, 4, DA_HEAD_DIM), 0.1),
        'diff_subln_g': 1.0 + nrm((n_a, DA_V_DIM), 0.02),
        'nsa_w_in': nrm((n_b, D, NSA_IN_DIM), D ** -0.5),
        'nsa_w_out': nrm((n_b, D, D), D ** -0.5),
        'nsa_cmp_pos': nrm((n_b, 2, NSA_CMP_BLOCK, NSA_HEAD_DIM), 0.02),
        'nsa_cmp_w1': nrm((n_b, 2, NSA_CMP_BLOCK * NSA_HEAD_DIM, NSA_CMP_HIDDEN), (NSA_CMP_BLOCK * NSA_HEAD_DIM) ** -0.5),
        'nsa_cmp_b1': nrm((n_b, 2, NSA_CMP_HIDDEN), 0.02),
        'nsa_cmp_w2': nrm((n_b, 2, NSA_CMP_HIDDEN, NSA_HEAD_DIM), NSA_CMP_HIDDEN ** -0.5),
        'moe_w_group': nrm((DEPTH, D, MOE_GROUPS), D ** -0.5),
        'moe_b_group': nrm((DEPTH, MOE_GROUPS), 0.01),
        'moe_w_expert': nrm((DEPTH, MOE_GROUPS, D, MOE_EXPERTS_PER_GROUP), D ** -0.5),
        'moe_b_expert': nrm((DEPTH, MOE_GROUPS, MOE_EXPERTS_PER_GROUP), 0.01),
        'moe_w_gate': nrm((DEPTH, MOE_EXPERTS, D, MOE_HIDDEN), D ** -0.5),
        'moe_w_up': nrm((DEPTH, MOE_EXPERTS, D, MOE_HIDDEN), D ** -0.5),
        'moe_w_down': nrm((DEPTH, MOE_EXPERTS, MOE_HIDDEN, D), MOE_HIDDEN ** -0.5),
    }


def reference(x, c, positions, ada_w, ada_b, norm_g, final_g,
              diff_w_in, diff_w_out, diff_lambda, diff_subln_g,
              nsa_w_in, nsa_w_out, nsa_cmp_pos, nsa_cmp_w1, nsa_cmp_b1, nsa_cmp_w2,
              moe_w_group, moe_b_group, moe_w_expert, moe_b_expert,
              moe_w_gate, moe_w_up, moe_w_down):
    cos, sin = _rope_tables(positions)
    c_act = jax.nn.silu(c)
    for i in range(DEPTH):
        mod = c_act @ ada_w[i] + ada_b[i]
        sh1, sc1, g1, sh2, sc2, g2 = jnp.split(mod[:, None, :], 6, axis=-1)
        h = _rmsnorm(x, norm_g[i, 0]) * (1.0 + sc1) + sh1
        j = i // N_MIXERS
        if i % N_MIXERS == 0:
            lambda_init = 0.8 - 0.6 * math.exp(-0.3 * i)
            mix = diff_attention(h, cos, sin, diff_w_in[j], diff_w_out[j], diff_lambda[j],
                                 diff_subln_g[j], lambda_init)
        else:
            mix = nsa_attention(h, cos, sin, nsa_w_in[j], nsa_w_out[j], nsa_cmp_pos[j],
                                nsa_cmp_w1[j], nsa_cmp_b1[j], nsa_cmp_w2[j])
        x = x + g1 * mix
        h = _rmsnorm(x, norm_g[i, 1]) * (1.0 + sc2) + sh2
        x = x + g2 * hier_moe(h, moe_w_group[i], moe_b_group[i], moe_w_expert[i], moe_b_expert[i],
                              moe_w_gate[i], moe_w_up[i], moe_w_down[i])
    return _rmsnorm(x, final_g)
```

```python
import math
import numpy as np
import ml_dtypes
from contextlib import ExitStack
import concourse.bass as bass
import concourse.mybir as mybir
from concourse.bass_utils import run_bass_kernel_spmd

F32 = mybir.dt.float32
BF16 = mybir.dt.bfloat16
I32 = mybir.dt.int32
ALU = mybir.AluOpType
AF = mybir.ActivationFunctionType
AX = mybir.AxisListType
NPBF = ml_dtypes.bfloat16

D = 1024
SEQ = 16384
NT = 32
TOK = NT * 128
NEG = -30000.0


class Buf:
    __slots__ = ("w", "r")

    def __init__(self):
        self.w = None
        self.r = {}


class Sched:
    ENG = ("pe", "act", "dve", "pool", "sp")

    def __init__(self, nc, es, n_dma_sems=32):
        self.nc = nc
        self.sems = {}
        self.count = {}
        for e in self.ENG:
            self.sems[e] = es.enter_context(nc.semaphore("s_" + e))
            self.count[e] = 0
        self.dma_sems = []
        for i in range(n_dma_sems):
            k = "d%d" % i
            self.sems[k] = es.enter_context(nc.semaphore("s_" + k))
            self.count[k] = 0
            self.dma_sems.append(k)
        self.dma_rr = 0
        self.dma_rr_sw = 0
        self.n_hw = n_dma_sems - 8
        self.seen = {e: {} for e in self.ENG}
        self.prog = {e: [] for e in self.ENG}

    def _need(self, eng, tok):
        k, v = tok
        if self.seen[eng].get(k, 0) >= v:
            return
        self.seen[eng][k] = v
        self.prog[eng].append(("w", k, v))

    def _deps(self, eng, reads, writes):
        for b in reads:
            if b.w is not None and not (eng == "pe" and b.w[0] == "pe"):
                self._need(eng, b.w)
        for b in writes:
            if b.w is not None and b.w[0] != eng:
                self._need(eng, b.w)
            for k, v in b.r.items():
                if k != eng:
                    self._need(eng, (k, v))

    def _mark(self, tok, reads, writes):
        k, v = tok
        for b in reads:
            if b.r.get(k, 0) < v:
                b.r[k] = v
        for b in writes:
            b.w = tok
            b.r = {}

    def op(self, eng, fn, reads=(), writes=()):
        self._deps(eng, reads, writes)
        self.count[eng] += 1
        tok = (eng, self.count[eng])
        self.prog[eng].append(("i", fn, eng, 1))
        self._mark(tok, reads, writes)
        return tok

    def dma(self, eng, fn, reads=(), writes=()):
        self._deps(eng, reads, writes)
        if eng == "pool":
            k = self.dma_sems[self.n_hw + self.dma_rr_sw]
            self.dma_rr_sw = (self.dma_rr_sw + 1) % (len(self.dma_sems) - self.n_hw)
        else:
            k = self.dma_sems[self.dma_rr]
            self.dma_rr = (self.dma_rr + 1) % self.n_hw
        if self.count[k] > 0:
            self._need(eng, (k, self.count[k]))
        self.count[k] += 16
        tok = (k, self.count[k])
        self.prog[eng].append(("i", fn, k, 16))
        self._mark(tok, reads, writes)
        return tok

    def barrier(self):
        for e in self.ENG:
            for k, c in self.count.items():
                if c > 0 and k != e:
                    self._need(e, (k, c))

    def flush(self):
        self.barrier()
        nc = self.nc
        prog = self.prog
        sems = self.sems

        def run(e, items):
            for it in items:
                if it[0] == "w":
                    e.wait_ge(sems[it[1]], it[2])
                else:
                    it[1](e).then_inc(sems[it[2]], it[3])

        with nc.Block() as block:
            @block.tensor
            def _(e):
                run(e, prog["pe"])

            @block.scalar
            def _(e):
                run(e, prog["act"])

            @block.vector
            def _(e):
                run(e, prog["dve"])

            @block.gpsimd
            def _(e):
                run(e, prog["pool"])

            @block.sync
            def _(e):
                run(e, prog["sp"])
        self.prog = {e: [] for e in self.ENG}


class Ring:
    def __init__(self, items):
        self.items = items
        self.i = 0

    def next(self):
        it = self.items[self.i]
        self.i = (self.i + 1) % len(self.items)
        return it


class K:
    def __init__(self, nc, es):
        self.nc = nc
        self.es = es
        self.S = Sched(nc, es)
        self.dram = {}

    def din(self, name, shape, dt):
        t = self.nc.dram_tensor(name, list(shape), dt, kind="ExternalInput")
        self.dram[name] = (t.ap(), Buf())
        return self.dram[name]

    def dout(self, name, shape, dt):
        t = self.nc.dram_tensor(name, list(shape), dt, kind="ExternalOutput")
        self.dram[name] = (t.ap(), Buf())
        return self.dram[name]

    def dint(self, name, shape, dt):
        t = self.nc.dram_tensor(name, list(shape), dt, kind="Internal")
        self.dram[name] = (t.ap(), Buf())
        return self.dram[name]


_UID = [0]


def _uname(name):
    _UID[0] += 1
    return "%s_%d" % (name, _UID[0])


def sbt(nc, es, name, shape, dt):
    return es.enter_context(nc.sbuf_tensor(_uname(name), list(shape), dt)), Buf()


def pst(nc, es, name, shape, dt):
    return es.enter_context(nc.psum_tensor(_uname(name), list(shape), dt)), Buf()


def sring(nc, es, name, shape, dt, n):
    return Ring([sbt(nc, es, "%s%d" % (name, i), shape, dt) for i in range(n)])


def pring(nc, es, name, shape, dt, n):
    return Ring([pst(nc, es, "%s%d" % (name, i), shape, dt) for i in range(n)])


def AP(t, off, dims):
    return bass.AP(t, off, [list(d) for d in dims])


def emit_consts(k, es):
    nc, S = k.nc, k.S
    c = {}
    identf, bidf = sbt(nc, es, "identf", [128, 128], F32)
    ident, bid = sbt(nc, es, "ident", [128, 128], BF16)
    ones, bones = sbt(nc, es, "ones", [128, 128], F32)
    S.op("pool", lambda e: e.memset(identf[:], 1.0), writes=[bidf])
    S.op("pool", lambda e: e.affine_select(out=identf[:], in_=identf[:], pattern=[[-1, 128]],
                                           compare_op=ALU.is_equal, fill=0.0, base=0, channel_multiplier=1),
         reads=[bidf], writes=[bidf])
    S.op("pool", lambda e: e.tensor_copy(out=ident[:], in_=identf[:]), reads=[bidf], writes=[bid])
    S.op("pool", lambda e: e.memset(ones[:], 1.0), writes=[bones])
    c["identf"] = (identf, bidf)
    c["ident"] = (ident, bid)
    c["ones"] = (ones, bones)
    return c


def emit_mod(k, es, cst, layer, tag):
    nc, S = k.nc, k.S
    c_ap, c_b = k.dram["c"]
    w_ap, w_b = k.dram["ada_w%d" % layer]
    b_ap, b_b = k.dram["ada_b%d" % layer]
    g_ap, g_b = k.dram["ng%d" % layer]
    ones, bones = cst["ones"]
    out = {}
    modcol, bmc = sbt(nc, es, "modcol" + tag, [128, 48], F32)
    gcol, bgc = sbt(nc, es, "gcol" + tag, [128, 16], F32)
    AB, bAB = sbt(nc, es, "AB" + tag, [128, 32], F32)
    gbs = {nm: sbt(nc, es, nm + tag, [128, 1024], F32) for nm in ("g1b", "g2b")}
    with ExitStack() as ps:
        cact, bcact = sbt(nc, ps, "cact" + tag, [128, 8], F32)
        csig, bcsig = sbt(nc, ps, "csig" + tag, [128, 8], F32)
        row, brow = sbt(nc, ps, "modrow" + tag, [1, 6144], F32)
        brow_t, bbrow = sbt(nc, ps, "modb" + tag, [1, 6144], F32)
        wr = sring(nc, ps, "modw" + tag, [128, 8, 512], F32, 2)
        pr = pring(nc, ps, "modp" + tag, [128, 512], F32, 2)
        pcol, bpcol = pst(nc, ps, "modpc" + tag, [128, 512], F32)
        S.dma("sp", lambda e: e.dma_start(out=cact[:], in_=c_ap), reads=[c_b], writes=[bcact])
        S.dma("sp", lambda e: e.dma_start(out=brow_t[:], in_=b_ap), reads=[b_b], writes=[bbrow])
        S.op("act", lambda e: e.activation(out=csig[:], in_=cact[:], func=AF.Sigmoid), reads=[bcact], writes=[bcsig])
        S.op("dve", lambda e: e.tensor_tensor(out=cact[:], in0=cact[:], in1=csig[:], op=ALU.mult),
             reads=[bcact, bcsig], writes=[bcact])
        for nb in range(12):
            wt, bw = wr.next()
            pt, bp = pr.next()
            S.dma("sp", lambda e, wt=wt, nb=nb: e.dma_start(
                out=wt[:], in_=w_ap[:, nb * 512:(nb + 1) * 512].rearrange("(c p) n -> p c n", p=128)),
                reads=[w_b], writes=[bw])
            for kc in range(8):
                S.op("pe", lambda e, pt=pt, wt=wt, kc=kc: e.matmul(
                    pt[0:1, :], lhsT=cact[:, kc:kc + 1], rhs=wt[:, kc, :], start=(kc == 0), stop=(kc == 7)),
                    reads=[bcact, bw], writes=[bp])
            S.op("dve", lambda e, pt=pt, nb=nb: e.tensor_tensor(
                out=row[0:1, nb * 512:(nb + 1) * 512], in0=pt[0:1, :], in1=brow_t[0:1, nb * 512:(nb + 1) * 512],
                op=ALU.add), reads=[bp, bbrow], writes=[brow])
        for kk in range(48):
            S.op("pe", lambda e, kk=kk: e.matmul(pcol[:, kk:kk + 1], lhsT=row[0:1, kk * 128:(kk + 1) * 128],
                                                 rhs=ones[0:1, 0:1], start=True, stop=True),
                 reads=[brow, bones], writes=[bpcol])
        S.op("dve", lambda e: e.tensor_copy(out=modcol[:], in_=pcol[:, 0:48]), reads=[bpcol], writes=[bmc])
        S.dma("sp", lambda e: e.dma_start(out=gcol[:], in_=g_ap), reads=[g_b], writes=[bgc])
        S.op("dve", lambda e: e.scalar_tensor_tensor(out=AB[:, 0:8], in0=modcol[:, 8:16], scalar=1.0, in1=gcol[:, 0:8],
                                                     op0=ALU.add, op1=ALU.mult), reads=[bmc, bgc], writes=[bAB])
        S.op("dve", lambda e: e.tensor_copy(out=AB[:, 8:16], in_=modcol[:, 0:8]), reads=[bmc], writes=[bAB])
        S.op("dve", lambda e: e.scalar_tensor_tensor(out=AB[:, 16:24], in0=modcol[:, 32:40], scalar=1.0, in1=gcol[:, 8:16],
                                                     op0=ALU.add, op1=ALU.mult), reads=[bmc, bgc], writes=[bAB])
        S.op("dve", lambda e: e.tensor_copy(out=AB[:, 24:32], in_=modcol[:, 24:32]), reads=[bmc], writes=[bAB])
        out["AB"] = (AB, bAB)
        for nm, ch in (("g1b", 2), ("g2b", 5)):
            gb, bgb = gbs[nm]
            for hf in range(2):
                pt, bp = pr.next()
                S.op("pe", lambda e, pt=pt, ch=ch, hf=hf: e.matmul(
                    pt[:, :], lhsT=ones[0:1, :], rhs=row[0:1, ch * 1024 + hf * 512: ch * 1024 + (hf + 1) * 512],
                    start=True, stop=True), reads=[bones, brow], writes=[bp])
                S.op("act", lambda e, pt=pt, gb=gb, hf=hf: e.copy(out=gb[:, hf * 512:(hf + 1) * 512], in_=pt[:, :]),
                     reads=[bp], writes=[bgb])
            out[nm] = (gb, bgb)
        S.flush()
    return out


def emit_rope_tables(k, es, tag):
    nc, S = k.nc, k.S
    pos_ap, pos_b = k.dram["pos"]
    inv_ap, inv_b = k.dram["invf"]
    cs, bcs = sbt(nc, es, "ropecs" + tag, [128, NT, 32], F32)
    with ExitStack() as ps:
        posi, bpi = sbt(nc, ps, "posi" + tag, [128, NT], I32)
        posf, bpf = sbt(nc, ps, "posf" + tag, [128, NT], F32)
        invf, binv = sbt(nc, ps, "invf" + tag, [128, 8], F32)
        ang, bang = sbt(nc, ps, "ang" + tag, [128, NT, 8], F32)
        red, bred = sbt(nc, ps, "red" + tag, [128, NT, 16], F32)
        S.dma("sp", lambda e: e.dma_start(out=posi[:], in_=pos_ap), reads=[pos_b], writes=[bpi])
        S.dma("sp", lambda e: e.dma_start(out=invf[:], in_=inv_ap), reads=[inv_b], writes=[binv])
        S.op("dve", lambda e: e.tensor_copy(out=posf[:], in_=posi[:]), reads=[bpi], writes=[bpf])
        S.op("dve", lambda e: e.tensor_tensor(out=ang[:], in0=AP(posf, 0, [[NT, 128], [1, NT], [0, 8]]),
                                              in1=AP(invf, 0, [[8, 128], [0, NT], [1, 8]]), op=ALU.mult),
             reads=[bpf, binv], writes=[bang])
        twopi = 2.0 * math.pi
        ki, bki = sbt(nc, ps, "ropeki" + tag, [128, NT, 16], I32)
        kf, bkf = sbt(nc, ps, "ropekf" + tag, [128, NT, 16], F32)
        S.op("dve", lambda e: e.tensor_scalar(out=red[:, :, 0:8], in0=ang[:], scalar1=0.5 * math.pi, scalar2=None,
                                              op0=ALU.add), reads=[bang], writes=[bred])
        S.op("dve", lambda e: e.tensor_copy(out=red[:, :, 8:16], in_=ang[:]), reads=[bang], writes=[bred])
        S.op("dve", lambda e: e.tensor_scalar(out=kf[:], in0=red[:], scalar1=1.0 / twopi, scalar2=None, op0=ALU.mult),
             reads=[bred], writes=[bkf])
        S.op("dve", lambda e: e.tensor_copy(out=ki[:], in_=kf[:]), reads=[bkf], writes=[bki])
        S.op("dve", lambda e: e.tensor_copy(out=kf[:], in_=ki[:]), reads=[bki], writes=[bkf])
        S.op("dve", lambda e: e.scalar_tensor_tensor(out=red[:], in0=kf[:], scalar=-twopi, in1=red[:],
                                                     op0=ALU.mult, op1=ALU.add), reads=[bkf, bred], writes=[bred])
        S.op("dve", lambda e: e.tensor_scalar(out=kf[:], in0=red[:], scalar1=math.pi, scalar2=-twopi,
                                              op0=ALU.is_gt, op1=ALU.mult), reads=[bred], writes=[bkf])
        S.op("dve", lambda e: e.tensor_tensor(out=red[:], in0=red[:], in1=kf[:], op=ALU.add),
             reads=[bred, bkf], writes=[bred])
        S.op("dve", lambda e: e.tensor_scalar(out=kf[:], in0=red[:], scalar1=-math.pi, scalar2=twopi,
                                              op0=ALU.is_lt, op1=ALU.mult), reads=[bred], writes=[bkf])
        S.op("dve", lambda e: e.tensor_tensor(out=red[:], in0=red[:], in1=kf[:], op=ALU.add),
             reads=[bred, bkf], writes=[bred])
        S.op("dve", lambda e: e.tensor_scalar(out=red[:], in0=red[:], scalar1=-3.1415925, scalar2=3.1415925,
                                              op0=ALU.max, op1=ALU.min), reads=[bred], writes=[bred])
        S.op("act", lambda e: e.activation(out=cs[:, :, 0:16], in_=red[:], func=AF.Sin), reads=[bred], writes=[bcs])
        S.op("dve", lambda e: e.tensor_scalar(out=cs[:, :, 16:32], in0=cs[:, :, 0:16], scalar1=0.125, scalar2=None,
                                              op0=ALU.mult), reads=[bcs], writes=[bcs])
        S.flush()
    return cs, bcs


def emit_norm_T_a(k, cst, xt, bx, tmp):
    S = k.S
    sq, bsq = tmp["sq"].next()
    st, bst = tmp["st"].next()
    xs, bxs = tmp["xs"].next()
    S.op("act", lambda e: e.activation(out=sq[:], in_=xt, func=AF.Square, accum_out=st[:, 0:1]),
         reads=[bx], writes=[bsq, bst])
    S.op("act", lambda e: e.activation(out=st[:, 1:2], in_=st[:, 0:1], func=AF.Sqrt, scale=1.0 / D, bias=1e-6),
         reads=[bst], writes=[bst])
    S.op("dve", lambda e: e.reciprocal(out=st[:, 1:2], in_=st[:, 1:2]), reads=[bst], writes=[bst])
    S.op("act", lambda e: e.activation(out=xs[:], in_=xt, func=AF.Copy, scale=st[:, 1:2]),
         reads=[bx, bst], writes=[bxs])
    return xs, bxs


def emit_norm_T_b(k, cst, xs, bxs, AB, bAB, abcol, hT, bhT, tmp):
    S = k.S
    ident, bid = cst["ident"]
    pT, bpT = tmp["pT"].next()
    for c in range(8):
        S.op("pe", lambda e, c=c: e.transpose(out=pT[:, c, :], in_=xs[:, c * 128:(c + 1) * 128], identity=ident[:]),
             reads=[bxs, bid], writes=[bpT])
    for c in range(8):
        S.op("dve", lambda e, c=c: e.tensor_scalar(out=hT[:, c, :], in0=pT[:, c, :], scalar1=AB[:, abcol + c:abcol + c + 1],
                                                   scalar2=AB[:, abcol + 8 + c:abcol + 9 + c], op0=ALU.mult, op1=ALU.add),
             reads=[bpT, bAB], writes=[bhT])


def emit_norm_T(k, cst, xt, bx, AB, bAB, abcol, hT, bhT, tmp):
    xs, bxs = emit_norm_T_a(k, cst, xt, bx, tmp)
    emit_norm_T_b(k, cst, xs, bxs, AB, bAB, abcol, hT, bhT, tmp)


def norm_tmp(nc, es, tag):
    return {
        "sq": sring(nc, es, "nsq" + tag, [128, 1024], F32, 1),
        "st": sring(nc, es, "nst" + tag, [128, 2], F32, 2),
        "xs": sring(nc, es, "nxs" + tag, [128, 1024], BF16, 2),
        "pT": pring(nc, es, "npT" + tag, [128, 8, 128], BF16, 1),
    }


def emit_rope(k, src, bsrc, dst, bdst, nh, dstw, cs, bcs, t, coff, tmp, btmp):
    S = k.S
    cosb = AP(cs, t * 32 + coff, [[NT * 32, 128], [0, nh], [1, 8]])
    sinb = AP(cs, t * 32 + coff + 8, [[NT * 32, 128], [0, nh], [1, 8]])
    x1 = src(0, 8)
    x2 = src(8, 16)
    S.op("dve", lambda e: e.tensor_tensor(out=tmp[:, 0, 0:nh, :], in0=x1, in1=cosb, op=ALU.mult),
         reads=[bsrc, bcs], writes=[btmp])
    S.op("dve", lambda e: e.tensor_tensor(out=tmp[:, 1, 0:nh, :], in0=x2, in1=sinb, op=ALU.mult),
         reads=[bsrc, bcs], writes=[btmp])
    S.op("dve", lambda e: e.tensor_tensor(out=tmp[:, 2, 0:nh, :], in0=x2, in1=cosb, op=ALU.mult),
         reads=[bsrc, bcs], writes=[btmp])
    S.op("dve", lambda e: e.tensor_tensor(out=tmp[:, 3, 0:nh, :], in0=x1, in1=sinb, op=ALU.mult),
         reads=[bsrc, bcs], writes=[btmp])
    S.op("dve", lambda e: e.tensor_tensor(out=dst(0, 8), in0=tmp[:, 0, 0:nh, :], in1=tmp[:, 1, 0:nh, :], op=ALU.subtract),
         reads=[btmp], writes=[bdst])
    S.op("dve", lambda e: e.tensor_tensor(out=dst(8, 16), in0=tmp[:, 2, 0:nh, :], in1=tmp[:, 3, 0:nh, :], op=ALU.add),
         reads=[btmp], writes=[bdst])


def load_cast_weight(k, w_ap, w_b, wt, bw, ncols, piece=512):
    S = k.S
    for c0 in range(0, ncols, piece):
        c1 = min(ncols, c0 + piece)
        S.dma("pool", lambda e, c0=c0, c1=c1: e.dma_start(
            out=wt[:, :, c0:c1], in_=w_ap[:, c0:c1].rearrange("(c p) n -> p c n", p=128)),
            reads=[w_b], writes=[bw])


def phase_A0(k, cst, mod, cs, bcs):
    nc, S = k.nc, k.S
    ident, bid = cst["ident"]
    AB, bAB = mod["AB"]
    x_ap, x_b = k.dram["x"]
    w_ap, w_b = k.dram["diff_w_in"]
    QT_ap, QT_b = k.dram["QT0"]
    KT_ap, KT_b = k.dram["KT0"]
    V_ap, V_b = k.dram["V0"]
    kn_ap, kn_b = k.dram["knm0"]
    with ExitStack() as es:
        Win, bWin = sbt(nc, es, "a0win", [128, 8, 3072], BF16)
        load_cast_weight(k, w_ap, w_b, Win, bWin, 3072)
        xr = sring(nc, es, "a0x", [128, 1024], F32, 2)
        hr = sring(nc, es, "a0hT", [128, 8, 128], BF16, 2)
        ntmp = norm_tmp(nc, es, "a0")
        ppr = pring(nc, es, "a0pp", [128, 1024], F32, 2)
        pTr = pring(nc, es, "a0pqt", [128, 16, 128], BF16, 1)
        Qsr = sring(nc, es, "a0qs", [128, 16, 65], BF16, 2)
        Ksr = sring(nc, es, "a0ks", [128, 16, 64], BF16, 2)
        Vsr = sring(nc, es, "a0vs", [128, 8, 129], BF16, 2)
        for (Vs_, bVs_) in Vsr.items:
            S.op("pool", lambda e, Vs_=Vs_: e.memset(Vs_[:, :, 128:129], 1.0), writes=[bVs_])
        QTr = sring(nc, es, "a0qts", [128, 16, 128], BF16, 2)
        KTr = sring(nc, es, "a0kts", [128, 16, 128], BF16, 2)
        rtmp, brtmp = sbt(nc, es, "a0rt", [128, 4, 16, 8], F32)
        sqv, bsqv = sbt(nc, es, "a0sqv", [128, 16, 64], F32)
        nrm, bnrm = sbt(nc, es, "a0nrm", [128, 16], F32)
        knm, bknm = sbt(nc, es, "a0knm", [128, 2], F32)
        S.op("dve", lambda e: e.memset(knm[:], 0.0), writes=[bknm])
        hts = {}

        def normT(t):
            xt, bx = xr.next()
            S.dma("sp", lambda e: e.dma_start(out=xt[:], in_=x_ap[t * 128:(t + 1) * 128, :]), reads=[x_b], writes=[bx])
            hT, bhT = hr.next()
            emit_norm_T(k, cst, xt[:], bx, AB, bAB, 0, hT, bhT, ntmp)
            hts[t] = (hT, bhT)

        def mm(t, sec):
            hT, bhT = hts[t]
            pp, bpp = ppr.next()
            for nb in range(2):
                for c in range(8):
                    S.op("pe", lambda e, c=c, nb=nb: e.matmul(
                        pp[:, nb * 512:(nb + 1) * 512], lhsT=hT[:, c, :],
                        rhs=Win[:, c, sec * 1024 + nb * 512: sec * 1024 + (nb + 1) * 512],
                        start=(c == 0), stop=(c == 7)), reads=[bhT, bWin], writes=[bpp])
            return pp, bpp

        def qpost(t, pp, bpp):
            src = lambda d0, d1: AP(pp, d0, [[1024, 128], [64, 16], [1, d1 - d0]])
            Qs, bQs = Qsr.next()
            dst = lambda d0, d1: AP(Qs, d0, [[1040, 128], [65, 16], [1, d1 - d0]])
            emit_rope(k, src, bpp, dst, bQs, 16, 65, cs, bcs, t, 16, rtmp, brtmp)
            S.op("act", lambda e: e.activation(out=dst(16, 64), in_=src(16, 64), func=AF.Copy, scale=0.125), reads=[bpp], writes=[bQs])
            S.op("dve", lambda e: e.tensor_tensor(out=sqv[:], in0=dst(0, 64), in1=dst(0, 64), op=ALU.mult), reads=[bQs], writes=[bsqv])
            S.op("dve", lambda e: e.tensor_reduce(out=nrm[:], in_=sqv[:], axis=AX.X, op=ALU.add), reads=[bsqv], writes=[bnrm])
            S.op("act", lambda e: e.activation(out=nrm[:], in_=nrm[:], func=AF.Sqrt), reads=[bnrm], writes=[bnrm])
            S.op("dve", lambda e: e.tensor_scalar(out=dst(64, 65), in0=AP(nrm, 0, [[16, 128], [1, 16], [1, 1]]),
                                                  scalar1=-1.0, scalar2=None, op0=ALU.mult), reads=[bnrm], writes=[bQs])
            return Qs, bQs

        def qtr(t, Qs, bQs):
            pT, bpT = pTr.next()
            for hc in range(16):
                S.op("pe", lambda e, hc=hc: e.transpose(out=pT[0:65, hc, :], in_=Qs[:, hc, :], identity=ident[:]),
                     reads=[bQs, bid], writes=[bpT])
            QTs, bQTs = QTr.next()
            S.op("act", lambda e: e.copy(out=QTs[0:65, :, :], in_=pT[0:65, :, :]), reads=[bpT], writes=[bQTs])
            S.dma("sp", lambda e: e.dma_start(out=QT_ap[:, :, t * 128:(t + 1) * 128].rearrange("h r q -> r h q"), in_=QTs[0:65, :, :]),
                  reads=[bQTs], writes=[QT_b])

        def kpost(t, pp, bpp):
            src = lambda d0, d1: AP(pp, d0, [[1024, 128], [64, 16], [1, d1 - d0]])
            Ks, bKs = Ksr.next()
            dst = lambda d0, d1: AP(Ks, d0, [[1024, 128], [64, 16], [1, d1 - d0]])
            emit_rope(k, src, bpp, dst, bKs, 16, 64, cs, bcs, t, 0, rtmp, brtmp)
            S.op("act", lambda e: e.copy(out=dst(16, 64), in_=src(16, 64)), reads=[bpp], writes=[bKs])
            S.op("dve", lambda e: e.tensor_tensor(out=sqv[:], in0=dst(0, 64), in1=dst(0, 64), op=ALU.mult), reads=[bKs], writes=[bsqv])
            S.op("dve", lambda e: e.tensor_reduce(out=nrm[:], in_=sqv[:], axis=AX.X, op=ALU.add), reads=[bsqv], writes=[bnrm])
            S.op("dve", lambda e: e.tensor_reduce(out=knm[:, 1:2], in_=nrm[:], axis=AX.X, op=ALU.max), reads=[bnrm], writes=[bknm])
            S.op("dve", lambda e: e.tensor_tensor(out=knm[:, 0:1], in0=knm[:, 0:1], in1=knm[:, 1:2], op=ALU.max), reads=[bknm], writes=[bknm])
            return Ks, bKs

        def ktr(t, Ks, bKs):
            pT, bpT = pTr.next()
            for hc in range(16):
                S.op("pe", lambda e, hc=hc: e.transpose(out=pT[0:64, hc, :], in_=Ks[:, hc, :], identity=ident[:]),
                     reads=[bKs, bid], writes=[bpT])
            KTs, bKTs = KTr.next()
            S.op("act", lambda e: e.copy(out=KTs[0:64, :, :], in_=pT[0:64, :, :]), reads=[bpT], writes=[bKTs])
            S.dma("sp", lambda e: e.dma_start(out=KT_ap[:, :, t * 128:(t + 1) * 128].rearrange("h r q -> r h q"), in_=KTs[0:64, :, :]),
                  reads=[bKTs], writes=[KT_b])

        def vpost(t, pp, bpp):
            Vs, bVs = Vsr.next()
            S.op("act", lambda e: e.copy(out=Vs[:, :, 0:128], in_=AP(pp, 0, [[1024, 128], [128, 8], [1, 128]])), reads=[bpp], writes=[bVs])
            S.dma("sp", lambda e: e.dma_start(out=V_ap[:, :, t, :].rearrange("h p v -> p h v"), in_=Vs[:]), reads=[bVs], writes=[V_b])

        normT(0)
        for t in range(NT):
            ppq = mm(t, 0)
            Qs = qpost(t, *ppq)
            ppk = mm(t, 1)
            Ks = kpost(t, *ppk)
            qtr(t, *Qs)
            if t + 1 < NT:
                normT(t + 1)
            ppv = mm(t, 2)
            vpost(t, *ppv)
            ktr(t, *Ks)
        S.dma("sp", lambda e: e.dma_start(out=kn_ap, in_=knm[:, 0:1]), reads=[bknm], writes=[kn_b])
        S.flush()


def core_tiles(j):
    return [4 * i + j for i in range(NT)]


def shard_rows(a_b, j):
    a = a_b.reshape(SEQ // 128, 128, *a_b.shape[1:])
    return np.ascontiguousarray(a[j::4].reshape(TOK, *a_b.shape[1:]))


def col_layout(v):
    return np.ascontiguousarray(v.reshape(-1, 128).T)


INVF = (500000.0 ** (-np.arange(0, 16, 2, dtype=np.float32) / 16)).astype(np.float32)


def common_inputs(core, x, c, positions, ada_w, ada_b, norm_g, layers):
    b, j = core // 4, core % 4
    m = {
        "c": col_layout(c[b]),
        "pos": np.ascontiguousarray(shard_rows(positions[b], j).reshape(NT, 128).T),
        "invf": np.ascontiguousarray(np.broadcast_to(INVF[None, :], (128, 8))),
    }
    for l in layers:
        m["ada_w%d" % l] = ada_w[l]
        m["ada_b%d" % l] = ada_b[l][None, :]
        m["ng%d" % l] = np.concatenate([col_layout(norm_g[l, 0]), col_layout(norm_g[l, 1])], axis=1)
    return m


def declare_common(k, layers):
    k.din("c", [128, 8], F32)
    k.din("pos", [128, NT], I32)
    k.din("invf", [128, 8], F32)
    for l in layers:
        k.din("ada_w%d" % l, [1024, 6144], F32)
        k.din("ada_b%d" % l, [1, 6144], F32)
        k.din("ng%d" % l, [128, 16], F32)


def build_L1():
    nc = bass.Bass("TRN2", target_bir_lowering=False)
    with ExitStack() as es:
        k = K(nc, es)
        declare_common(k, [0])
        k.din("x", [TOK, D], F32)
        k.din("diff_w_in", [D, 3072], F32)
        k.dout("QT0", [16, 65, TOK], BF16)
        k.dout("KT0", [16, 64, TOK], BF16)
        k.dout("V0", [8, 128, NT, 129], BF16)
        k.dout("knm0", [128, 1], F32)
        cst = emit_consts(k, es)
        mod = emit_mod(k, es, cst, 0, "m0")
        cs, bcs = emit_rope_tables(k, es, "r")
        phase_A0(k, cst, mod, cs, bcs)
    return nc


def emit_kmax(k, es, cst, knm_name, tag):
    nc, S = k.nc, k.S
    kn_ap, kn_b = k.dram[knm_name]
    identf, bidf = cst["identf"]
    ones, bones = cst["ones"]
    kmx, bkmx = sbt(nc, es, "kmx" + tag, [128, 1], F32)
    with ExitStack() as ps:
        a, ba = sbt(nc, ps, "kma" + tag, [128, 4], F32)
        m, bm = sbt(nc, ps, "kmm" + tag, [128, 1], F32)
        r, br = sbt(nc, ps, "kmr" + tag, [1, 128], F32)
        s, bs = sbt(nc, ps, "kms" + tag, [1, 1], F32)
        p1, bp1 = pst(nc, ps, "kmp" + tag, [128, 512], F32)
        S.dma("sp", lambda e: e.dma_start(out=a[:], in_=kn_ap), reads=[kn_b], writes=[ba])
        S.op("dve", lambda e: e.tensor_reduce(out=m[:], in_=a[:], axis=AX.X, op=ALU.max), reads=[ba], writes=[bm])
        S.op("pe", lambda e: e.transpose(out=p1[0:1, 0:128], in_=m[:, 0:1], identity=identf[:]),
             reads=[bm, bidf], writes=[bp1])
        S.op("dve", lambda e: e.tensor_copy(out=r[:], in_=p1[0:1, 0:128]), reads=[bp1], writes=[br])
        S.op("dve", lambda e: e.tensor_reduce(out=s[:], in_=r[:], axis=AX.X, op=ALU.max), reads=[br], writes=[bs])
        S.op("act", lambda e: e.activation(out=s[:], in_=s[:], func=AF.Sqrt, scale=1.02), reads=[bs], writes=[bs])
        S.op("pe", lambda e: e.matmul(p1[:, 256:257], lhsT=ones[0:1, :], rhs=s[0:1, 0:1], start=True, stop=True),
             reads=[bs, bones, bp1], writes=[bp1])
        S.op("dve", lambda e: e.tensor_copy(out=kmx[:], in_=p1[:, 256:257]), reads=[bp1], writes=[bkmx])
        S.flush()
    return kmx, bkmx


def phase_B0(k, cst):
    nc, S = k.nc, k.S
    ident, bid = cst["ident"]
    ones, bones = cst["ones"]
    QT_ap, QT_b = k.dram["QT0"]
    KT_ap, KT_b = k.dram["KT0g"]
    V_ap, V_b = k.dram["V0g"]
    O_ap, O_b = k.dram["O0"]
    lam_ap, lam_b = k.dram["lam"]
    sg_ap, sg_b = k.dram["subg"]
    dm_ap, dm_b = k.dram["dmask"]
    lam_init = 0.8 - 0.6 * math.exp(-0.3 * 0)
    with ExitStack() as es:
        kmx, bkmx = emit_kmax(k, es, cst, "knm0g", "b0")
        nlam, bnlam = sbt(nc, es, "b0nlam", [128, 1], F32)
        gsub, bgsub = sbt(nc, es, "b0gsub", [128, 128], F32)
        dmask, bdmask = sbt(nc, es, "b0dmask", [128, 4, 128], BF16)
        S.dma("sp", lambda e: e.dma_start(out=gsub[:], in_=sg_ap), reads=[sg_b], writes=[bgsub])
        S.dma("sp", lambda e: e.dma_start(out=dmask[:], in_=dm_ap), reads=[dm_b], writes=[bdmask])
        with ExitStack() as ps:
            lt, blt = sbt(nc, ps, "b0lt", [1, 256], F32)
            lp, blp = sbt(nc, ps, "b0lp", [1, 2, 64], F32)
            ls, bls = sbt(nc, ps, "b0ls", [1, 2], F32)
            pl, bpl = pst(nc, ps, "b0pl", [128, 512], F32)
            S.dma("sp", lambda e: e.dma_start(out=lt[:], in_=lam_ap), reads=[lam_b], writes=[blt])
            S.op("dve", lambda e: e.tensor_tensor(out=lp[:], in0=AP(lt, 0, [[256, 1], [128, 2], [1, 64]]),
                                                  in1=AP(lt, 64, [[256, 1], [128, 2], [1, 64]]), op=ALU.mult),
                 reads=[blt], writes=[blp])
            S.op("dve", lambda e: e.tensor_reduce(out=ls[:], in_=lp[:], axis=AX.X, op=ALU.add), reads=[blp], writes=[bls])
            S.op("act", lambda e: e.activation(out=ls[:], in_=ls[:], func=AF.Exp), reads=[bls], writes=[bls])
            S.op("dve", lambda e: e.scalar_tensor_tensor(out=ls[0:1, 0:1], in0=ls[0:1, 1:2], scalar=-lam_init, in1=ls[0:1, 0:1],
                                                         op0=ALU.add, op1=ALU.subtract), reads=[bls], writes=[bls])
            S.op("pe", lambda e: e.matmul(pl[:, 0:1], lhsT=ones[0:1, :], rhs=ls[0:1, 0:1], start=True, stop=True),
                 reads=[bls, bones], writes=[bpl])
            S.op("dve", lambda e: e.tensor_copy(out=nlam[:], in_=pl[:, 0:1]), reads=[bpl], writes=[bnlam])
            S.flush()
        NCH = 16
        Qr = sring(nc, es, "b0q", [65, 2, 512], BF16, 3)
        Kr = sring(nc, es, "b0k", [65, 2, 4, 512], BF16, 3)
        Vr = sring(nc, es, "b0v", [128, 4, 4, 129], BF16, 3)
        Pr = sring(nc, es, "b0p", [128, 512], BF16, 5)
        STr = pring(nc, es, "b0st", [128, 512], F32, 4)
        accs = [pst(nc, es, "b0acc%d" % a, [128, 512], F32) for a in range(4)]
        rl, brl = sbt(nc, es, "b0rl", [128, 4], F32)
        o1r = sring(nc, es, "b0o1", [128, 128], F32, 2)
        o2r = sring(nc, es, "b0o2", [128, 128], F32, 2)
        sqr = sring(nc, es, "b0sq", [128, 128], F32, 2)
        str_ = sring(nc, es, "b0stt", [128, 2], F32, 2)
        Or = sring(nc, es, "b0o", [128, 128], BF16, 3)
        for (Kc, bK) in Kr.items:
            S.op("dve", lambda e, Kc=Kc: e.tensor_scalar(
                out=AP(Kc, 64 * 4096, [[4096, 1], [1, 4096]]),
                in0=AP(ones, 64 * 128, [[128, 1], [0, 4096]]), scalar1=kmx[64:65, 0:1], scalar2=None,
                op0=ALU.mult), reads=[bones, bkmx], writes=[bK])
        LA = 3
        pend = []

        def finish_tile(h, I, a):
            acc, bacc = accs[a]
            o1, bo1 = o1r.next()
            o2, bo2 = o2r.next()
            sq, bsq = sqr.next()
            stt, bstt = str_.next()
            Ot, bOt = Or.next()
            S.op("dve", lambda e: e.reciprocal(out=rl[:, 0:2], in_=AP(acc, 128, [[512, 128], [256, 2]])),
                 reads=[bacc], writes=[brl])
            S.op("dve", lambda e: e.tensor_tensor(out=rl[:, 2:3], in0=rl[:, 1:2], in1=nlam[:], op=ALU.mult),
                 reads=[brl, bnlam], writes=[brl])
            S.op("dve", lambda e: e.tensor_scalar(out=o1[:], in0=acc[:, 256:384], scalar1=rl[:, 2:3], scalar2=None, op0=ALU.mult),
                 reads=[bacc, brl], writes=[bo1])
            S.op("dve", lambda e: e.scalar_tensor_tensor(out=o2[:], in0=acc[:, 0:128], scalar=rl[:, 0:1], in1=o1[:],
                                                         op0=ALU.mult, op1=ALU.add), reads=[bacc, brl, bo1], writes=[bo2])
            S.op("act", lambda e: e.activation(out=sq[:], in_=o2[:], func=AF.Square, accum_out=stt[:, 0:1]),
                 reads=[bo2], writes=[bsq, bstt])
            f2 = (1.0 - lam_init) ** 2
            S.op("act", lambda e: e.activation(out=stt[:, 1:2], in_=stt[:, 0:1], func=AF.Sqrt, scale=1.0 / (128 * f2), bias=1e-6 / f2),
                 reads=[bstt], writes=[bstt])
            S.op("dve", lambda e: e.reciprocal(out=stt[:, 1:2], in_=stt[:, 1:2]), reads=[bstt], writes=[bstt])
            S.op("dve", lambda e: e.scalar_tensor_tensor(out=Ot[:], in0=o2[:], scalar=stt[:, 1:2], in1=gsub[:], op0=ALU.mult, op1=ALU.mult),
                 reads=[bo2, bstt, bgsub], writes=[bOt])
            tl = 4 * I + a
            S.dma("pool", lambda e: e.dma_start(out=O_ap[tl * 128:(tl + 1) * 128, h * 128:(h + 1) * 128], in_=Ot[:]),
                  reads=[bOt], writes=[O_b])

        def emit_pv(item):
            (h, I, kt, comp, a0, PT, bPT, Vc, bV, r, ip, diag, u) = item
            for a in range(a0, 4):
                acc, bacc = accs[a]
                last = (kt == 16 * I + 4 * a + 3)
                S.op("pe", lambda e, acc=acc, a=a, last=last: e.matmul(
                    acc[:, comp * 256: comp * 256 + 129], lhsT=PT[:, a * 128:(a + 1) * 128], rhs=Vc[:, r, ip, :],
                    start=(kt == 0 and comp == 0), stop=last, skip_group_check=True), reads=[bPT, bV], writes=[bacc])
            if diag and u == 3 and comp == 1:
                finish_tile(h, I, a0)

        for h in range(8):
            for I in range(8):
                Qc, bQ = Qr.next()
                S.dma("sp", lambda e, Qc=Qc, h=h, I=I: e.dma_start(
                    out=Qc[:, :, :], in_=QT_ap[2 * h:2 * h + 2, :, I * 512:(I + 1) * 512].rearrange("c r q -> r c q")),
                    reads=[QT_b], writes=[bQ])
                for ch in range(I + 1):
                    Kc, bK = Kr.next()
                    Vc, bV = Vr.next()
                    for r in range(4):
                        for comp in range(2):
                            S.dma("sp", lambda e, Kc=Kc, h=h, ch=ch, r=r, comp=comp: e.dma_start(
                                out=Kc[0:64, comp, r, :], in_=KT_ap[r, 2 * h + comp, :, ch * 512:(ch + 1) * 512]),
                                reads=[KT_b], writes=[bK])
                        S.dma("sp", lambda e, Vc=Vc, h=h, ch=ch, r=r: e.dma_start(
                            out=Vc[:, r, :, :], in_=V_ap[r, h, :, 4 * ch:4 * ch + 4, :]), reads=[V_b], writes=[bV])
                    diag = (ch == I)
                    for kl in range(NCH):
                        kt = 16 * ch + kl
                        ip, r = kl // 4, kl % 4
                        a0 = kl // 4 if diag else 0
                        u = kl % 4
                        for comp in range(2):
                            ST, bST = STr.next()
                            ksl = lambda Kc=Kc, comp=comp, r=r, ip=ip: Kc[:, comp, r, ip * 128:(ip + 1) * 128]
                            if diag:
                                S.op("pe", lambda e, ST=ST, ksl=ksl, Qc=Qc, comp=comp, a0=a0: e.matmul(
                                    ST[:, a0 * 128:(a0 + 1) * 128], lhsT=ksl(), rhs=Qc[:, comp, a0 * 128:(a0 + 1) * 128],
                                    start=True, stop=False), reads=[bK, bQ], writes=[bST])
                                S.op("pe", lambda e, ST=ST, a0=a0, u=u: e.matmul(
                                    ST[:, a0 * 128:(a0 + 1) * 128], lhsT=ident[:], rhs=dmask[:, u, :],
                                    start=False, stop=True), reads=[bid, bdmask], writes=[bST])
                                if a0 < 3:
                                    S.op("pe", lambda e, ST=ST, ksl=ksl, Qc=Qc, comp=comp, a0=a0: e.matmul(
                                        ST[:, (a0 + 1) * 128:512], lhsT=ksl(), rhs=Qc[:, comp, (a0 + 1) * 128:512],
                                        start=True, stop=True), reads=[bK, bQ], writes=[bST])
                            else:
                                S.op("pe", lambda e, ST=ST, ksl=ksl, Qc=Qc, comp=comp: e.matmul(
                                    ST[:, :], lhsT=ksl(), rhs=Qc[:, comp, :], start=True, stop=True),
                                    reads=[bK, bQ], writes=[bST])
                            PT, bPT = Pr.next()
                            S.op("act", lambda e, ST=ST, PT=PT, a0=a0: e.activation(
                                out=PT[:, a0 * 128:512], in_=ST[:, a0 * 128:512], func=AF.Exp), reads=[bST], writes=[bPT])
                            pend.append((h, I, kt, comp, a0, PT, bPT, Vc, bV, r, ip, diag, u))
                            if len(pend) > LA:
                                emit_pv(pend.pop(0))
        while pend:
            emit_pv(pend.pop(0))
        S.flush()


def phase_C(k, cst, mod, layer, o_name, xin_name, xout_name, wout_name, final):
    nc, S = k.nc, k.S
    ident, bid = cst["ident"]
    AB, bAB = mod["AB"]
    g1b, bg1b = mod["g1b"]
    g2b, bg2b = mod["g2b"]
    O_ap, O_b = k.dram[o_name]
    xi_ap, xi_b = k.dram[xin_name]
    xo_ap, xo_b = k.dram[xout_name]
    x1_ap, x1_b = k.dram["x1s%d" % layer]
    wo_ap, wo_b = k.dram[wout_name]
    wr_ap, wr_b = k.dram["moe_wr%d" % layer]
    br_ap, br_b = k.dram["moe_br%d" % layer]
    wg_ap, wg_b = k.dram["moe_wg%d" % layer]
    wu_ap, wu_b = k.dram["moe_wu%d" % layer]
    wd_ap, wd_b = k.dram["moe_wd%d" % layer]
    HT = 16
    with ExitStack() as es:
        yacc, byacc = sbt(nc, es, "c_yacc", [128, HT, 1024], F32)
        byt = [Buf() for _ in range(HT)]
        h2T, _ = sbt(nc, es, "c_h2T", [128, 8, HT * 128], BF16)
        bh2 = [Buf() for _ in range(HT)]
        comb, _ = sbt(nc, es, "c_comb", [128, HT, 32], F32)
        bcomb = [Buf() for _ in range(HT)]
        if final:
            fgb, bfgb = sbt(nc, es, "c_fgb", [128, 1024], F32)
            fg_ap, fg_b = k.dram["final_g"]
            S.dma("sp", lambda e: e.dma_start(out=fgb[:], in_=fg_ap), reads=[fg_b], writes=[bfgb])
        for half in range(2):
            with ExitStack() as p1:
                Wout, bWout = sbt(nc, p1, "c_wout", [128, 8, 1024], BF16)
                load_cast_weight(k, wo_ap, wo_b, Wout, bWout, 1024)
                Wr, bWr = sbt(nc, p1, "c_wr", [128, 8, 36], BF16)
                S.dma("pool", lambda e: e.dma_start(out=Wr[:], in_=wr_ap.rearrange("(c p) n -> p c n", p=128)),
                      reads=[wr_b], writes=[bWr])
                brt, bbrt = sbt(nc, p1, "c_br", [128, 36], F32)
                S.dma("sp", lambda e: e.dma_start(out=brt[:], in_=br_ap), reads=[br_b], writes=[bbrt])
                xr = sring(nc, p1, "c_x", [128, 1024], F32, 2)
                Otr = sring(nc, p1, "c_o", [128, 1024], BF16, 2)
                OTr = sring(nc, p1, "c_oT", [128, 8, 128], BF16, 2)
                x1r = sring(nc, p1, "c_x1", [128, 1024], F32, 2)
                tmpr = sring(nc, p1, "c_tmp", [128, 1024], F32, 1)
                ntmp = norm_tmp(nc, p1, "c")
                ppr = pring(nc, p1, "c_pp", [128, 1024], F32, 2)
                pOT = pring(nc, p1, "c_pOT", [128, 8, 128], BF16, 1)
                plg = pring(nc, p1, "c_plg", [128, 512], F32, 2)
                Lr = sring(nc, p1, "c_L", [128, 36], F32, 2)
                Lmr = sring(nc, p1, "c_Lm", [128, 32], F32, 2)
                smr = sring(nc, p1, "c_sm", [128, 24], F32, 2)
                e1r = sring(nc, p1, "c_e1", [128, 32], F32, 2)
                xss = {}

                def stage1(tl):
                        t = half * HT + tl
                        xt, bx = xr.next()
                        Ot, bOt = Otr.next()
                        S.dma("sp", lambda e, xt=xt, t=t: e.dma_start(out=xt[:], in_=xi_ap[t * 128:(t + 1) * 128, :]),
                              reads=[xi_b], writes=[bx])
                        S.dma("sp", lambda e, Ot=Ot, t=t: e.dma_start(out=Ot[:], in_=O_ap[t * 128:(t + 1) * 128, :]),
                              reads=[O_b], writes=[bOt])
                        pT, bpT = pOT.next()
                        for c in range(8):
                            S.op("pe", lambda e, pT=pT, Ot=Ot, c=c: e.transpose(out=pT[:, c, :], in_=Ot[:, c * 128:(c + 1) * 128],
                                                                              identity=ident[:]), reads=[bOt, bid], writes=[bpT])
                        OT, bOT = OTr.next()
                        S.op("act", lambda e, OT=OT, pT=pT: e.copy(out=OT[:], in_=pT[:]), reads=[bpT], writes=[bOT])
                        pp, bpp = ppr.next()
                        for nb in range(2):
                            for c in range(8):
                                S.op("pe", lambda e, pp=pp, OT=OT, c=c, nb=nb: e.matmul(
                                    pp[:, nb * 512:(nb + 1) * 512], lhsT=OT[:, c, :], rhs=Wout[:, c, nb * 512:(nb + 1) * 512],
                                    start=(c == 0), stop=(c == 7)), reads=[bOT, bWout], writes=[bpp])
                        tmp, btmp = tmpr.next()
                        x1, bx1 = x1r.next()
                        S.op("dve", lambda e, tmp=tmp, pp=pp: e.tensor_tensor(out=tmp[:], in0=pp[:], in1=g1b[:], op=ALU.mult),
                             reads=[bpp, bg1b], writes=[btmp])
                        S.op("dve", lambda e, tmp=tmp, x1=x1, xt=xt: e.tensor_tensor(out=x1[:], in0=tmp[:], in1=xt[:], op=ALU.add),
                             reads=[btmp, bx], writes=[bx1])
                        S.dma("pool", lambda e, x1=x1, t=t: e.dma_start(out=x1_ap[t * 128:(t + 1) * 128, :], in_=x1[:]),
                              reads=[bx1], writes=[x1_b])
                        xss[tl] = emit_norm_T_a(k, cst, x1[:], bx1, ntmp)

                def stage2(tl):
                    xs, bxs = xss[tl]
                    hT = AP(h2T, tl * 128, [[8 * HT * 128, 128], [HT * 128, 8], [1, 128]])
                    emit_norm_T_b(k, cst, xs, bxs, AB, bAB, 16, hT, bh2[tl], ntmp)
                    pl, bpl = plg.next()
                    for c in range(8):
                        S.op("pe", lambda e, pl=pl, tl=tl, c=c: e.matmul(
                            pl[:, 0:36], lhsT=h2T[:, c, tl * 128:(tl + 1) * 128], rhs=Wr[:, c, :],
                            start=(c == 0), stop=(c == 7)), reads=[bh2[tl], bWr], writes=[bpl])
                    L, bL = Lr.next()
                    Lm, bLm = Lmr.next()
                    sm, bsm = smr.next()
                    e1, be1 = e1r.next()
                    S.op("dve", lambda e, L=L, pl=pl: e.tensor_tensor(out=L[:], in0=pl[:, 0:36], in1=brt[:], op=ALU.add),
                         reads=[bpl, bbrt], writes=[bL])
                    S.op("dve", lambda e, L=L, sm=sm: e.tensor_reduce(out=sm[:, 0:1], in_=L[:, 0:4], axis=AX.X, op=ALU.max),
                         reads=[bL], writes=[bsm])
                    S.op("dve", lambda e, sm=sm: e.tensor_scalar(out=sm[:, 1:2], in0=sm[:, 0:1], scalar1=-1.0, scalar2=None,
                                                                 op0=ALU.mult), reads=[bsm], writes=[bsm])
                    S.op("act", lambda e, L=L, sm=sm: e.activation(out=sm[:, 20:24], in_=L[:, 0:4], func=AF.Exp, bias=sm[:, 1:2],
                                                                   scale=1.0, accum_out=sm[:, 2:3]), reads=[bL, bsm], writes=[bsm])
                    S.op("dve", lambda e, sm=sm: e.reciprocal(out=sm[:, 3:4], in_=sm[:, 2:3]), reads=[bsm], writes=[bsm])
                    S.op("dve", lambda e, L=L, sm=sm: e.tensor_scalar(out=sm[:, 4:8], in0=L[:, 0:4], scalar1=sm[:, 0:1], scalar2=None,
                                                                      op0=ALU.is_ge), reads=[bL, bsm], writes=[bsm])
                    S.op("dve", lambda e, sm=sm: e.tensor_scalar(out=sm[:, 4:8], in0=sm[:, 4:8], scalar1=1e30, scalar2=-1e30,
                                                                 op0=ALU.mult, op1=ALU.add), reads=[bsm], writes=[bsm])
                    S.op("dve", lambda e, L=L, Lm=Lm, sm=sm: e.tensor_tensor(
                        out=AP(Lm, 0, [[32, 128], [8, 4], [1, 8]]), in0=AP(L, 4, [[36, 128], [8, 4], [1, 8]]),
                        in1=AP(sm, 4, [[24, 128], [1, 4], [0, 8]]), op=ALU.add), reads=[bL, bsm], writes=[bLm])
                    S.op("dve", lambda e, Lm=Lm, sm=sm: e.max(out=sm[:, 8:16], in_=Lm[:]), reads=[bLm], writes=[bsm])
                    S.op("dve", lambda e, sm=sm: e.tensor_tensor(out=sm[:, 16:17], in0=sm[:, 9:10], in1=sm[:, 8:9], op=ALU.subtract),
                         reads=[bsm], writes=[bsm])
                    S.op("act", lambda e, sm=sm: e.activation(out=sm[:, 17:18], in_=sm[:, 16:17], func=AF.Exp), reads=[bsm], writes=[bsm])
                    S.op("dve", lambda e, sm=sm: e.tensor_scalar(out=sm[:, 18:19], in0=sm[:, 17:18], scalar1=1.0, scalar2=None,
                                                                 op0=ALU.add), reads=[bsm], writes=[bsm])
                    S.op("dve", lambda e, sm=sm: e.reciprocal(out=sm[:, 18:19], in_=sm[:, 18:19]), reads=[bsm], writes=[bsm])
                    S.op("dve", lambda e, sm=sm: e.tensor_tensor(out=sm[:, 18:19], in0=sm[:, 18:19], in1=sm[:, 3:4], op=ALU.mult),
                         reads=[bsm], writes=[bsm])
                    S.op("dve", lambda e, sm=sm: e.tensor_tensor(out=sm[:, 19:20], in0=sm[:, 18:19], in1=sm[:, 17:18], op=ALU.mult),
                         reads=[bsm], writes=[bsm])
                    S.op("dve", lambda e, Lm=Lm, sm=sm, e1=e1: e.tensor_scalar(out=e1[:], in0=Lm[:], scalar1=sm[:, 8:9], scalar2=sm[:, 18:19],
                                                                               op0=ALU.is_equal, op1=ALU.mult), reads=[bLm, bsm], writes=[be1])
                    S.op("dve", lambda e, Lm=Lm, sm=sm, tl=tl: e.tensor_scalar(out=comb[:, tl, :], in0=Lm[:], scalar1=sm[:, 9:10],
                                                                               scalar2=sm[:, 19:20], op0=ALU.is_equal, op1=ALU.mult),
                         reads=[bLm, bsm], writes=[bcomb[tl]])
                    S.op("dve", lambda e, e1=e1, tl=tl: e.tensor_tensor(out=comb[:, tl, :], in0=comb[:, tl, :], in1=e1[:], op=ALU.add),
                         reads=[be1, bcomb[tl]], writes=[bcomb[tl]])

                stage1(0)
                for tl in range(HT):
                    if tl + 1 < HT:
                        stage1(tl + 1)
                    stage2(tl)
                S.flush()
            with ExitStack() as p2:
                Wgr = sring(nc, p2, "c_wgu", [128, 8, 2, 512], BF16, 2)
                Wdr = sring(nc, p2, "c_wd", [128, 2, 2, 1024], BF16, 2)
                gur = pring(nc, p2, "c_gu", [128, 512], F32, 2)
                yr = pring(nc, p2, "c_y", [128, 1024], F32, 2)
                pATr = pring(nc, p2, "c_pAT", [128, 2, 128], BF16, 2)
                sgr = sring(nc, p2, "c_sg", [128, 256], F32, 4)
                Ar = sring(nc, p2, "c_A", [128, 256], BF16, 4)
                ATr = sring(nc, p2, "c_AT", [128, 2, 128], BF16, 4)
                units = []

                def stage_G(u):
                    (ep, tl, e2, Wg, bWg, Wd, bWd) = u["k"]
                    gu, bgu = gur.next()
                    for c in range(8):
                        S.op("pe", lambda e, c=c: e.matmul(
                            gu[:, :], lhsT=h2T[:, c, tl * 128:(tl + 1) * 128], rhs=Wg[:, c, e2, :],
                            start=(c == 0), stop=(c == 7)), reads=[bh2[tl], bWg], writes=[bgu])
                    sg, bsg = sgr.next()
                    A, bA = Ar.next()
                    ex = 2 * ep + e2
                    S.op("act", lambda e: e.activation(out=sg[:], in_=gu[:, 0:256], func=AF.Silu), reads=[bgu], writes=[bsg])
                    S.op("dve", lambda e: e.scalar_tensor_tensor(
                        out=A[:], in0=gu[:, 256:512], scalar=comb[:, tl, ex:ex + 1], in1=sg[:], op0=ALU.mult, op1=ALU.mult),
                        reads=[bgu, bsg, bcomb[tl]], writes=[bA])
                    u["A"] = (A, bA)

                def stage_T(u):
                    A, bA = u["A"]
                    pAT, bpAT = pATr.next()
                    for hh in range(2):
                        S.op("pe", lambda e, hh=hh: e.transpose(out=pAT[:, hh, :], in_=A[:, hh * 128:(hh + 1) * 128], identity=ident[:]),
                             reads=[bA, bid], writes=[bpAT])
                    AT, bAT = ATr.next()
                    S.op("act", lambda e: e.copy(out=AT[:], in_=pAT[:]), reads=[bpAT], writes=[bAT])
                    u["AT"] = (AT, bAT)

                ycur = {}

                def stage_D(u):
                    (ep, tl, e2, Wg, bWg, Wd, bWd) = u["k"]
                    AT, bAT = u["AT"]
                    if e2 == 0:
                        ycur[tl] = yr.next()
                    y, by = ycur[tl]
                    for nb in range(2):
                        for hh in range(2):
                            S.op("pe", lambda e, nb=nb, hh=hh: e.matmul(
                                y[:, nb * 512:(nb + 1) * 512], lhsT=AT[:, hh, :], rhs=Wd[:, e2, hh, nb * 512:(nb + 1) * 512],
                                start=(e2 == 0 and hh == 0), stop=(e2 == 1 and hh == 1)), reads=[bAT, bWd], writes=[by])
                    if e2 == 1:
                        if ep == 0:
                            S.op("dve", lambda e: e.tensor_copy(out=yacc[:, tl, :], in_=y[:]), reads=[by], writes=[byt[tl]])
                        else:
                            S.op("dve", lambda e: e.tensor_tensor(out=yacc[:, tl, :], in0=yacc[:, tl, :], in1=y[:], op=ALU.add),
                                 reads=[by, byt[tl]], writes=[byt[tl]])

                for ep in range(16):
                    Wg, bWg = Wgr.next()
                    Wd, bWd = Wdr.next()
                    for e2 in range(2):
                        ex = 2 * ep + e2
                        S.dma("pool", lambda e, Wg=Wg, ex=ex, e2=e2: e.dma_start(
                            out=Wg[:, :, e2, 0:256], in_=wg_ap[ex].rearrange("(c p) n -> p c n", p=128)),
                            reads=[wg_b], writes=[bWg])
                        S.dma("pool", lambda e, Wg=Wg, ex=ex, e2=e2: e.dma_start(
                            out=Wg[:, :, e2, 256:512], in_=wu_ap[ex].rearrange("(c p) n -> p c n", p=128)),
                            reads=[wu_b], writes=[bWg])
                        S.dma("pool", lambda e, Wd=Wd, ex=ex, e2=e2: e.dma_start(
                            out=Wd[:, e2, :, :], in_=wd_ap[ex].rearrange("(c p) n -> p c n", p=128)),
                            reads=[wd_b], writes=[bWd])
                    for tl in range(HT):
                        for e2 in range(2):
                            units.append({"k": (ep, tl, e2, Wg, bWg, Wd, bWd)})
                            n = len(units) - 1
                            stage_G(units[n])
                            if n >= 1:
                                stage_T(units[n - 1])
                            if n >= 2:
                                stage_D(units[n - 2])
                n = len(units)
                stage_T(units[n - 1])
                stage_D(units[n - 2])
                stage_D(units[n - 1])
                S.flush()
            with ExitStack() as p3:
                x1r = sring(nc, p3, "c3_x1", [128, 1024], F32, 2)
                tmpr = sring(nc, p3, "c3_tmp", [128, 1024], F32, 2)
                x2r = sring(nc, p3, "c3_x2", [128, 1024], F32, 2)
                sqr = sring(nc, p3, "c3_sq", [128, 1024], F32, 1)
                str_ = sring(nc, p3, "c3_st", [128, 2], F32, 2)
                for tl in range(HT):
                    t = half * HT + tl
                    x1, bx1 = x1r.next()
                    S.dma("sp", lambda e, x1=x1, t=t: e.dma_start(out=x1[:], in_=x1_ap[t * 128:(t + 1) * 128, :]),
                          reads=[x1_b], writes=[bx1])
                    tmp, btmp = tmpr.next()
                    x2, bx2 = x2r.next()
                    S.op("dve", lambda e, tmp=tmp, tl=tl: e.tensor_tensor(out=tmp[:], in0=yacc[:, tl, :], in1=g2b[:], op=ALU.mult),
                         reads=[byt[tl], bg2b], writes=[btmp])
                    S.op("dve", lambda e, tmp=tmp, x1=x1, x2=x2: e.tensor_tensor(out=x2[:], in0=tmp[:], in1=x1[:], op=ALU.add),
                         reads=[btmp, bx1], writes=[bx2])
                    if final:
                        sq, bsq = sqr.next()
                        st, bst = str_.next()
                        S.op("act", lambda e, sq=sq, x2=x2, st=st: e.activation(out=sq[:], in_=x2[:], func=AF.Square, accum_out=st[:, 0:1]),
                             reads=[bx2], writes=[bsq, bst])
                        S.op("act", lambda e, st=st: e.activation(out=st[:, 1:2], in_=st[:, 0:1], func=AF.Sqrt, scale=1.0 / D, bias=1e-6),
                             reads=[bst], writes=[bst])
                        S.op("dve", lambda e, st=st: e.reciprocal(out=st[:, 1:2], in_=st[:, 1:2]), reads=[bst], writes=[bst])
                        S.op("dve", lambda e, x2=x2, st=st, tmp=tmp: e.scalar_tensor_tensor(
                            out=tmp[:], in0=x2[:], scalar=st[:, 1:2], in1=fgb[:], op0=ALU.mult, op1=ALU.mult),
                            reads=[bx2, bst, bfgb], writes=[btmp])
                        S.dma("pool", lambda e, tmp=tmp, t=t: e.dma_start(out=xo_ap[t * 128:(t + 1) * 128, :], in_=tmp[:]),
                              reads=[btmp], writes=[xo_b])
                    else:
                        S.dma("pool", lambda e, x2=x2, t=t: e.dma_start(out=xo_ap[t * 128:(t + 1) * 128, :], in_=x2[:]),
                              reads=[bx2], writes=[xo_b])
                S.flush()


def make_dmask(j):
    m = np.zeros((128, 4, 128), np.float32)
    kk = np.arange(128)[:, None]
    qq = np.arange(128)[None, :]
    for u in range(4):
        if u == j:
            m[:, u, :] = np.where(kk > qq, NEG, 0.0)
        elif u > j:
            m[:, u, :] = NEG
    return m.astype(NPBF)


def declare_moe(k, l):
    k.din("moe_wr%d" % l, [1024, 36], F32)
    k.din("moe_br%d" % l, [128, 36], F32)
    k.din("moe_wg%d" % l, [32, 1024, 256], F32)
    k.din("moe_wu%d" % l, [32, 1024, 256], F32)
    k.din("moe_wd%d" % l, [32, 256, 1024], F32)


def moe_inputs(m, l, moe_w_group, moe_b_group, moe_w_expert, moe_b_expert, moe_w_gate, moe_w_up, moe_w_down):
    wr = np.concatenate([moe_w_group[l]] + [moe_w_expert[l, g] for g in range(4)], axis=1)
    br = np.concatenate([moe_b_group[l]] + [moe_b_expert[l, g] for g in range(4)], axis=0)
    m["moe_wr%d" % l] = np.ascontiguousarray(wr)
    m["moe_br%d" % l] = np.ascontiguousarray(np.broadcast_to(br[None, :], (128, 36)))
    m["moe_wg%d" % l] = moe_w_gate[l]
    m["moe_wu%d" % l] = moe_w_up[l]
    m["moe_wd%d" % l] = moe_w_down[l]


def build_L2(with_A1=False):
    nc = bass.Bass("TRN2", target_bir_lowering=False)
    with ExitStack() as es:
        k = K(nc, es)
        declare_common(k, [0, 1] if with_A1 else [0])
        k.din("x", [TOK, D], F32)
        k.din("QT0", [16, 65, TOK], BF16)
        k.din("KT0g", [4, 16, 64, TOK], BF16)
        k.din("V0g", [4, 8, 128, NT, 129], BF16)
        k.din("knm0g", [128, 4], F32)
        k.din("lam", [1, 256], F32)
        k.din("subg", [128, 128], F32)
        k.din("dmask", [128, 4, 128], BF16)
        k.din("diff_w_out", [D, D], F32)
        declare_moe(k, 0)
        k.dint("O0", [TOK, D], BF16)
        k.dint("x1s0", [TOK, D], F32)
        k.dout("xm0", [TOK, D], F32)
        cst = emit_consts(k, es)
        mod = emit_mod(k, es, cst, 0, "m0")
        phase_B0(k, cst)
        phase_C(k, cst, mod, 0, "O0", "x", "xm0", "diff_w_out", False)
    return nc


def phase_A1(k, cst, mod, cs, bcs, xin_name):
    nc, S = k.nc, k.S
    ident, bid = cst["ident"]
    AB, bAB = mod["AB"]
    x_ap, x_b = k.dram[xin_name]
    w_ap, w_b = k.dram["nsa_w_in"]
    QT_ap, QT_b = k.dram["QT1"]
    kc_ap, kc_b = k.dram["kcT1"]
    vc_ap, vc_b = k.dram["vcT1"]
    ks_ap, ks_b = k.dram["ksT1"]
    kw_ap, kw_b = k.dram["kwT1"]
    vs_ap, vs_b = k.dram["vs1"]
    vw_ap, vw_b = k.dram["vw1"]
    kn_ap, kn_b = k.dram["knm1"]
    gt_ap, gt_b = k.dram["gates1"]
    with ExitStack() as es:
        Win, bWin = sbt(nc, es, "a1win", [128, 8, 1840], BF16)
        load_cast_weight(k, w_ap, w_b, Win, bWin, 1840, piece=368)
        xr = sring(nc, es, "a1x", [128, 1024], F32, 2)
        hr = sring(nc, es, "a1hT", [128, 8, 128], BF16, 2)
        ntmp = norm_tmp(nc, es, "a1")
        ppr = pring(nc, es, "a1pp", [128, 1024], F32, 2)
        pTr = pring(nc, es, "a1pqt", [128, 16, 128], BF16, 1)
        Qsr = sring(nc, es, "a1qs", [128, 16, 65], BF16, 2)
        Rr = sring(nc, es, "a1r", [128, 8, 64], BF16, 2)
        Cr = sring(nc, es, "a1c", [128, 256], BF16, 2)
        R32r = sring(nc, es, "a1r32", [128, 512], F32, 2)
        QTr = sring(nc, es, "a1qts", [128, 16, 128], BF16, 2)
        KTr = sring(nc, es, "a1kts", [128, 6, 128], BF16, 2)
        Gr = sring(nc, es, "a1g", [128, 48], F32, 2)
        rtmp, brtmp = sbt(nc, es, "a1rt", [128, 4, 16, 8], F32)
        sqv, bsqv = sbt(nc, es, "a1sqv", [128, 16, 64], F32)
        nrm, bnrm = sbt(nc, es, "a1nrm", [128, 16], F32)
        knm, bknm = sbt(nc, es, "a1knm", [128, 4], F32)
        S.op("dve", lambda e: e.memset(knm[:], 0.0), writes=[bknm])
        hts = {}

        def normT(t):
            xt, bx = xr.next()
            S.dma("sp", lambda e: e.dma_start(out=xt[:], in_=x_ap[t * 128:(t + 1) * 128, :]), reads=[x_b], writes=[bx])
            hT, bhT = hr.next()
            emit_norm_T(k, cst, xt[:], bx, AB, bAB, 0, hT, bhT, ntmp)
            hts[t] = (hT, bhT)

        def qmm(t):
            hT, bhT = hts[t]
            pp, bpp = ppr.next()
            for nb in range(2):
                for c in range(8):
                    S.op("pe", lambda e, c=c, nb=nb: e.matmul(
                        pp[:, nb * 512:(nb + 1) * 512], lhsT=hT[:, c, :], rhs=Win[:, c, nb * 512:(nb + 1) * 512],
                        start=(c == 0), stop=(c == 7)), reads=[bhT, bWin], writes=[bpp])
            return pp, bpp

        def qpost(t, pp, bpp):
            src = lambda d0, d1: AP(pp, d0, [[1024, 128], [64, 16], [1, d1 - d0]])
            Qs, bQs = Qsr.next()
            dst = lambda d0, d1: AP(Qs, d0, [[1040, 128], [65, 16], [1, d1 - d0]])
            emit_rope(k, src, bpp, dst, bQs, 16, 65, cs, bcs, t, 16, rtmp, brtmp)
            S.op("act", lambda e: e.activation(out=dst(16, 64), in_=src(16, 64), func=AF.Copy, scale=0.125), reads=[bpp], writes=[bQs])
            S.op("dve", lambda e: e.tensor_tensor(out=sqv[:], in0=dst(0, 64), in1=dst(0, 64), op=ALU.mult), reads=[bQs], writes=[bsqv])
            S.op("dve", lambda e: e.tensor_reduce(out=nrm[:], in_=sqv[:], axis=AX.X, op=ALU.add), reads=[bsqv], writes=[bnrm])
            S.op("act", lambda e: e.activation(out=nrm[:], in_=nrm[:], func=AF.Sqrt), reads=[bnrm], writes=[bnrm])
            S.op("dve", lambda e: e.tensor_scalar(out=dst(64, 65), in0=AP(nrm, 0, [[16, 128], [1, 16], [1, 1]]),
                                                  scalar1=-1.0, scalar2=None, op0=ALU.mult), reads=[bnrm], writes=[bQs])
            return Qs, bQs

        def qtr(t, Qs, bQs):
            pT, bpT = pTr.next()
            for hc in range(16):
                S.op("pe", lambda e, hc=hc: e.transpose(out=pT[0:65, hc, :], in_=Qs[:, hc, :], identity=ident[:]),
                     reads=[bQs, bid], writes=[bpT])
            QTs, bQTs = QTr.next()
            S.op("act", lambda e: e.copy(out=QTs[0:65, :, :], in_=pT[0:65, :, :]), reads=[bpT], writes=[bQTs])
            S.dma("sp", lambda e: e.dma_start(out=QT_ap[:, :, t * 128:(t + 1) * 128].rearrange("h r q -> r h q"), in_=QTs[0:65, :, :]),
                  reads=[bQTs], writes=[QT_b])

        def kvmm(t):
            hT, bhT = hts[t]
            pp, bpp = ppr.next()
            for nb, (c0, c1) in enumerate(((1024, 1536), (1536, 1840))):
                for c in range(8):
                    S.op("pe", lambda e, c=c, nb=nb, c0=c0, c1=c1: e.matmul(
                        pp[:, nb * 512: nb * 512 + (c1 - c0)], lhsT=hT[:, c, :], rhs=Win[:, c, c0:c1],
                        start=(c == 0), stop=(c == 7)), reads=[bhT, bWin], writes=[bpp])
            return pp, bpp

        def kvpost(t, pp, bpp):
            Ct, bCt = Cr.next()
            S.op("act", lambda e: e.copy(out=Ct[:], in_=pp[:, 0:256]), reads=[bpp], writes=[bCt])
            R32, bR32 = R32r.next()
            S.op("act", lambda e: e.copy(out=R32[:], in_=pp[:, 256:768]), reads=[bpp], writes=[bR32])
            src = lambda d0, d1: AP(R32, d0, [[512, 128], [64, 8], [1, d1 - d0]])
            Rt, bRt = Rr.next()
            dst = lambda d0, d1: AP(Rt, d0, [[512, 128], [64, 8], [1, d1 - d0]])
            emit_rope(k, src, bR32, dst, bRt, 8, 64, cs, bcs, t, 0, rtmp, brtmp)
            S.op("act", lambda e: e.copy(out=dst(16, 64), in_=src(16, 64)), reads=[bR32], writes=[bRt])
            Gt, bGt = Gr.next()
            S.op("act", lambda e: e.activation(out=Gt[:], in_=pp[:, 768:816], func=AF.Sigmoid), reads=[bpp], writes=[bGt])
            S.dma("pool", lambda e: e.dma_start(out=gt_ap[t * 128:(t + 1) * 128, :], in_=Gt[:]), reads=[bGt], writes=[gt_b])
            S.op("dve", lambda e: e.tensor_tensor(out=sqv[:, 0:8, :], in0=Rt[:], in1=Rt[:], op=ALU.mult), reads=[bRt], writes=[bsqv])
            S.op("dve", lambda e: e.tensor_reduce(out=nrm[:, 0:8], in_=sqv[:, 0:8, :], axis=AX.X, op=ALU.add), reads=[bsqv], writes=[bnrm])
            S.op("dve", lambda e: e.tensor_reduce(out=knm[:, 2:3], in_=nrm[:, 0:2], axis=AX.X, op=ALU.max), reads=[bnrm], writes=[bknm])
            S.op("dve", lambda e: e.tensor_reduce(out=knm[:, 3:4], in_=nrm[:, 4:6], axis=AX.X, op=ALU.max), reads=[bnrm], writes=[bknm])
            S.op("dve", lambda e: e.tensor_tensor(out=knm[:, 0:2], in0=knm[:, 0:2], in1=knm[:, 2:4], op=ALU.max), reads=[bknm], writes=[bknm])
            return Ct, bCt, Rt, bRt

        def kvtr(t, Ct, bCt, Rt, bRt):
            pT, bpT = pTr.next()
            S.op("pe", lambda e: e.transpose(out=pT[:, 0, :], in_=Ct[:, 0:128], identity=ident[:]), reads=[bCt, bid], writes=[bpT])
            S.op("pe", lambda e: e.transpose(out=pT[:, 1, :], in_=Ct[:, 128:256], identity=ident[:]), reads=[bCt, bid], writes=[bpT])
            for ii, hh in enumerate((0, 1, 4, 5)):
                S.op("pe", lambda e, ii=ii, hh=hh: e.transpose(out=pT[0:64, 2 + ii, :], in_=Rt[:, hh, :], identity=ident[:]),
                     reads=[bRt, bid], writes=[bpT])
            KTs, bKTs = KTr.next()
            S.op("act", lambda e: e.copy(out=KTs[:, 0:2, :], in_=pT[:, 0:2, :]), reads=[bpT], writes=[bKTs])
            S.op("act", lambda e: e.copy(out=KTs[0:64, 2:6, :], in_=pT[0:64, 2:6, :]), reads=[bpT], writes=[bKTs])
            sl = slice(t * 128, (t + 1) * 128)
            S.dma("sp", lambda e: e.dma_start(out=kc_ap[:, sl], in_=KTs[:, 0, :]), reads=[bKTs], writes=[kc_b])
            S.dma("sp", lambda e: e.dma_start(out=vc_ap[:, sl], in_=KTs[:, 1, :]), reads=[bKTs], writes=[vc_b])
            S.dma("sp", lambda e: e.dma_start(out=ks_ap[:, :, sl].rearrange("g d q -> d g q"), in_=KTs[0:64, 2:4, :]), reads=[bKTs], writes=[ks_b])
            S.dma("sp", lambda e: e.dma_start(out=kw_ap[:, :, sl].rearrange("g d q -> d g q"), in_=KTs[0:64, 4:6, :]), reads=[bKTs], writes=[kw_b])
            S.dma("pool", lambda e: e.dma_start(out=vs_ap[sl, :].rearrange("p (g d) -> p g d", g=2), in_=Rt[:, 2:4, :]), reads=[bRt], writes=[vs_b])
            S.dma("pool", lambda e: e.dma_start(out=vw_ap[sl, :].rearrange("p (g d) -> p g d", g=2), in_=Rt[:, 6:8, :]), reads=[bRt], writes=[vw_b])

        normT(0)
        for t in range(NT):
            ppq = qmm(t)
            Qs = qpost(t, *ppq)
            ppkv = kvmm(t)
            kv = kvpost(t, *ppkv)
            qtr(t, *Qs)
            if t + 1 < NT:
                normT(t + 1)
            kvtr(t, *kv)
        S.dma("sp", lambda e: e.dma_start(out=kn_ap, in_=knm[:, 0:2]), reads=[bknm], writes=[kn_b])
        S.flush()


def emit_kmax_multi(k, es, cst, knm_name, tag, G):
    nc, S = k.nc, k.S
    kn_ap, kn_b = k.dram[knm_name]
    identf, bidf = cst["identf"]
    ones, bones = cst["ones"]
    kmx, bkmx = sbt(nc, es, "kmx" + tag, [128, G], F32)
    with ExitStack() as ps:
        a, ba = sbt(nc, ps, "kma" + tag, [128, G, 4], F32)
        m, bm = sbt(nc, ps, "kmm" + tag, [128, G], F32)
        r, br = sbt(nc, ps, "kmr" + tag, [1, G, 128], F32)
        s, bs = sbt(nc, ps, "kms" + tag, [1, G], F32)
        p1, bp1 = pst(nc, ps, "kmp" + tag, [128, 512], F32)
        S.dma("sp", lambda e: e.dma_start(out=a[:], in_=kn_ap.rearrange("p (g r) -> p g r", g=G)), reads=[kn_b], writes=[ba])
        S.op("dve", lambda e: e.tensor_reduce(out=m[:], in_=a[:], axis=AX.X, op=ALU.max), reads=[ba], writes=[bm])
        for g in range(G):
            S.op("pe", lambda e, g=g: e.transpose(out=p1[0:1, g * 128:(g + 1) * 128], in_=m[:, g:g + 1], identity=identf[:]),
                 reads=[bm, bidf], writes=[bp1])
        S.op("dve", lambda e: e.tensor_copy(out=r[:], in_=p1[0:1, 0:G * 128]), reads=[bp1], writes=[br])
        S.op("dve", lambda e: e.tensor_reduce(out=s[:], in_=r[:], axis=AX.X, op=ALU.max), reads=[br], writes=[bs])
        S.op("act", lambda e: e.activation(out=s[:], in_=s[:], func=AF.Sqrt, scale=1.02), reads=[bs], writes=[bs])
        S.op("pe", lambda e: e.matmul(p1[:, 384:384 + G], lhsT=ones[0:1, :], rhs=s[0:1, 0:G], start=True, stop=True),
             reads=[bs, bones, bp1], writes=[bp1])
        S.op("dve", lambda e: e.tensor_copy(out=kmx[:], in_=p1[:, 384:384 + G]), reads=[bp1], writes=[bkmx])
        S.flush()
    return kmx, bkmx


def load_global_T(k, dst, bdst, nrows, p0, src_ap, src_b, width):
    S = k.S
    for r in range(4):
        S.dma("sp", lambda e, r=r: e.dma_start(
            out=AP(dst, p0 * width + r * 128, [[width, nrows], [512, NT], [1, 128]]),
            in_=src_ap(r).rearrange("d (i p) -> d i p", p=128)), reads=[src_b], writes=[bdst])


def phase_B1(k, cst):
    nc, S = k.nc, k.S
    ident, bid = cst["ident"]
    ones, bones = cst["ones"]
    QT_ap, QT_b = k.dram["QT1"]
    kc_ap, kc_b = k.dram["kcT1g"]
    vc_ap, vc_b = k.dram["vcT1g"]
    ks_ap, ks_b = k.dram["ksT1g"]
    kw_ap, kw_b = k.dram["kwT1g"]
    vs_ap, vs_b = k.dram["vs1g"]
    vw_ap, vw_b = k.dram["vw1g"]
    gt_ap, gt_b = k.dram["gates1"]
    O_ap, O_b = k.dram["O1"]
    w1_ap, w1_b = k.dram["cmp_w1"]
    b1_ap, b1_b = k.dram["cmp_b1c"]
    w2_ap, w2_b = k.dram["cmp_w2"]
    pos_ap, pos_b = k.dram["cmp_posT"]
    em_ap, em_b = k.dram["emat"]
    dm_ap, dm_b = k.dram["dmaskb"]
    wm_ap, wm_b = k.dram["wmaskb"]
    cm_ap, cm_b = k.dram["cmaskb"]
    cb_ap, cb_b = k.dram["cmaskTb"]
    cf_ap, cf_b = k.dram["cforce"]
    W = SEQ
    with ExitStack() as es:
        kmx, bkmx = emit_kmax_multi(k, es, cst, "knm1g", "b1", 2)
        kcmpT, bkcmpT = sbt(nc, es, "b1kcmpT", [65, 2, 1024], BF16)
        vcmp, bvcmp = sbt(nc, es, "b1vcmp", [128, 8, 2, 65], BF16)
        gts, bgts = sbt(nc, es, "b1gts", [128, NT, 48], F32)
        S.dma("sp", lambda e: e.dma_start(out=gts[:], in_=gt_ap.rearrange("(t p) c -> p t c", p=128)), reads=[gt_b], writes=[bgts])
        S.op("dve", lambda e: e.memset(kcmpT[:], 0.0), writes=[bkcmpT])
        S.op("dve", lambda e: e.memset(vcmp[:], 0.0), writes=[bvcmp])
        S.op("dve", lambda e: e.memset(vcmp[:, :, :, 64:65], 1.0), writes=[bvcmp])
        with ExitStack() as ps:
            cT, bcT = sbt(nc, ps, "b1cT", [128, W], BF16)
            W1, bW1 = sbt(nc, ps, "b1W1", [128, 32, 256], BF16)
            W2, bW2 = sbt(nc, ps, "b1W2", [128, 2, 64], BF16)
            posT, bposT = sbt(nc, ps, "b1posT", [128, 32], BF16)
            b1c, bb1c = sbt(nc, ps, "b1b1c", [128, 4], F32)
            cbias, bcbias = sbt(nc, ps, "b1cbias", [128, 2], F32)
            Hd, bHd = sbt(nc, ps, "b1Hd", [128, 2, 1024], BF16)
            ur = sring(nc, ps, "b1u", [128, 512], F32, 2)
            tr_ = sring(nc, ps, "b1t", [128, 512], F32, 2)
            sqk, bsqk = sbt(nc, ps, "b1sqk", [64, 512], BF16)
            kn, bkn = sbt(nc, ps, "b1kn", [1, 8], F32)
            onesb, bonesb = sbt(nc, ps, "b1onesb", [128, 1], BF16)
            ph = pring(nc, ps, "b1ph", [128, 512], F32, 2)
            pk = pring(nc, ps, "b1pk", [128, 512], F32, 2)
            pb, bpb = pst(nc, ps, "b1pb", [128, 512], F32)
            S.op("dve", lambda e: e.memset(onesb[:], 1.0), writes=[bonesb])
            S.op("dve", lambda e: e.memset(Hd[:], 0.0), writes=[bHd])
            S.op("dve", lambda e: e.memset(kn[:], 0.0), writes=[bkn])
            S.dma("sp", lambda e: e.dma_start(out=b1c[:], in_=b1_ap), reads=[b1_b], writes=[bb1c])
            for kv in range(2):
                src_ap, src_b = (kc_ap, kc_b) if kv == 0 else (vc_ap, vc_b)
                load_global_T(k, cT, bcT, 128, 0, lambda r, src_ap=src_ap: src_ap[r], src_b, W)
                for half in range(2):
                    S.dma("pool", lambda e, kv=kv, half=half: e.dma_start(
                        out=W1[half * 64:(half + 1) * 64, :, :], in_=w1_ap[kv].rearrange("(j d) n -> d j n", d=64)),
                        reads=[w1_b], writes=[bW1])
                    S.dma("pool", lambda e, kv=kv, half=half: e.dma_start(
                        out=posT[half * 64:(half + 1) * 64, :], in_=pos_ap[kv]), reads=[pos_b], writes=[bposT])
                S.dma("pool", lambda e, kv=kv: e.dma_start(out=W2[:], in_=w2_ap[kv].rearrange("(c p) n -> p c n", p=128)),
                      reads=[w2_b], writes=[bW2])
                for half in range(2):
                    for j in range(32):
                        S.op("pe", lambda e, half=half, j=j: e.matmul(
                            pb[:, half:half + 1], lhsT=W1[0:64, j, half * 128:(half + 1) * 128], rhs=posT[0:64, j:j + 1],
                            start=(j == 0 and half == 0), stop=(j == 31), skip_group_check=True), reads=[bW1, bposT], writes=[bpb])
                S.op("dve", lambda e, kv=kv: e.tensor_tensor(out=cbias[:], in0=pb[:, 0:2], in1=b1c[:, kv * 2:kv * 2 + 2], op=ALU.add),
                     reads=[bpb, bb1c], writes=[bcbias])
                for g in range(2):
                    for half in range(2):
                        for ci, (n0, nn) in enumerate(((0, 512), (512, 511))):
                            pt, bpt = ph.next()
                            for j in range(32):
                                S.op("pe", lambda e, pt=pt, g=g, half=half, n0=n0, nn=nn, j=j: e.matmul(
                                    pt[:, 0:nn], lhsT=W1[g * 64:(g + 1) * 64, j, half * 128:(half + 1) * 128],
                                    rhs=AP(cT, g * 64 * W + 16 * n0 + j * 16 // 16 * 0 + j, [[W, 64], [16, nn]]),
                                    start=(j == 0), stop=(j == 31)), reads=[bW1, bcT], writes=[bpt])
                            u, bu = ur.next()
                            tt, btt = tr_.next()
                            S.op("act", lambda e, u=u, pt=pt, nn=nn, half=half: e.activation(
                                out=u[:, 0:nn], in_=pt[:, 0:nn], func=AF.Identity, bias=cbias[:, half:half + 1], scale=1.0),
                                reads=[bpt, bcbias], writes=[bu])
                            S.op("dve", lambda e, u=u, tt=tt, nn=nn: e.tensor_tensor(out=tt[:, 0:nn], in0=u[:, 0:nn], in1=u[:, 0:nn], op=ALU.mult),
                                 reads=[bu], writes=[btt])
                            S.op("dve", lambda e, tt=tt, nn=nn: e.tensor_scalar(out=tt[:, 0:nn], in0=tt[:, 0:nn], scalar1=0.044715, scalar2=1.0,
                                                                                op0=ALU.mult, op1=ALU.add), reads=[btt], writes=[btt])
                            S.op("dve", lambda e, u=u, tt=tt, nn=nn: e.tensor_tensor(out=tt[:, 0:nn], in0=tt[:, 0:nn], in1=u[:, 0:nn], op=ALU.mult),
                                 reads=[bu, btt], writes=[btt])
                            S.op("act", lambda e, tt=tt, nn=nn: e.activation(out=tt[:, 0:nn], in_=tt[:, 0:nn], func=AF.Tanh,
                                                                             scale=0.7978845608028654), reads=[btt], writes=[btt])
                            S.op("dve", lambda e, u=u, tt=tt, nn=nn: e.scalar_tensor_tensor(
                                out=tt[:, 0:nn], in0=tt[:, 0:nn], scalar=1.0, in1=u[:, 0:nn], op0=ALU.add, op1=ALU.mult),
                                reads=[bu, btt], writes=[btt])
                            S.op("act", lambda e, tt=tt, nn=nn, half=half, n0=n0: e.activation(
                                out=Hd[:, half, n0:n0 + nn], in_=tt[:, 0:nn], func=AF.Copy, scale=0.5), reads=[btt], writes=[bHd])
                    if kv == 0:
                        for ci, (n0, nn) in enumerate(((0, 512), (512, 511))):
                            pt, bpt = pk.next()
                            for half in range(2):
                                S.op("pe", lambda e, pt=pt, half=half, n0=n0, nn=nn: e.matmul(
                                    pt[0:64, 0:nn], lhsT=W2[:, half, :], rhs=Hd[:, half, n0:n0 + nn], start=(half == 0), stop=(half == 1)),
                                    reads=[bW2, bHd], writes=[bpt])
                            S.op("act", lambda e, pt=pt, g=g, n0=n0, nn=nn: e.copy(out=kcmpT[0:64, g, n0:n0 + nn], in_=pt[0:64, 0:nn]),
                                 reads=[bpt], writes=[bkcmpT])
                            S.op("dve", lambda e, g=g, n0=n0, nn=nn: e.tensor_tensor(
                                out=sqk[:, 0:nn], in0=kcmpT[0:64, g, n0:n0 + nn], in1=kcmpT[0:64, g, n0:n0 + nn], op=ALU.mult),
                                reads=[bkcmpT], writes=[bsqk])
                            pt2, bpt2 = pk.next()
                            S.op("pe", lambda e, pt2=pt2, nn=nn: e.matmul(pt2[0:1, 0:nn], lhsT=onesb[0:64, 0:1], rhs=sqk[:, 0:nn],
                                                                          start=True, stop=True), reads=[bsqk, bonesb], writes=[bpt2])
                            S.op("dve", lambda e, pt2=pt2, nn=nn, g=g, ci=ci: e.tensor_reduce(
                                out=kn[0:1, g * 2 + ci:g * 2 + ci + 1], in_=pt2[0:1, 0:nn], axis=AX.X, op=ALU.max), reads=[bpt2], writes=[bkn])
                    else:
                        for nt in range(8):
                            pt, bpt = pk.next()
                            for half in range(2):
                                S.op("pe", lambda e, pt=pt, half=half, nt=nt: e.matmul(
                                    pt[:, 0:64], lhsT=Hd[:, half, nt * 128:(nt + 1) * 128], rhs=W2[:, half, :], start=(half == 0), stop=(half == 1)),
                                    reads=[bW2, bHd], writes=[bpt])
                            S.op("act", lambda e, pt=pt, g=g, nt=nt: e.copy(out=vcmp[:, nt, g, 0:64], in_=pt[:, 0:64]),
                                 reads=[bpt], writes=[bvcmp])
            S.op("dve", lambda e: e.tensor_reduce(out=kn[0:1, 4:5], in_=kn[0:1, 0:4], axis=AX.X, op=ALU.max), reads=[bkn], writes=[bkn])
            S.op("act", lambda e: e.activation(out=kn[0:1, 4:5], in_=kn[0:1, 4:5], func=AF.Sqrt, scale=1.05), reads=[bkn], writes=[bkn])
            S.op("pe", lambda e: e.matmul(pb[:, 8:9], lhsT=ones[0:1, :], rhs=kn[0:1, 4:5], start=True, stop=True),
                 reads=[bkn, bones], writes=[bpb])
            S.op("dve", lambda e: e.tensor_copy(out=cbias[:, 0:1], in_=pb[:, 8:9]), reads=[bpb], writes=[bcbias])
            S.op("dve", lambda e: e.tensor_scalar(
                out=AP(kcmpT, 64 * 2048, [[2048, 1], [1, 2048]]), in0=AP(ones, 64 * 128, [[128, 1], [0, 2048]]),
                scalar1=cbias[64:65, 0:1], scalar2=None, op0=ALU.mult), reads=[bones, bcbias], writes=[bkcmpT])
            S.flush()
        emat, bemat = sbt(nc, es, "b1emat", [128, 64, 128], BF16)
        dmask, bdmask = sbt(nc, es, "b1dmask", [128, 4, 128], BF16)
        wmask, bwmask = sbt(nc, es, "b1wmask", [128, 8, 128], BF16)
        cmask, bcmask = sbt(nc, es, "b1cmask", [128, 8, 128], BF16)
        cmTb, bcmTb = sbt(nc, es, "b1cmTb", [128, 8, 128], BF16)
        for (tt_, bb_, ap_, b_) in ((emat, bemat, em_ap, em_b), (dmask, bdmask, dm_ap, dm_b), (wmask, bwmask, wm_ap, wm_b),
                                    (cmask, bcmask, cm_ap, cm_b), (cmTb, bcmTb, cb_ap, cb_b)):
            S.dma("sp", lambda e, tt_=tt_, ap_=ap_: e.dma_start(out=tt_[:], in_=ap_), reads=[b_], writes=[bb_])
        ksT, bksT = sbt(nc, es, "b1ksT", [65, W], BF16)
        kwT, bkwT = sbt(nc, es, "b1kwT", [65, W], BF16)
        vsS, bvsS = sbt(nc, es, "b1vs", [128, 128, 65], BF16)
        vwS, bvwS = sbt(nc, es, "b1vw", [128, 128, 65], BF16)
        Qr = sring(nc, es, "b1q", [65, 8, 128], BF16, 2)
        PTr = sring(nc, es, "b1pt", [128, 8, 128], BF16, 3)
        P2r = sring(nc, es, "b1p2", [128, 1024], F32, 2)
        Pn, bPn = sbt(nc, es, "b1pn", [128, 1024], F32)
        imp, bimp = sbt(nc, es, "b1imp", [128, 256], F32)
        imp2, bimp2 = sbt(nc, es, "b1imp2", [128, 256], F32)
        selA, bselA = sbt(nc, es, "b1selA", [128, 256], F32)
        selb, bselb = sbt(nc, es, "b1selb", [128, 256], F32)
        selT, bselT = sbt(nc, es, "b1selT", [128, 2, 128], BF16)
        m8, bm8 = sbt(nc, es, "b1m8", [128, 16], F32)
        l2, bl2 = sbt(nc, es, "b1l2", [128, 16], F32)
        cfr = sring(nc, es, "b1cf", [128, 512], F32, 2)
        obr = [sbt(nc, es, "b1ob%d" % i, [128, 8, 65], F32) for i in range(3)]
        wgt, bwgt = sbt(nc, es, "b1wgt", [128, 3, 8], F32)
        ot1, bot1 = sbt(nc, es, "b1ot1", [128, 8, 64], F32)
        ot2, bot2 = sbt(nc, es, "b1ot2", [128, 8, 64], F32)
        Otr = sring(nc, es, "b1o", [128, 8, 64], BF16, 2)
        STr = pring(nc, es, "b1st", [128, 1024], F32, 2)
        acc, bacc = pst(nc, es, "b1acc", [128, 1024], F32)
        pmisc, _ = pst(nc, es, "b1pmisc", [128, 512], F32)
        pm = Ring([(pmisc[:, 0:128], Buf()), (pmisc[:, 128:256], Buf())])
        psT, bpsT = pmisc[:, 256:512], Buf()
        identf, bidf = cst["identf"]
        S.op("pool", lambda e: e.memset(vsS[:, :, 64:65], 1.0), writes=[bvsS])
        S.op("pool", lambda e: e.memset(vwS[:, :, 64:65], 1.0), writes=[bvwS])
        S.op("dve", lambda e: e.tensor_scalar(out=AP(ksT, 64 * W, [[W, 1], [1, W]]), in0=AP(ones, 64 * 128, [[128, 1], [0, W]]),
                                              scalar1=kmx[64:65, 0:1], scalar2=None, op0=ALU.mult), reads=[bones, bkmx], writes=[bksT])
        S.op("dve", lambda e: e.tensor_scalar(out=AP(kwT, 64 * W, [[W, 1], [1, W]]), in0=AP(ones, 64 * 128, [[128, 1], [0, W]]),
                                              scalar1=kmx[64:65, 1:2], scalar2=None, op0=ALU.mult), reads=[bones, bkmx], writes=[bkwT])

        def attend(Qg, bQ, KT_t, bKT, Vt, bV, kts, maskfn, ob, bob):
            pend = []

            def pv(item):
                (PT, bPT, kt, first) = item
                for h in range(8):
                    S.op("pe", lambda e, h=h: e.matmul(
                        acc[:, (h // 4) * 512 + (h % 4) * 65:(h // 4) * 512 + (h % 4) * 65 + 65], lhsT=PT[:, h, :], rhs=Vt(kt),
                        start=(first and h % 4 == 0), stop=False, skip_group_check=True), reads=[bPT, bV], writes=[bacc])

            for idx, kt in enumerate(kts):
                ST, bST = STr.next()
                mks = maskfn(idx, kt)
                for hh in range(2):
                    S.op("pe", lambda e, ST=ST, kt=kt, hh=hh, mks=mks: e.matmul(
                        ST[:, hh * 512:(hh + 1) * 512], lhsT=KT_t[0:65, kt * 128:(kt + 1) * 128], rhs=Qg[0:65, hh * 4:(hh + 1) * 4, :],
                        start=True, stop=(len(mks) == 0)), reads=[bKT, bQ], writes=[bST])
                    for mi, (ml, mr, mb) in enumerate(mks):
                        S.op("pe", lambda e, ST=ST, hh=hh, ml=ml, mr=mr, mi=mi, mks=mks: e.matmul(
                            ST[:, hh * 512:(hh + 1) * 512], lhsT=ml, rhs=mr, start=False, stop=(mi == len(mks) - 1)),
                            reads=mb, writes=[bST])
                PT, bPT = PTr.next()
                S.op("act", lambda e, ST=ST, PT=PT: e.activation(out=PT[:], in_=ST[:], func=AF.Exp), reads=[bST], writes=[bPT])
                pend.append((PT, bPT, kt, idx == 0))
                if len(pend) > 1:
                    pv(pend.pop(0))
            while pend:
                pv(pend.pop(0))
            S.op("dve", lambda e: e.tensor_copy(out=ob[:, 0:4, :], in_=acc[:, 0:260]), reads=[bacc], writes=[bob])
            S.op("dve", lambda e: e.tensor_copy(out=ob[:, 4:8, :], in_=acc[:, 512:772]), reads=[bacc], writes=[bob])

        def bc4(t_, off, pstride):
            return AP(t_, off, [[pstride, 128], [0, 4], [1, 128]])

        for g in range(2):
            load_global_T(k, ksT, bksT, 64, 0, lambda r, g=g: ks_ap[r, g], ks_b, W)
            load_global_T(k, kwT, bkwT, 64, 0, lambda r, g=g: kw_ap[r, g], kw_b, W)
            for r in range(4):
                S.dma("sp", lambda e, r=r, g=g: e.dma_start(
                    out=AP(vsS, r * 65, [[128 * 65, 128], [4 * 65, NT], [1, 64]]),
                    in_=vs_ap[r, :, g * 64:(g + 1) * 64].rearrange("(i p) d -> p i d", p=128)), reads=[vs_b], writes=[bvsS])
                S.dma("sp", lambda e, r=r, g=g: e.dma_start(
                    out=AP(vwS, r * 65, [[128 * 65, 128], [4 * 65, NT], [1, 64]]),
                    in_=vw_ap[r, :, g * 64:(g + 1) * 64].rearrange("(i p) d -> p i d", p=128)), reads=[vw_b], writes=[bvwS])
            for i in range(NT):
                Qg, bQ = Qr.next()
                S.dma("sp", lambda e, Qg=Qg, g=g, i=i: e.dma_start(
                    out=Qg[:, :, :], in_=QT_ap[8 * g:8 * g + 8, :, i * 128:(i + 1) * 128].rearrange("h r q -> r h q")),
                    reads=[QT_b], writes=[bQ])
                cf, bcf = cfr.next()
                S.dma("sp", lambda e, cf=cf, i=i: e.dma_start(out=cf[:], in_=cf_ap[i]), reads=[cf_b], writes=[bcf])
                nts = i // 4 + 1
                ncol = nts * 128
                attend(Qg, bQ, AP(kcmpT, g * 1024, [[2048, 65], [1, 1024]]), bkcmpT, lambda kt, g=g: vcmp[:, kt, g, :], bvcmp,
                       list(range(nts)),
                       lambda idx, kt, i=i, nts=nts: ([(ident[:], bc4(cmask, ((i % 4) * 2 + (nts - 1 - kt)) * 128, 1024), [bid, bcmask])]
                                                      if kt >= nts - 2 else []),
                       obr[0][0], obr[0][1])
                for h in range(8):
                    S2, bS2 = STr.next()
                    for c0 in range(0, ncol, 512):
                        c1 = min(ncol, c0 + 512)
                        lastc = (c1 == ncol)
                        S.op("pe", lambda e, S2=S2, h=h, c0=c0, c1=c1, g=g, lastc=lastc: e.matmul(
                            S2[:, c0:c1], lhsT=Qg[0:65, h, :], rhs=kcmpT[0:65, g, c0:c1], start=True, stop=not lastc),
                            reads=[bQ, bkcmpT], writes=[bS2])
                    S.op("pe", lambda e, S2=S2, i=i, ncol=ncol: e.matmul(
                        S2[:, ncol - 128:ncol], lhsT=ident[:], rhs=cmTb[:, (i % 4) * 2, :], start=False, stop=True),
                        reads=[bid, bcmTb], writes=[bS2])
                    if nts >= 2:
                        S.op("pe", lambda e, S2=S2, i=i, ncol=ncol: e.matmul(
                            S2[:, ncol - 256:ncol - 128], lhsT=ident[:], rhs=cmTb[:, (i % 4) * 2 + 1, :], start=False, stop=True),
                            reads=[bid, bcmTb], writes=[bS2])
                    P2, bP2 = P2r.next()
                    S.op("act", lambda e, S2=S2, P2=P2, ncol=ncol, h=h: e.activation(
                        out=P2[:, 0:ncol], in_=S2[:, 0:ncol], func=AF.Exp, accum_out=l2[:, h:h + 1]), reads=[bS2], writes=[bP2, bl2])
                    S.op("dve", lambda e, h=h: e.tensor_scalar(out=l2[:, 8 + h:9 + h], in0=l2[:, h:h + 1], scalar1=1e-30, scalar2=None,
                                                               op0=ALU.max), reads=[bl2], writes=[bl2])
                    S.op("dve", lambda e, h=h: e.reciprocal(out=l2[:, 8 + h:9 + h], in_=l2[:, 8 + h:9 + h]), reads=[bl2], writes=[bl2])
                    if h == 0:
                        S.op("dve", lambda e, P2=P2, ncol=ncol, h=h: e.tensor_scalar(
                            out=Pn[:, 0:ncol], in0=P2[:, 0:ncol], scalar1=l2[:, 8 + h:9 + h], scalar2=None, op0=ALU.mult),
                            reads=[bP2, bl2], writes=[bPn])
                    else:
                        S.op("dve", lambda e, P2=P2, ncol=ncol, h=h: e.scalar_tensor_tensor(
                            out=Pn[:, 0:ncol], in0=P2[:, 0:ncol], scalar=l2[:, 8 + h:9 + h], in1=Pn[:, 0:ncol], op0=ALU.mult, op1=ALU.add),
                            reads=[bP2, bl2, bPn], writes=[bPn])
                ns = ncol // 4
                S.op("dve", lambda e: e.memset(imp[:], 0.0), writes=[bimp])
                S.op("dve", lambda e, ns=ns: e.tensor_reduce(out=imp[:, 0:ns], in_=AP(Pn, 0, [[1024, 128], [4, ns], [1, 4]]), axis=AX.X, op=ALU.add),
                     reads=[bPn], writes=[bimp])
                S.op("dve", lambda e, ns=ns: e.tensor_tensor(out=imp[:, 1:ns], in0=imp[:, 1:ns], in1=AP(Pn, 3, [[1024, 128], [4, ns - 1]]), op=ALU.add),
                     reads=[bPn, bimp], writes=[bimp])
                S.op("dve", lambda e, cf=cf: e.tensor_tensor(out=imp[:], in0=imp[:], in1=cf[:, 0:256], op=ALU.mult), reads=[bimp, bcf], writes=[bimp])
                S.op("dve", lambda e, cf=cf: e.tensor_tensor(out=imp[:], in0=imp[:], in1=cf[:, 256:512], op=ALU.add), reads=[bimp, bcf], writes=[bimp])
                S.op("dve", lambda e: e.max(out=m8[:, 0:8], in_=imp[:]), reads=[bimp], writes=[bm8])
                S.op("dve", lambda e: e.match_replace(out=imp2[:], in_to_replace=m8[:, 0:8], in_values=imp[:], imm_value=-30000.0),
                     reads=[bimp, bm8], writes=[bimp2])
                S.op("dve", lambda e: e.max(out=m8[:, 8:16], in_=imp2[:]), reads=[bimp2], writes=[bm8])
                S.op("dve", lambda e: e.tensor_scalar(out=selA[:], in0=imp[:], scalar1=m8[:, 15:16], scalar2=None, op0=ALU.is_ge),
                     reads=[bimp, bm8], writes=[bselA])
                S.op("dve", lambda e: e.tensor_scalar(out=imp2[:], in0=imp[:], scalar1=-5000.0, scalar2=None, op0=ALU.is_gt),
                     reads=[bimp], writes=[bimp2])
                S.op("dve", lambda e: e.tensor_tensor(out=selb[:], in0=selA[:], in1=imp2[:], op=ALU.mult), reads=[bselA, bimp2], writes=[bselb])
                wk = [4 * (i - 1) + u for u in range(8) if 4 * (i - 1) + u >= 0]
                attend(Qg, bQ, kwT, bkwT, lambda kt: vwS[:, kt, :], bvwS, wk,
                       lambda idx, kt, i=i: [(ident[:], bc4(wmask, (kt - 4 * (i - 1)) * 128, 1024), [bid, bwmask])],
                       obr[2][0], obr[2][1])
                for c in range(2):
                    S.op("pe", lambda e, c=c: e.transpose(out=psT[:, c * 128:(c + 1) * 128], in_=selb[:, c * 128:(c + 1) * 128], identity=identf[:]),
                         reads=[bselb, bidf], writes=[bpsT])
                S.op("dve", lambda e: e.tensor_scalar(out=selT[:].rearrange("p a b -> p (a b)"), in0=psT, scalar1=-1.0, scalar2=30000.0,
                                                      op0=ALU.add, op1=ALU.mult), reads=[bpsT], writes=[bselT])

                def selmask(idx, kt, i=i):
                    mks = [(emat[:, kt % 64, :], bc4(selT, (kt // 64) * 128, 256), [bemat, bselT])]
                    if kt >= 4 * i:
                        mks.append((ident[:], bc4(dmask, (kt - 4 * i) * 128, 512), [bid, bdmask]))
                    return mks

                attend(Qg, bQ, ksT, bksT, lambda kt: vsS[:, kt, :], bvsS, list(range(4 * i + 4)), selmask, obr[1][0], obr[1][1])
                for br in range(3):
                    ob, bob = obr[br]
                    S.op("dve", lambda e, ob=ob, br=br: e.tensor_scalar(out=wgt[:, br, :], in0=AP(ob, 64, [[520, 128], [65, 8]]), scalar1=1e-30,
                                                                        scalar2=None, op0=ALU.max), reads=[bob], writes=[bwgt])
                S.op("dve", lambda e: e.reciprocal(out=wgt[:], in_=wgt[:]), reads=[bwgt], writes=[bwgt])
                S.op("dve", lambda e, g=g, i=i: e.tensor_tensor(out=wgt[:], in0=wgt[:],
                                                               in1=AP(gts, i * 48 + g * 24, [[NT * 48, 128], [1, 3], [3, 8]]), op=ALU.mult),
                     reads=[bwgt, bgts], writes=[bwgt])
                for br in range(3):
                    ob, bob = obr[br]
                    dstt, bdstt = (ot1, bot1) if br == 0 else (ot2, bot2)
                    S.op("dve", lambda e, ob=ob, br=br, dstt=dstt: e.tensor_tensor(
                        out=dstt[:], in0=ob[:, :, 0:64], in1=AP(wgt, br * 8, [[24, 128], [1, 8], [0, 64]]), op=ALU.mult),
                        reads=[bob, bwgt], writes=[bdstt])
                    if br > 0:
                        S.op("dve", lambda e: e.tensor_tensor(out=ot1[:], in0=ot1[:], in1=ot2[:], op=ALU.add), reads=[bot1, bot2], writes=[bot1])
                Ot, bOt = Otr.next()
                S.op("act", lambda e, Ot=Ot: e.copy(out=Ot[:], in_=ot1[:]), reads=[bot1], writes=[bOt])
                S.dma("pool", lambda e, Ot=Ot, g=g, i=i: e.dma_start(
                    out=O_ap[i * 128:(i + 1) * 128, g * 512:(g + 1) * 512].rearrange("p (h d) -> p h d", h=8), in_=Ot[:]),
                    reads=[bOt], writes=[O_b])
        S.flush()


def make_nsa_masks(j):
    kk = np.arange(128)[:, None]
    qq = np.arange(128)[None, :]
    dm = np.zeros((128, 4, 128), np.float32)
    for u in range(4):
        if u < j:
            dm[:, u, :] = 1.0
        elif u == j:
            dm[:, u, :] = (kk <= qq)
    wm = np.zeros((128, 8, 128), np.float32)
    for u in range(8):
        off = u - 4 - j
        if off == -4:
            wm[:, u, :] = (kk > qq)
        elif -4 < off < 0:
            wm[:, u, :] = 1.0
        elif off == 0:
            wm[:, u, :] = (kk <= qq)
    cm = np.zeros((128, 8, 128), np.float32)
    for m in range(4):
        for w in range(2):
            nloc = kk - 128 * w
            cm[:, m * 2 + w, :] = (16 * nloc + 31 <= 128 * (4 * m + j) + qq)
    cmT = np.where(cm.transpose(2, 1, 0) > 0.5, 0.0, NEG)
    cf = np.zeros((NT, 128, 512), np.float32)
    ss = np.arange(256)[None, :]
    for i in range(NT):
        gq = 4 * i + j
        qblk = 2 * gq + (np.arange(128)[:, None] >= 64)
        forced = (ss == 0) | (ss == qblk) | (ss == qblk - 1)
        causal = ss <= qblk
        cf[i, :, 0:256] = (causal & ~forced)
        cf[i, :, 256:512] = np.where(forced, 1e4, np.where(causal, 0.0, -1e4))
    em = np.zeros((128, 64, 128), np.float32)
    for m in range(64):
        em[2 * m, m, 0:64] = 1.0
        em[2 * m + 1, m, 64:128] = 1.0
    nb = lambda m01: np.where(m01 > 0.5, 0.0, NEG).astype(NPBF)
    return {"dmaskb": nb(dm), "wmaskb": nb(wm), "cmaskb": nb(cm),
            "cmaskTb": np.ascontiguousarray(cmT).astype(NPBF), "cforce": cf, "emat": em.astype(NPBF)}


def declare_A1_outputs(k, kind):
    f = k.dout if kind == "out" else k.dint
    f("QT1", [16, 65, TOK], BF16)
    f("kcT1", [128, TOK], BF16)
    f("vcT1", [128, TOK], BF16)
    f("ksT1", [2, 64, TOK], BF16)
    f("kwT1", [2, 64, TOK], BF16)
    f("vs1", [TOK, 128], BF16)
    f("vw1", [TOK, 128], BF16)
    f("knm1", [128, 2], F32)
    f("gates1", [TOK, 48], F32)


def build_L2():
    nc = bass.Bass("TRN2", target_bir_lowering=False)
    with ExitStack() as es:
        k = K(nc, es)
        declare_common(k, [0, 1])
        k.din("x", [TOK, D], F32)
        k.din("QT0", [16, 65, TOK], BF16)
        k.din("KT0g", [4, 16, 64, TOK], BF16)
        k.din("V0g", [4, 8, 128, NT, 129], BF16)
        k.din("knm0g", [128, 4], F32)
        k.din("lam", [1, 256], F32)
        k.din("subg", [128, 128], F32)
        k.din("dmask", [128, 4, 128], BF16)
        k.din("diff_w_out", [D, D], F32)
        k.din("nsa_w_in", [D, 1840], F32)
        declare_moe(k, 0)
        k.dint("O0", [TOK, D], BF16)
        k.dint("x1s0", [TOK, D], F32)
        k.dout("xm0", [TOK, D], F32)
        declare_A1_outputs(k, "out")
        cst = emit_consts(k, es)
        mod0 = emit_mod(k, es, cst, 0, "m0")
        phase_B0(k, cst)
        phase_C(k, cst, mod0, 0, "O0", "x", "xm0", "diff_w_out", False)
        mod1 = emit_mod(k, es, cst, 1, "m1")
        cs, bcs = emit_rope_tables(k, es, "r")
        phase_A1(k, cst, mod1, cs, bcs, "xm0")
    return nc


def build_L3():
    nc = bass.Bass("TRN2", target_bir_lowering=False)
    with ExitStack() as es:
        k = K(nc, es)
        declare_common(k, [1])
        k.din("xm0", [TOK, D], F32)
        k.din("QT1", [16, 65, TOK], BF16)
        k.din("kcT1g", [4, 128, TOK], BF16)
        k.din("vcT1g", [4, 128, TOK], BF16)
        k.din("ksT1g", [4, 2, 64, TOK], BF16)
        k.din("kwT1g", [4, 2, 64, TOK], BF16)
        k.din("vs1g", [4, TOK, 128], BF16)
        k.din("vw1g", [4, TOK, 128], BF16)
        k.din("knm1g", [128, 8], F32)
        k.din("gates1", [TOK, 48], F32)
        k.din("cmp_w1", [2, 2048, 256], F32)
        k.din("cmp_b1c", [128, 4], F32)
        k.din("cmp_w2", [2, 256, 64], F32)
        k.din("cmp_posT", [2, 64, 32], F32)
        k.din("emat", [128, 64, 128], BF16)
        k.din("dmaskb", [128, 4, 128], BF16)
        k.din("wmaskb", [128, 8, 128], BF16)
        k.din("cmaskb", [128, 8, 128], BF16)
        k.din("cmaskTb", [128, 8, 128], BF16)
        k.din("cforce", [NT, 128, 512], F32)
        k.din("nsa_w_out", [D, D], F32)
        k.din("final_g", [128, D], F32)
        declare_moe(k, 1)
        k.dint("O1", [TOK, D], BF16)
        k.dint("x1s1", [TOK, D], F32)
        k.dout("out", [TOK, D], F32)
        cst = emit_consts(k, es)
        mod1 = emit_mod(k, es, cst, 1, "m1")
        phase_B1(k, cst)
        phase_C(k, cst, mod1, 1, "O1", "xm0", "out", "nsa_w_out", True)
    return nc


_NC_CACHE = {}


def _get(name, fn):
    if name not in _NC_CACHE:
        _NC_CACHE[name] = fn()
    return _NC_CACHE[name]


def kernel(x, c, positions, ada_w, ada_b, norm_g, final_g, diff_w_in, diff_w_out, diff_lambda, diff_subln_g,
           nsa_w_in, nsa_w_out, nsa_cmp_pos, nsa_cmp_w1, nsa_cmp_b1, nsa_cmp_w2,
           moe_w_group, moe_b_group, moe_w_expert, moe_b_expert, moe_w_gate, moe_w_up, moe_w_down):
    A = lambda a: np.ascontiguousarray(np.asarray(a))
    x, c, positions, ada_w, ada_b, norm_g, final_g = map(A, (x, c, positions, ada_w, ada_b, norm_g, final_g))
    moe = tuple(map(A, (moe_w_group, moe_b_group, moe_w_expert, moe_b_expert, moe_w_gate, moe_w_up, moe_w_down)))
    cores = list(range(8))
    maps = []
    for core in cores:
        b, j = core // 4, core % 4
        m = common_inputs(core, x, c, positions, ada_w, ada_b, norm_g, [0])
        m["x"] = shard_rows(x[b], j)
        m["diff_w_in"] = A(diff_w_in)[0]
        maps.append(m)
    r1 = run_bass_kernel_spmd(_get("L1", build_L1), maps, core_ids=cores).results
    maps = []
    for core in cores:
        b, j = core // 4, core % 4
        m = common_inputs(core, x, c, positions, ada_w, ada_b, norm_g, [0, 1])
        m["x"] = shard_rows(x[b], j)
        m["QT0"] = r1[core]["QT0"]
        m["KT0g"] = np.stack([r1[4 * b + r]["KT0"] for r in range(4)])
        m["V0g"] = np.stack([r1[4 * b + r]["V0"] for r in range(4)])
        m["knm0g"] = np.concatenate([r1[4 * b + r]["knm0"] for r in range(4)], axis=1)
        m["lam"] = A(diff_lambda)[0].reshape(1, 256)
        m["subg"] = A(np.broadcast_to(A(diff_subln_g)[0][None, :], (128, 128)))
        m["dmask"] = make_dmask(j)
        m["diff_w_out"] = A(diff_w_out)[0]
        m["nsa_w_in"] = A(nsa_w_in)[0]
        moe_inputs(m, 0, *moe)
        maps.append(m)
    r2 = run_bass_kernel_spmd(_get("L2", build_L2), maps, core_ids=cores).results
    maps = []
    w1 = A(nsa_cmp_w1)[0]
    b1 = A(nsa_cmp_b1)[0]
    for core in cores:
        b, j = core // 4, core % 4
        m = common_inputs(core, x, c, positions, ada_w, ada_b, norm_g, [1])
        m["xm0"] = r2[core]["xm0"]
        m["QT1"] = r2[core]["QT1"]
        for nm in ("kcT1", "vcT1", "ksT1", "kwT1", "vs1", "vw1"):
            m[nm + "g"] = np.stack([r2[4 * b + r][nm] for r in range(4)])
        kn = np.stack([r2[4 * b + r]["knm1"] for r in range(4)], axis=2)
        m["knm1g"] = A(kn.reshape(128, 8))
        m["gates1"] = r2[core]["gates1"]
        m["cmp_w1"] = w1
        m["cmp_b1c"] = A(np.concatenate([col_layout(b1[0]), col_layout(b1[1])], axis=1))
        m["cmp_w2"] = A(nsa_cmp_w2)[0]
        m["cmp_posT"] = A(A(nsa_cmp_pos)[0].transpose(0, 2, 1))
        m.update(make_nsa_masks(j))
        m["nsa_w_out"] = A(nsa_w_out)[0]
        m["final_g"] = A(np.broadcast_to(final_g[None, :], (128, D)))
        moe_inputs(m, 1, *moe)
        maps.append(m)
    r3 = run_bass_kernel_spmd(_get("L3", build_L3), maps, core_ids=cores).results
    out = np.empty((2, SEQ // 128, 128, D), np.float32)
    for core in cores:
        b, j = core // 4, core % 4
        out[b, j::4] = np.asarray(r3[core]["out"]).reshape(NT, 128, D)
    return out.reshape(2, SEQ, D)
```

```python
import math
import numpy as np
import ml_dtypes
from contextlib import ExitStack
import concourse.bass as bass
import concourse.mybir as mybir
from concourse.bass_utils import run_bass_kernel_spmd

F32 = mybir.dt.float32
BF16 = mybir.dt.bfloat16
I32 = mybir.dt.int32
ALU = mybir.AluOpType
AF = mybir.ActivationFunctionType
AX = mybir.AxisListType
NPBF = ml_dtypes.bfloat16

D = 1024
SEQ = 16384
NT = 32
TOK = NT * 128
NEG = -30000.0


class Buf:
    __slots__ = ("w", "r")

    def __init__(self):
        self.w = None
        self.r = {}


class Sched:
    ENG = ("pe", "act", "dve", "pool", "sp")

    def __init__(self, nc, es, n_dma_sems=32):
        self.nc = nc
        self.sems = {}
        self.count = {}
        for e in self.ENG:
            self.sems[e] = es.enter_context(nc.semaphore("s_" + e))
            self.count[e] = 0
        self.dma_sems = []
        for i in range(n_dma_sems):
            k = "d%d" % i
            self.sems[k] = es.enter_context(nc.semaphore("s_" + k))
            self.count[k] = 0
            self.dma_sems.append(k)
        self.dma_rr = 0
        self.dma_rr_sw = 0
        self.n_hw = n_dma_sems - 8
        self.seen = {e: {} for e in self.ENG}
        self.prog = {e: [] for e in self.ENG}

    def _need(self, eng, tok):
        k, v = tok
        if self.seen[eng].get(k, 0) >= v:
            return
        self.seen[eng][k] = v
        self.prog[eng].append(("w", k, v))

    def _deps(self, eng, reads, writes):
        for b in reads:
            if b.w is not None and not (eng == "pe" and b.w[0] == "pe"):
                self._need(eng, b.w)
        for b in writes:
            if b.w is not None and b.w[0] != eng:
                self._need(eng, b.w)
            for k, v in b.r.items():
                if k != eng:
                    self._need(eng, (k, v))

    def _mark(self, tok, reads, writes):
        k, v = tok
        for b in reads:
            if b.r.get(k, 0) < v:
                b.r[k] = v
        for b in writes:
            b.w = tok
            b.r = {}

    def op(self, eng, fn, reads=(), writes=()):
        self._deps(eng, reads, writes)
        self.count[eng] += 1
        tok = (eng, self.count[eng])
        self.prog[eng].append(("i", fn, eng, 1))
        self._mark(tok, reads, writes)
        return tok

    def dma(self, eng, fn, reads=(), writes=()):
        self._deps(eng, reads, writes)
        if eng == "pool":
            k = self.dma_sems[self.n_hw + self.dma_rr_sw]
            self.dma_rr_sw = (self.dma_rr_sw + 1) % (len(self.dma_sems) - self.n_hw)
        else:
            k = self.dma_sems[self.dma_rr]
            self.dma_rr = (self.dma_rr + 1) % self.n_hw
        if self.count[k] > 0:
            self._need(eng, (k, self.count[k]))
        self.count[k] += 16
        tok = (k, self.count[k])
        self.prog[eng].append(("i", fn, k, 16))
        self._mark(tok, reads, writes)
        return tok

    def barrier(self):
        for e in self.ENG:
            for k, c in self.count.items():
                if c > 0 and k != e:
                    self._need(e, (k, c))

    def flush(self):
        self.barrier()
        nc = self.nc
        prog = self.prog
        sems = self.sems

        def run(e, items):
            for it in items:
                if it[0] == "w":
                    e.wait_ge(sems[it[1]], it[2])
                else:
                    it[1](e).then_inc(sems[it[2]], it[3])

        with nc.Block() as block:
            @block.tensor
            def _(e):
                run(e, prog["pe"])

            @block.scalar
            def _(e):
                run(e, prog["act"])

            @block.vector
            def _(e):
                run(e, prog["dve"])

            @block.gpsimd
            def _(e):
                run(e, prog["pool"])

            @block.sync
            def _(e):
                run(e, prog["sp"])
        self.prog = {e: [] for e in self.ENG}


class Ring:
    def __init__(self, items):
        self.items = items
        self.i = 0

    def next(self):
        it = self.items[self.i]
        self.i = (self.i + 1) % len(self.items)
        return it


class K:
    def __init__(self, nc, es):
        self.nc = nc
        self.es = es
        self.S = Sched(nc, es)
        self.dram = {}

    def din(self, name, shape, dt):
        t = self.nc.dram_tensor(name, list(shape), dt, kind="ExternalInput")
        self.dram[name] = (t.ap(), Buf())
        return self.dram[name]

    def dout(self, name, shape, dt):
        t = self.nc.dram_tensor(name, list(shape), dt, kind="ExternalOutput")
        self.dram[name] = (t.ap(), Buf())
        return self.dram[name]

    def dint(self, name, shape, dt):
        t = self.nc.dram_tensor(name, list(shape), dt, kind="Internal")
        self.dram[name] = (t.ap(), Buf())
        return self.dram[name]


_UID = [0]


def _uname(name):
    _UID[0] += 1
    return "%s_%d" % (name, _UID[0])


def sbt(nc, es, name, shape, dt):
    return es.enter_context(nc.sbuf_tensor(_uname(name), list(shape), dt)), Buf()


def pst(nc, es, name, shape, dt):
    return es.enter_context(nc.psum_tensor(_uname(name), list(shape), dt)), Buf()


def sring(nc, es, name, shape, dt, n):
    return Ring([sbt(nc, es, "%s%d" % (name, i), shape, dt) for i in range(n)])


def pring(nc, es, name, shape, dt, n):
    return Ring([pst(nc, es, "%s%d" % (name, i), shape, dt) for i in range(n)])


def AP(t, off, dims):
    return bass.AP(t, off, [list(d) for d in dims])


def emit_consts(k, es):
    nc, S = k.nc, k.S
    c = {}
    identf, bidf = sbt(nc, es, "identf", [128, 128], F32)
    ident, bid = sbt(nc, es, "ident", [128, 128], BF16)
    ones, bones = sbt(nc, es, "ones", [128, 128], F32)
    S.op("pool", lambda e: e.memset(identf[:], 1.0), writes=[bidf])
    S.op("pool", lambda e: e.affine_select(out=identf[:], in_=identf[:], pattern=[[-1, 128]],
                                           compare_op=ALU.is_equal, fill=0.0, base=0, channel_multiplier=1),
         reads=[bidf], writes=[bidf])
    S.op("pool", lambda e: e.tensor_copy(out=ident[:], in_=identf[:]), reads=[bidf], writes=[bid])
    S.op("pool", lambda e: e.memset(ones[:], 1.0), writes=[bones])
    c["identf"] = (identf, bidf)
    c["ident"] = (ident, bid)
    c["ones"] = (ones, bones)
    return c


def emit_mod(k, es, cst, layer, tag):
    nc, S = k.nc, k.S
    c_ap, c_b = k.dram["c"]
    w_ap, w_b = k.dram["ada_w%d" % layer]
    b_ap, b_b = k.dram["ada_b%d" % layer]
    g_ap, g_b = k.dram["ng%d" % layer]
    ones, bones = cst["ones"]
    out = {}
    modcol, bmc = sbt(nc, es, "modcol" + tag, [128, 48], F32)
    gcol, bgc = sbt(nc, es, "gcol" + tag, [128, 16], F32)
    AB, bAB = sbt(nc, es, "AB" + tag, [128, 32], F32)
    gbs = {nm: sbt(nc, es, nm + tag, [128, 1024], F32) for nm in ("g1b", "g2b")}
    with ExitStack() as ps:
        cact, bcact = sbt(nc, ps, "cact" + tag, [128, 8], F32)
        csig, bcsig = sbt(nc, ps, "csig" + tag, [128, 8], F32)
        row, brow = sbt(nc, ps, "modrow" + tag, [1, 6144], F32)
        brow_t, bbrow = sbt(nc, ps, "modb" + tag, [1, 6144], F32)
        wr = sring(nc, ps, "modw" + tag, [128, 8, 512], F32, 2)
        pr = pring(nc, ps, "modp" + tag, [128, 512], F32, 2)
        pcol, bpcol = pst(nc, ps, "modpc" + tag, [128, 512], F32)
        S.dma("sp", lambda e: e.dma_start(out=cact[:], in_=c_ap), reads=[c_b], writes=[bcact])
        S.dma("sp", lambda e: e.dma_start(out=brow_t[:], in_=b_ap), reads=[b_b], writes=[bbrow])
        S.op("act", lambda e: e.activation(out=csig[:], in_=cact[:], func=AF.Sigmoid), reads=[bcact], writes=[bcsig])
        S.op("dve", lambda e: e.tensor_tensor(out=cact[:], in0=cact[:], in1=csig[:], op=ALU.mult),
             reads=[bcact, bcsig], writes=[bcact])
        for nb in range(12):
            wt, bw = wr.next()
            pt, bp = pr.next()
            S.dma("sp", lambda e, wt=wt, nb=nb: e.dma_start(
                out=wt[:], in_=w_ap[:, nb * 512:(nb + 1) * 512].rearrange("(c p) n -> p c n", p=128)),
                reads=[w_b], writes=[bw])
            for kc in range(8):
                S.op("pe", lambda e, pt=pt, wt=wt, kc=kc: e.matmul(
                    pt[0:1, :], lhsT=cact[:, kc:kc + 1], rhs=wt[:, kc, :], start=(kc == 0), stop=(kc == 7)),
                    reads=[bcact, bw], writes=[bp])
            S.op("dve", lambda e, pt=pt, nb=nb: e.tensor_tensor(
                out=row[0:1, nb * 512:(nb + 1) * 512], in0=pt[0:1, :], in1=brow_t[0:1, nb * 512:(nb + 1) * 512],
                op=ALU.add), reads=[bp, bbrow], writes=[brow])
        for kk in range(48):
            S.op("pe", lambda e, kk=kk: e.matmul(pcol[:, kk:kk + 1], lhsT=row[0:1, kk * 128:(kk + 1) * 128],
                                                 rhs=ones[0:1, 0:1], start=True, stop=True),
                 reads=[brow, bones], writes=[bpcol])
        S.op("dve", lambda e: e.tensor_copy(out=modcol[:], in_=pcol[:, 0:48]), reads=[bpcol], writes=[bmc])
        S.dma("sp", lambda e: e.dma_start(out=gcol[:], in_=g_ap), reads=[g_b], writes=[bgc])
        S.op("dve", lambda e: e.scalar_tensor_tensor(out=AB[:, 0:8], in0=modcol[:, 8:16], scalar=1.0, in1=gcol[:, 0:8],
                                                     op0=ALU.add, op1=ALU.mult), reads=[bmc, bgc], writes=[bAB])
        S.op("dve", lambda e: e.tensor_copy(out=AB[:, 8:16], in_=modcol[:, 0:8]), reads=[bmc], writes=[bAB])
        S.op("dve", lambda e: e.scalar_tensor_tensor(out=AB[:, 16:24], in0=modcol[:, 32:40], scalar=1.0, in1=gcol[:, 8:16],
                                                     op0=ALU.add, op1=ALU.mult), reads=[bmc, bgc], writes=[bAB])
        S.op("dve", lambda e: e.tensor_copy(out=AB[:, 24:32], in_=modcol[:, 24:32]), reads=[bmc], writes=[bAB])
        out["AB"] = (AB, bAB)
        for nm, ch in (("g1b", 2), ("g2b", 5)):
            gb, bgb = gbs[nm]
            for hf in range(2):
                pt, bp = pr.next()
                S.op("pe", lambda e, pt=pt, ch=ch, hf=hf: e.matmul(
                    pt[:, :], lhsT=ones[0:1, :], rhs=row[0:1, ch * 1024 + hf * 512: ch * 1024 + (hf + 1) * 512],
                    start=True, stop=True), reads=[bones, brow], writes=[bp])
                S.op("act", lambda e, pt=pt, gb=gb, hf=hf: e.copy(out=gb[:, hf * 512:(hf + 1) * 512], in_=pt[:, :]),
                     reads=[bp], writes=[bgb])
            out[nm] = (gb, bgb)
        S.flush()
    return out


def emit_rope_tables(k, es, tag):
    nc, S = k.nc, k.S
    pos_ap, pos_b = k.dram["pos"]
    inv_ap, inv_b = k.dram["invf"]
    cs, bcs = sbt(nc, es, "ropecs" + tag, [128, NT, 32], F32)
    with ExitStack() as ps:
        posi, bpi = sbt(nc, ps, "posi" + tag, [128, NT], I32)
        posf, bpf = sbt(nc, ps, "posf" + tag, [128, NT], F32)
        invf, binv = sbt(nc, ps, "invf" + tag, [128, 8], F32)
        ang, bang = sbt(nc, ps, "ang" + tag, [128, NT, 8], F32)
        red, bred = sbt(nc, ps, "red" + tag, [128, NT, 16], F32)
        S.dma("sp", lambda e: e.dma_start(out=posi[:], in_=pos_ap), reads=[pos_b], writes=[bpi])
        S.dma("sp", lambda e: e.dma_start(out=invf[:], in_=inv_ap), reads=[inv_b], writes=[binv])
        S.op("dve", lambda e: e.tensor_copy(out=posf[:], in_=posi[:]), reads=[bpi], writes=[bpf])
        S.op("dve", lambda e: e.tensor_tensor(out=ang[:], in0=AP(posf, 0, [[NT, 128], [1, NT], [0, 8]]),
                                              in1=AP(invf, 0, [[8, 128], [0, NT], [1, 8]]), op=ALU.mult),
             reads=[bpf, binv], writes=[bang])
        twopi = 2.0 * math.pi
        ki, bki = sbt(nc, ps, "ropeki" + tag, [128, NT, 16], I32)
        kf, bkf = sbt(nc, ps, "ropekf" + tag, [128, NT, 16], F32)
        S.op("dve", lambda e: e.tensor_scalar(out=red[:, :, 0:8], in0=ang[:], scalar1=0.5 * math.pi, scalar2=None,
                                              op0=ALU.add), reads=[bang], writes=[bred])
        S.op("dve", lambda e: e.tensor_copy(out=red[:, :, 8:16], in_=ang[:]), reads=[bang], writes=[bred])
        S.op("dve", lambda e: e.tensor_scalar(out=kf[:], in0=red[:], scalar1=1.0 / twopi, scalar2=None, op0=ALU.mult),
             reads=[bred], writes=[bkf])
        S.op("dve", lambda e: e.tensor_copy(out=ki[:], in_=kf[:]), reads=[bkf], writes=[bki])
        S.op("dve", lambda e: e.tensor_copy(out=kf[:], in_=ki[:]), reads=[bki], writes=[bkf])
        S.op("dve", lambda e: e.scalar_tensor_tensor(out=red[:], in0=kf[:], scalar=-twopi, in1=red[:],
                                                     op0=ALU.mult, op1=ALU.add), reads=[bkf, bred], writes=[bred])
        S.op("dve", lambda e: e.tensor_scalar(out=kf[:], in0=red[:], scalar1=math.pi, scalar2=-twopi,
                                              op0=ALU.is_gt, op1=ALU.mult), reads=[bred], writes=[bkf])
        S.op("dve", lambda e: e.tensor_tensor(out=red[:], in0=red[:], in1=kf[:], op=ALU.add),
             reads=[bred, bkf], writes=[bred])
        S.op("dve", lambda e: e.tensor_scalar(out=kf[:], in0=red[:], scalar1=-math.pi, scalar2=twopi,
                                              op0=ALU.is_lt, op1=ALU.mult), reads=[bred], writes=[bkf])
        S.op("dve", lambda e: e.tensor_tensor(out=red[:], in0=red[:], in1=kf[:], op=ALU.add),
             reads=[bred, bkf], writes=[bred])
        S.op("dve", lambda e: e.tensor_scalar(out=red[:], in0=red[:], scalar1=-3.1415925, scalar2=3.1415925,
                                              op0=ALU.max, op1=ALU.min), reads=[bred], writes=[bred])
        S.op("act", lambda e: e.activation(out=cs[:, :, 0:16], in_=red[:], func=AF.Sin), reads=[bred], writes=[bcs])
        S.op("dve", lambda e: e.tensor_scalar(out=cs[:, :, 16:32], in0=cs[:, :, 0:16], scalar1=0.125, scalar2=None,
                                              op0=ALU.mult), reads=[bcs], writes=[bcs])
        S.flush()
    return cs, bcs


def emit_norm_T_a(k, cst, xt, bx, tmp):
    S = k.S
    sq, bsq = tmp["sq"].next()
    st, bst = tmp["st"].next()
    xs, bxs = tmp["xs"].next()
    S.op("act", lambda e: e.activation(out=sq[:], in_=xt, func=AF.Square, accum_out=st[:, 0:1]),
         reads=[bx], writes=[bsq, bst])
    S.op("act", lambda e: e.activation(out=st[:, 1:2], in_=st[:, 0:1], func=AF.Sqrt, scale=1.0 / D, bias=1e-6),
         reads=[bst], writes=[bst])
    S.op("dve", lambda e: e.reciprocal(out=st[:, 1:2], in_=st[:, 1:2]), reads=[bst], writes=[bst])
    S.op("act", lambda e: e.activation(out=xs[:], in_=xt, func=AF.Copy, scale=st[:, 1:2]),
         reads=[bx, bst], writes=[bxs])
    return xs, bxs


def emit_norm_T_b(k, cst, xs, bxs, AB, bAB, abcol, hT, bhT, tmp):
    S = k.S
    ident, bid = cst["ident"]
    pT, bpT = tmp["pT"].next()
    for c in range(8):
        S.op("pe", lambda e, c=c: e.transpose(out=pT[:, c, :], in_=xs[:, c * 128:(c + 1) * 128], identity=ident[:]),
             reads=[bxs, bid], writes=[bpT])
    for c in range(8):
        S.op("dve", lambda e, c=c: e.tensor_scalar(out=hT[:, c, :], in0=pT[:, c, :], scalar1=AB[:, abcol + c:abcol + c + 1],
                                                   scalar2=AB[:, abcol + 8 + c:abcol + 9 + c], op0=ALU.mult, op1=ALU.add),
             reads=[bpT, bAB], writes=[bhT])


def emit_norm_T(k, cst, xt, bx, AB, bAB, abcol, hT, bhT, tmp):
    xs, bxs = emit_norm_T_a(k, cst, xt, bx, tmp)
    emit_norm_T_b(k, cst, xs, bxs, AB, bAB, abcol, hT, bhT, tmp)


def norm_tmp(nc, es, tag):
    return {
        "sq": sring(nc, es, "nsq" + tag, [128, 1024], F32, 1),
        "st": sring(nc, es, "nst" + tag, [128, 2], F32, 2),
        "xs": sring(nc, es, "nxs" + tag, [128, 1024], BF16, 2),
        "pT": pring(nc, es, "npT" + tag, [128, 8, 128], BF16, 1),
    }


def emit_rope(k, src, bsrc, dst, bdst, nh, dstw, cs, bcs, t, coff, tmp, btmp):
    S = k.S
    cosb = AP(cs, t * 32 + coff, [[NT * 32, 128], [0, nh], [1, 8]])
    sinb = AP(cs, t * 32 + coff + 8, [[NT * 32, 128], [0, nh], [1, 8]])
    x1 = src(0, 8)
    x2 = src(8, 16)
    S.op("dve", lambda e: e.tensor_tensor(out=tmp[:, 0, 0:nh, :], in0=x1, in1=cosb, op=ALU.mult),
         reads=[bsrc, bcs], writes=[btmp])
    S.op("dve", lambda e: e.tensor_tensor(out=tmp[:, 1, 0:nh, :], in0=x2, in1=sinb, op=ALU.mult),
         reads=[bsrc, bcs], writes=[btmp])
    S.op("dve", lambda e: e.tensor_tensor(out=tmp[:, 2, 0:nh, :], in0=x2, in1=cosb, op=ALU.mult),
         reads=[bsrc, bcs], writes=[btmp])
    S.op("dve", lambda e: e.tensor_tensor(out=tmp[:, 3, 0:nh, :], in0=x1, in1=sinb, op=ALU.mult),
         reads=[bsrc, bcs], writes=[btmp])
    S.op("dve", lambda e: e.tensor_tensor(out=dst(0, 8), in0=tmp[:, 0, 0:nh, :], in1=tmp[:, 1, 0:nh, :], op=ALU.subtract),
         reads=[btmp], writes=[bdst])
    S.op("dve", lambda e: e.tensor_tensor(out=dst(8, 16), in0=tmp[:, 2, 0:nh, :], in1=tmp[:, 3, 0:nh, :], op=ALU.add),
         reads=[btmp], writes=[bdst])


def load_cast_weight(k, w_ap, w_b, wt, bw, ncols, piece=512):
    S = k.S
    for c0 in range(0, ncols, piece):
        c1 = min(ncols, c0 + piece)
        S.dma("pool", lambda e, c0=c0, c1=c1: e.dma_start(
            out=wt[:, :, c0:c1], in_=w_ap[:, c0:c1].rearrange("(c p) n -> p c n", p=128)),
            reads=[w_b], writes=[bw])


def phase_A0(k, cst, mod, cs, bcs):
    nc, S = k.nc, k.S
    ident, bid = cst["ident"]
    AB, bAB = mod["AB"]
    x_ap, x_b = k.dram["x"]
    w_ap, w_b = k.dram["diff_w_in"]
    QT_ap, QT_b = k.dram["QT0"]
    KT_ap, KT_b = k.dram["KT0"]
    V_ap, V_b = k.dram["V0"]
    kn_ap, kn_b = k.dram["knm0"]
    with ExitStack() as es:
        Win, bWin = sbt(nc, es, "a0win", [128, 8, 3072], BF16)
        load_cast_weight(k, w_ap, w_b, Win, bWin, 3072)
        xr = sring(nc, es, "a0x", [128, 1024], F32, 2)
        hr = sring(nc, es, "a0hT", [128, 8, 128], BF16, 2)
        ntmp = norm_tmp(nc, es, "a0")
        ppr = pring(nc, es, "a0pp", [128, 1024], F32, 2)
        pTr = pring(nc, es, "a0pqt", [128, 16, 128], BF16, 1)
        Qsr = sring(nc, es, "a0qs", [128, 16, 65], BF16, 2)
        Ksr = sring(nc, es, "a0ks", [128, 16, 64], BF16, 2)
        Vsr = sring(nc, es, "a0vs", [128, 8, 129], BF16, 2)
        for (Vs_, bVs_) in Vsr.items:
            S.op("pool", lambda e, Vs_=Vs_: e.memset(Vs_[:, :, 128:129], 1.0), writes=[bVs_])
        QTr = sring(nc, es, "a0qts", [128, 16, 128], BF16, 2)
        KTr = sring(nc, es, "a0kts", [128, 16, 128], BF16, 2)
        rtmp, brtmp = sbt(nc, es, "a0rt", [128, 4, 16, 8], F32)
        sqv, bsqv = sbt(nc, es, "a0sqv", [128, 16, 64], F32)
        nrm, bnrm = sbt(nc, es, "a0nrm", [128, 16], F32)
        knm, bknm = sbt(nc, es, "a0knm", [128, 2], F32)
        S.op("dve", lambda e: e.memset(knm[:], 0.0), writes=[bknm])
        hts = {}

        def normT(t):
            xt, bx = xr.next()
            S.dma("sp", lambda e: e.dma_start(out=xt[:], in_=x_ap[t * 128:(t + 1) * 128, :]), reads=[x_b], writes=[bx])
            hT, bhT = hr.next()
            emit_norm_T(k, cst, xt[:], bx, AB, bAB, 0, hT, bhT, ntmp)
            hts[t] = (hT, bhT)

        def mm(t, sec):
            hT, bhT = hts[t]
            pp, bpp = ppr.next()
            for nb in range(2):
                for c in range(8):
                    S.op("pe", lambda e, c=c, nb=nb: e.matmul(
                        pp[:, nb * 512:(nb + 1) * 512], lhsT=hT[:, c, :],
                        rhs=Win[:, c, sec * 1024 + nb * 512: sec * 1024 + (nb + 1) * 512],
                        start=(c == 0), stop=(c == 7)), reads=[bhT, bWin], writes=[bpp])
            return pp, bpp

        def qpost(t, pp, bpp):
            src = lambda d0, d1: AP(pp, d0, [[1024, 128], [64, 16], [1, d1 - d0]])
            Qs, bQs = Qsr.next()
            dst = lambda d0, d1: AP(Qs, d0, [[1040, 128], [65, 16], [1, d1 - d0]])
            emit_rope(k, src, bpp, dst, bQs, 16, 65, cs, bcs, t, 16, rtmp, brtmp)
            S.op("act", lambda e: e.activation(out=dst(16, 64), in_=src(16, 64), func=AF.Copy, scale=0.125), reads=[bpp], writes=[bQs])
            S.op("dve", lambda e: e.tensor_tensor(out=sqv[:], in0=dst(0, 64), in1=dst(0, 64), op=ALU.mult), reads=[bQs], writes=[bsqv])
            S.op("dve", lambda e: e.tensor_reduce(out=nrm[:], in_=sqv[:], axis=AX.X, op=ALU.add), reads=[bsqv], writes=[bnrm])
            S.op("act", lambda e: e.activation(out=nrm[:], in_=nrm[:], func=AF.Sqrt), reads=[bnrm], writes=[bnrm])
            S.op("dve", lambda e: e.tensor_scalar(out=dst(64, 65), in0=AP(nrm, 0, [[16, 128], [1, 16], [1, 1]]),
                                                  scalar1=-1.0, scalar2=None, op0=ALU.mult), reads=[bnrm], writes=[bQs])
            return Qs, bQs

        def qtr(t, Qs, bQs):
            pT, bpT = pTr.next()
            for hc in range(16):
                S.op("pe", lambda e, hc=hc: e.transpose(out=pT[0:65, hc, :], in_=Qs[:, hc, :], identity=ident[:]),
                     reads=[bQs, bid], writes=[bpT])
            QTs, bQTs = QTr.next()
            S.op("act", lambda e: e.copy(out=QTs[0:65, :, :], in_=pT[0:65, :, :]), reads=[bpT], writes=[bQTs])
            S.dma("sp", lambda e: e.dma_start(out=QT_ap[:, :, t * 128:(t + 1) * 128].rearrange("h r q -> r h q"), in_=QTs[0:65, :, :]),
                  reads=[bQTs], writes=[QT_b])

        def kpost(t, pp, bpp):
            src = lambda d0, d1: AP(pp, d0, [[1024, 128], [64, 16], [1, d1 - d0]])
            Ks, bKs = Ksr.next()
            dst = lambda d0, d1: AP(Ks, d0, [[1024, 128], [64, 16], [1, d1 - d0]])
            emit_rope(k, src, bpp, dst, bKs, 16, 64, cs, bcs, t, 0, rtmp, brtmp)
            S.op("act", lambda e: e.copy(out=dst(16, 64), in_=src(16, 64)), reads=[bpp], writes=[bKs])
            S.op("dve", lambda e: e.tensor_tensor(out=sqv[:], in0=dst(0, 64), in1=dst(0, 64), op=ALU.mult), reads=[bKs], writes=[bsqv])
            S.op("dve", lambda e: e.tensor_reduce(out=nrm[:], in_=sqv[:], axis=AX.X, op=ALU.add), reads=[bsqv], writes=[bnrm])
            S.op("dve", lambda e: e.tensor_reduce(out=knm[:, 1:2], in_=nrm[:], axis=AX.X, op=ALU.max), reads=[bnrm], writes=[bknm])
            S.op("dve", lambda e: e.tensor_tensor(out=knm[:, 0:1], in0=knm[:, 0:1], in1=knm[:, 1:2], op=ALU.max), reads=[bknm], writes=[bknm])
            return Ks, bKs

        def ktr(t, Ks, bKs):
            pT, bpT = pTr.next()
            for hc in range(16):
                S.op("pe", lambda e, hc=hc: e.transpose(out=pT[0:64, hc, :], in_=Ks[:, hc, :], identity=ident[:]),
                     reads=[bKs, bid], writes=[bpT])
            KTs, bKTs = KTr.next()
            S.op("act", lambda e: e.copy(out=KTs[0:64, :, :], in_=pT[0:64, :, :]), reads=[bpT], writes=[bKTs])
            S.dma("sp", lambda e: e.dma_start(out=KT_ap[:, :, t * 128:(t + 1) * 128].rearrange("h r q -> r h q"), in_=KTs[0:64, :, :]),
                  reads=[bKTs], writes=[KT_b])

        def vpost(t, pp, bpp):
            Vs, bVs = Vsr.next()
            S.op("act", lambda e: e.copy(out=Vs[:, :, 0:128], in_=AP(pp, 0, [[1024, 128], [128, 8], [1, 128]])), reads=[bpp], writes=[bVs])
            S.dma("sp", lambda e: e.dma_start(out=V_ap[:, :, t, :].rearrange("h p v -> p h v"), in_=Vs[:]), reads=[bVs], writes=[V_b])

        normT(0)
        for t in range(NT):
            ppq = mm(t, 0)
            Qs = qpost(t, *ppq)
            ppk = mm(t, 1)
            Ks = kpost(t, *ppk)
            qtr(t, *Qs)
            if t + 1 < NT:
                normT(t + 1)
            ppv = mm(t, 2)
            vpost(t, *ppv)
            ktr(t, *Ks)
        S.dma("sp", lambda e: e.dma_start(out=kn_ap, in_=knm[:, 0:1]), reads=[bknm], writes=[kn_b])
        S.flush()


def core_tiles(j):
    return [4 * i + j for i in range(NT)]


def shard_rows(a_b, j):
    a = a_b.reshape(SEQ // 128, 128, *a_b.shape[1:])
    return np.ascontiguousarray(a[j::4].reshape(TOK, *a_b.shape[1:]))


def col_layout(v):
    return np.ascontiguousarray(v.reshape(-1, 128).T)


INVF = (500000.0 ** (-np.arange(0, 16, 2, dtype=np.float32) / 16)).astype(np.float32)


def common_inputs(core, x, c, positions, ada_w, ada_b, norm_g, layers):
    b, j = core // 4, core % 4
    m = {
        "c": col_layout(c[b]),
        "pos": np.ascontiguousarray(shard_rows(positions[b], j).reshape(NT, 128).T),
        "invf": np.ascontiguousarray(np.broadcast_to(INVF[None, :], (128, 8))),
    }
    for l in layers:
        m["ada_w%d" % l] = ada_w[l]
        m["ada_b%d" % l] = ada_b[l][None, :]
        m["ng%d" % l] = np.concatenate([col_layout(norm_g[l, 0]), col_layout(norm_g[l, 1])], axis=1)
    return m


def declare_common(k, layers):
    k.din("c", [128, 8], F32)
    k.din("pos", [128, NT], I32)
    k.din("invf", [128, 8], F32)
    for l in layers:
        k.din("ada_w%d" % l, [1024, 6144], F32)
        k.din("ada_b%d" % l, [1, 6144], F32)
        k.din("ng%d" % l, [128, 16], F32)


def build_L1():
    nc = bass.Bass("TRN2", target_bir_lowering=False)
    with ExitStack() as es:
        k = K(nc, es)
        declare_common(k, [0])
        k.din("x", [TOK, D], F32)
        k.din("diff_w_in", [D, 3072], F32)
        k.dout("QT0", [16, 65, TOK], BF16)
        k.dout("KT0", [16, 64, TOK], BF16)
        k.dout("V0", [8, 128, NT, 129], BF16)
        k.dout("knm0", [128, 1], F32)
        cst = emit_consts(k, es)
        mod = emit_mod(k, es, cst, 0, "m0")
        cs, bcs = emit_rope_tables(k, es, "r")
        phase_A0(k, cst, mod, cs, bcs)
    return nc


def emit_kmax(k, es, cst, knm_name, tag):
    nc, S = k.nc, k.S
    kn_ap, kn_b = k.dram[knm_name]
    identf, bidf = cst["identf"]
    ones, bones = cst["ones"]
    kmx, bkmx = sbt(nc, es, "kmx" + tag, [128, 1], F32)
    with ExitStack() as ps:
        a, ba = sbt(nc, ps, "kma" + tag, [128, 4], F32)
        m, bm = sbt(nc, ps, "kmm" + tag, [128, 1], F32)
        r, br = sbt(nc, ps, "kmr" + tag, [1, 128], F32)
        s, bs = sbt(nc, ps, "kms" + tag, [1, 1], F32)
        p1, bp1 = pst(nc, ps, "kmp" + tag, [128, 512], F32)
        S.dma("sp", lambda e: e.dma_start(out=a[:], in_=kn_ap), reads=[kn_b], writes=[ba])
        S.op("dve", lambda e: e.tensor_reduce(out=m[:], in_=a[:], axis=AX.X, op=ALU.max), reads=[ba], writes=[bm])
        S.op("pe", lambda e: e.transpose(out=p1[0:1, 0:128], in_=m[:, 0:1], identity=identf[:]),
             reads=[bm, bidf], writes=[bp1])
        S.op("dve", lambda e: e.tensor_copy(out=r[:], in_=p1[0:1, 0:128]), reads=[bp1], writes=[br])
        S.op("dve", lambda e: e.tensor_reduce(out=s[:], in_=r[:], axis=AX.X, op=ALU.max), reads=[br], writes=[bs])
        S.op("act", lambda e: e.activation(out=s[:], in_=s[:], func=AF.Sqrt, scale=1.02), reads=[bs], writes=[bs])
        S.op("pe", lambda e: e.matmul(p1[:, 256:257], lhsT=ones[0:1, :], rhs=s[0:1, 0:1], start=True, stop=True),
             reads=[bs, bones, bp1], writes=[bp1])
        S.op("dve", lambda e: e.tensor_copy(out=kmx[:], in_=p1[:, 256:257]), reads=[bp1], writes=[bkmx])
        S.flush()
    return kmx, bkmx


def phase_B0(k, cst):
    nc, S = k.nc, k.S
    ident, bid = cst["ident"]
    ones, bones = cst["ones"]
    QT_ap, QT_b = k.dram["QT0"]
    KT_ap, KT_b = k.dram["KT0g"]
    V_ap, V_b = k.dram["V0g"]
    O_ap, O_b = k.dram["O0"]
    lam_ap, lam_b = k.dram["lam"]
    sg_ap, sg_b = k.dram["subg"]
    dm_ap, dm_b = k.dram["dmask"]
    lam_init = 0.8 - 0.6 * math.exp(-0.3 * 0)
    with ExitStack() as es:
        kmx, bkmx = emit_kmax(k, es, cst, "knm0g", "b0")
        nlam, bnlam = sbt(nc, es, "b0nlam", [128, 1], F32)
        gsub, bgsub = sbt(nc, es, "b0gsub", [128, 128], F32)
        dmask, bdmask = sbt(nc, es, "b0dmask", [128, 4, 128], BF16)
        S.dma("sp", lambda e: e.dma_start(out=gsub[:], in_=sg_ap), reads=[sg_b], writes=[bgsub])
        S.dma("sp", lambda e: e.dma_start(out=dmask[:], in_=dm_ap), reads=[dm_b], writes=[bdmask])
        with ExitStack() as ps:
            lt, blt = sbt(nc, ps, "b0lt", [1, 256], F32)
            lp, blp = sbt(nc, ps, "b0lp", [1, 2, 64], F32)
            ls, bls = sbt(nc, ps, "b0ls", [1, 2], F32)
            pl, bpl = pst(nc, ps, "b0pl", [128, 512], F32)
            S.dma("sp", lambda e: e.dma_start(out=lt[:], in_=lam_ap), reads=[lam_b], writes=[blt])
            S.op("dve", lambda e: e.tensor_tensor(out=lp[:], in0=AP(lt, 0, [[256, 1], [128, 2], [1, 64]]),
                                                  in1=AP(lt, 64, [[256, 1], [128, 2], [1, 64]]), op=ALU.mult),
                 reads=[blt], writes=[blp])
            S.op("dve", lambda e: e.tensor_reduce(out=ls[:], in_=lp[:], axis=AX.X, op=ALU.add), reads=[blp], writes=[bls])
            S.op("act", lambda e: e.activation(out=ls[:], in_=ls[:], func=AF.Exp), reads=[bls], writes=[bls])
            S.op("dve", lambda e: e.scalar_tensor_tensor(out=ls[0:1, 0:1], in0=ls[0:1, 1:2], scalar=-lam_init, in1=ls[0:1, 0:1],
                                                         op0=ALU.add, op1=ALU.subtract), reads=[bls], writes=[bls])
            S.op("pe", lambda e: e.matmul(pl[:, 0:1], lhsT=ones[0:1, :], rhs=ls[0:1, 0:1], start=True, stop=True),
                 reads=[bls, bones], writes=[bpl])
            S.op("dve", lambda e: e.tensor_copy(out=nlam[:], in_=pl[:, 0:1]), reads=[bpl], writes=[bnlam])
            S.flush()
        NCH = 16
        Qr = sring(nc, es, "b0q", [65, 2, 512], BF16, 3)
        Kr = sring(nc, es, "b0k", [65, 2, 4, 512], BF16, 4)
        Vr = sring(nc, es, "b0v", [128, 4, 4, 129], BF16, 4)
        Pr = sring(nc, es, "b0p", [128, 512], BF16, 5)
        STr = pring(nc, es, "b0st", [128, 512], F32, 4)
        accs = [pst(nc, es, "b0acc%d" % a, [128, 512], F32) for a in range(4)]
        rl, brl = sbt(nc, es, "b0rl", [128, 4], F32)
        o1r = sring(nc, es, "b0o1", [128, 128], F32, 2)
        o2r = sring(nc, es, "b0o2", [128, 128], F32, 2)
        sqr = sring(nc, es, "b0sq", [128, 128], F32, 2)
        str_ = sring(nc, es, "b0stt", [128, 2], F32, 2)
        Or = sring(nc, es, "b0o", [128, 128], BF16, 3)
        for (Kc, bK) in Kr.items:
            S.op("dve", lambda e, Kc=Kc: e.tensor_scalar(
                out=AP(Kc, 64 * 4096, [[4096, 1], [1, 4096]]),
                in0=AP(ones, 64 * 128, [[128, 1], [0, 4096]]), scalar1=kmx[64:65, 0:1], scalar2=None,
                op0=ALU.mult), reads=[bones, bkmx], writes=[bK])
        LA = 3
        pend = []

        def finish_tile(h, I, a):
            acc, bacc = accs[a]
            o1, bo1 = o1r.next()
            o2, bo2 = o2r.next()
            sq, bsq = sqr.next()
            stt, bstt = str_.next()
            Ot, bOt = Or.next()
            S.op("dve", lambda e: e.reciprocal(out=rl[:, 0:2], in_=AP(acc, 128, [[512, 128], [256, 2]])),
                 reads=[bacc], writes=[brl])
            S.op("dve", lambda e: e.tensor_tensor(out=rl[:, 2:3], in0=rl[:, 1:2], in1=nlam[:], op=ALU.mult),
                 reads=[brl, bnlam], writes=[brl])
            S.op("dve", lambda e: e.tensor_scalar(out=o1[:], in0=acc[:, 256:384], scalar1=rl[:, 2:3], scalar2=None, op0=ALU.mult),
                 reads=[bacc, brl], writes=[bo1])
            S.op("dve", lambda e: e.scalar_tensor_tensor(out=o2[:], in0=acc[:, 0:128], scalar=rl[:, 0:1], in1=o1[:],
                                                         op0=ALU.mult, op1=ALU.add), reads=[bacc, brl, bo1], writes=[bo2])
            S.op("act", lambda e: e.activation(out=sq[:], in_=o2[:], func=AF.Square, accum_out=stt[:, 0:1]),
                 reads=[bo2], writes=[bsq, bstt])
            f2 = (1.0 - lam_init) ** 2
            S.op("act", lambda e: e.activation(out=stt[:, 1:2], in_=stt[:, 0:1], func=AF.Sqrt, scale=1.0 / (128 * f2), bias=1e-6 / f2),
                 reads=[bstt], writes=[bstt])
            S.op("dve", lambda e: e.reciprocal(out=stt[:, 1:2], in_=stt[:, 1:2]), reads=[bstt], writes=[bstt])
            S.op("dve", lambda e: e.scalar_tensor_tensor(out=Ot[:], in0=o2[:], scalar=stt[:, 1:2], in1=gsub[:], op0=ALU.mult, op1=ALU.mult),
                 reads=[bo2, bstt, bgsub], writes=[bOt])
            tl = 4 * I + a
            S.dma("pool", lambda e: e.dma_start(out=O_ap[tl * 128:(tl + 1) * 128, h * 128:(h + 1) * 128], in_=Ot[:]),
                  reads=[bOt], writes=[O_b])

        def emit_pv(item):
            (h, I, kt, comp, a0, PT, bPT, Vc, bV, r, ip, diag, u) = item
            for a in range(a0, 4):
                acc, bacc = accs[a]
                last = (kt == 16 * I + 4 * a + 3)
                S.op("pe", lambda e, acc=acc, a=a, last=last: e.matmul(
                    acc[:, comp * 256: comp * 256 + 129], lhsT=PT[:, a * 128:(a + 1) * 128], rhs=Vc[:, r, ip, :],
                    start=(kt == 0 and comp == 0), stop=last, skip_group_check=True), reads=[bPT, bV], writes=[bacc])
            if diag and u == 3 and comp == 1:
                finish_tile(h, I, a0)

        for h in range(8):
            for I in range(8):
                Qc, bQ = Qr.next()
                S.dma("sp", lambda e, Qc=Qc, h=h, I=I: e.dma_start(
                    out=Qc[:, :, :], in_=QT_ap[2 * h:2 * h + 2, :, I * 512:(I + 1) * 512].rearrange("c r q -> r c q")),
                    reads=[QT_b], writes=[bQ])
                for ch in range(I + 1):
                    Kc, bK = Kr.next()
                    Vc, bV = Vr.next()
                    for r in range(4):
                        for comp in range(2):
                            S.dma("sp", lambda e, Kc=Kc, h=h, ch=ch, r=r, comp=comp: e.dma_start(
                                out=Kc[0:64, comp, r, :], in_=KT_ap[r, 2 * h + comp, :, ch * 512:(ch + 1) * 512]),
                                reads=[KT_b], writes=[bK])
                        S.dma("sp", lambda e, Vc=Vc, h=h, ch=ch, r=r: e.dma_start(
                            out=Vc[:, r, :, :], in_=V_ap[r, h, :, 4 * ch:4 * ch + 4, :]), reads=[V_b], writes=[bV])
                    diag = (ch == I)
                    for kl in range(NCH):
                        kt = 16 * ch + kl
                        ip, r = kl // 4, kl % 4
                        a0 = kl // 4 if diag else 0
                        u = kl % 4
                        for comp in range(2):
                            ST, bST = STr.next()
                            ksl = lambda Kc=Kc, comp=comp, r=r, ip=ip: Kc[:, comp, r, ip * 128:(ip + 1) * 128]
                            if diag:
                                S.op("pe", lambda e, ST=ST, ksl=ksl, Qc=Qc, comp=comp, a0=a0: e.matmul(
                                    ST[:, a0 * 128:(a0 + 1) * 128], lhsT=ksl(), rhs=Qc[:, comp, a0 * 128:(a0 + 1) * 128],
                                    start=True, stop=False), reads=[bK, bQ], writes=[bST])
                                S.op("pe", lambda e, ST=ST, a0=a0, u=u: e.matmul(
                                    ST[:, a0 * 128:(a0 + 1) * 128], lhsT=ident[:], rhs=dmask[:, u, :],
                                    start=False, stop=True), reads=[bid, bdmask], writes=[bST])
                                if a0 < 3:
                                    S.op("pe", lambda e, ST=ST, ksl=ksl, Qc=Qc, comp=comp, a0=a0: e.matmul(
                                        ST[:, (a0 + 1) * 128:512], lhsT=ksl(), rhs=Qc[:, comp, (a0 + 1) * 128:512],
                                        start=True, stop=True), reads=[bK, bQ], writes=[bST])
                            else:
                                S.op("pe", lambda e, ST=ST, ksl=ksl, Qc=Qc, comp=comp: e.matmul(
                                    ST[:, :], lhsT=ksl(), rhs=Qc[:, comp, :], start=True, stop=True),
                                    reads=[bK, bQ], writes=[bST])
                            PT, bPT = Pr.next()
                            S.op("act", lambda e, ST=ST, PT=PT, a0=a0: e.activation(
                                out=PT[:, a0 * 128:512], in_=ST[:, a0 * 128:512], func=AF.Exp), reads=[bST], writes=[bPT])
                            pend.append((h, I, kt, comp, a0, PT, bPT, Vc, bV, r, ip, diag, u))
                            if len(pend) > LA:
                                emit_pv(pend.pop(0))
        while pend:
            emit_pv(pend.pop(0))
        S.flush()


def phase_C(k, cst, mod, layer, o_name, xin_name, xout_name, wout_name, final):
    nc, S = k.nc, k.S
    ident, bid = cst["ident"]
    AB, bAB = mod["AB"]
    g1b, bg1b = mod["g1b"]
    g2b, bg2b = mod["g2b"]
    O_ap, O_b = k.dram[o_name]
    xi_ap, xi_b = k.dram[xin_name]
    xo_ap, xo_b = k.dram[xout_name]
    x1_ap, x1_b = k.dram["x1s%d" % layer]
    wo_ap, wo_b = k.dram[wout_name]
    wr_ap, wr_b = k.dram["moe_wr%d" % layer]
    br_ap, br_b = k.dram["moe_br%d" % layer]
    wg_ap, wg_b = k.dram["moe_wg%d" % layer]
    wu_ap, wu_b = k.dram["moe_wu%d" % layer]
    wd_ap, wd_b = k.dram["moe_wd%d" % layer]
    HT = 16
    with ExitStack() as es:
        yacc, byacc = sbt(nc, es, "c_yacc", [128, HT, 1024], F32)
        byt = [Buf() for _ in range(HT)]
        h2T, _ = sbt(nc, es, "c_h2T", [128, 8, HT * 128], BF16)
        bh2 = [Buf() for _ in range(HT)]
        comb, _ = sbt(nc, es, "c_comb", [128, HT, 32], F32)
        bcomb = [Buf() for _ in range(HT)]
        if final:
            fgb, bfgb = sbt(nc, es, "c_fgb", [128, 1024], F32)
            fg_ap, fg_b = k.dram["final_g"]
            S.dma("sp", lambda e: e.dma_start(out=fgb[:], in_=fg_ap), reads=[fg_b], writes=[bfgb])
        for half in range(2):
            with ExitStack() as p1:
                Wout, bWout = sbt(nc, p1, "c_wout", [128, 8, 1024], BF16)
                load_cast_weight(k, wo_ap, wo_b, Wout, bWout, 1024)
                Wr, bWr = sbt(nc, p1, "c_wr", [128, 8, 36], BF16)
                S.dma("pool", lambda e: e.dma_start(out=Wr[:], in_=wr_ap.rearrange("(c p) n -> p c n", p=128)),
                      reads=[wr_b], writes=[bWr])
                brt, bbrt = sbt(nc, p1, "c_br", [128, 36], F32)
                S.dma("sp", lambda e: e.dma_start(out=brt[:], in_=br_ap), reads=[br_b], writes=[bbrt])
                xr = sring(nc, p1, "c_x", [128, 1024], F32, 2)
                Otr = sring(nc, p1, "c_o", [128, 1024], BF16, 2)
                OTr = sring(nc, p1, "c_oT", [128, 8, 128], BF16, 2)
                x1r = sring(nc, p1, "c_x1", [128, 1024], F32, 2)
                tmpr = sring(nc, p1, "c_tmp", [128, 1024], F32, 1)
                ntmp = norm_tmp(nc, p1, "c")
                ppr = pring(nc, p1, "c_pp", [128, 1024], F32, 2)
                pOT = pring(nc, p1, "c_pOT", [128, 8, 128], BF16, 1)
                plg = pring(nc, p1, "c_plg", [128, 512], F32, 2)
                Lr = sring(nc, p1, "c_L", [128, 36], F32, 2)
                Lmr = sring(nc, p1, "c_Lm", [128, 32], F32, 2)
                smr = sring(nc, p1, "c_sm", [128, 24], F32, 2)
                e1r = sring(nc, p1, "c_e1", [128, 32], F32, 2)
                xss = {}

                def stage1(tl):
                        t = half * HT + tl
                        xt, bx = xr.next()
                        Ot, bOt = Otr.next()
                        S.dma("sp", lambda e, xt=xt, t=t: e.dma_start(out=xt[:], in_=xi_ap[t * 128:(t + 1) * 128, :]),
                              reads=[xi_b], writes=[bx])
                        S.dma("sp", lambda e, Ot=Ot, t=t: e.dma_start(out=Ot[:], in_=O_ap[t * 128:(t + 1) * 128, :]),
                              reads=[O_b], writes=[bOt])
                        pT, bpT = pOT.next()
                        for c in range(8):
                            S.op("pe", lambda e, pT=pT, Ot=Ot, c=c: e.transpose(out=pT[:, c, :], in_=Ot[:, c * 128:(c + 1) * 128],
                                                                              identity=ident[:]), reads=[bOt, bid], writes=[bpT])
                        OT, bOT = OTr.next()
                        S.op("act", lambda e, OT=OT, pT=pT: e.copy(out=OT[:], in_=pT[:]), reads=[bpT], writes=[bOT])
                        pp, bpp = ppr.next()
                        for nb in range(2):
                            for c in range(8):
                                S.op("pe", lambda e, pp=pp, OT=OT, c=c, nb=nb: e.matmul(
                                    pp[:, nb * 512:(nb + 1) * 512], lhsT=OT[:, c, :], rhs=Wout[:, c, nb * 512:(nb + 1) * 512],
                                    start=(c == 0), stop=(c == 7)), reads=[bOT, bWout], writes=[bpp])
                        tmp, btmp = tmpr.next()
                        x1, bx1 = x1r.next()
                        S.op("dve", lambda e, tmp=tmp, pp=pp: e.tensor_tensor(out=tmp[:], in0=pp[:], in1=g1b[:], op=ALU.mult),
                             reads=[bpp, bg1b], writes=[btmp])
                        S.op("dve", lambda e, tmp=tmp, x1=x1, xt=xt: e.tensor_tensor(out=x1[:], in0=tmp[:], in1=xt[:], op=ALU.add),
                             reads=[btmp, bx], writes=[bx1])
                        S.dma("pool", lambda e, x1=x1, t=t: e.dma_start(out=x1_ap[t * 128:(t + 1) * 128, :], in_=x1[:]),
                              reads=[bx1], writes=[x1_b])
                        xss[tl] = emit_norm_T_a(k, cst, x1[:], bx1, ntmp)

                def stage2(tl):
                    xs, bxs = xss[tl]
                    hT = AP(h2T, tl * 128, [[8 * HT * 128, 128], [HT * 128, 8], [1, 128]])
                    emit_norm_T_b(k, cst, xs, bxs, AB, bAB, 16, hT, bh2[tl], ntmp)
                    pl, bpl = plg.next()
                    for c in range(8):
                        S.op("pe", lambda e, pl=pl, tl=tl, c=c: e.matmul(
                            pl[:, 0:36], lhsT=h2T[:, c, tl * 128:(tl + 1) * 128], rhs=Wr[:, c, :],
                            start=(c == 0), stop=(c == 7)), reads=[bh2[tl], bWr], writes=[bpl])
                    L, bL = Lr.next()
                    Lm, bLm = Lmr.next()
                    sm, bsm = smr.next()
                    e1, be1 = e1r.next()
                    S.op("dve", lambda e, L=L, pl=pl: e.tensor_tensor(out=L[:], in0=pl[:, 0:36], in1=brt[:], op=ALU.add),
                         reads=[bpl, bbrt], writes=[bL])
                    S.op("dve", lambda e, L=L, sm=sm: e.tensor_reduce(out=sm[:, 0:1], in_=L[:, 0:4], axis=AX.X, op=ALU.max),
                         reads=[bL], writes=[bsm])
                    S.op("dve", lambda e, sm=sm: e.tensor_scalar(out=sm[:, 1:2], in0=sm[:, 0:1], scalar1=-1.0, scalar2=None,
                                                                 op0=ALU.mult), reads=[bsm], writes=[bsm])
                    S.op("act", lambda e, L=L, sm=sm: e.activation(out=sm[:, 20:24], in_=L[:, 0:4], func=AF.Exp, bias=sm[:, 1:2],
                                                                   scale=1.0, accum_out=sm[:, 2:3]), reads=[bL, bsm], writes=[bsm])
                    S.op("dve", lambda e, sm=sm: e.reciprocal(out=sm[:, 3:4], in_=sm[:, 2:3]), reads=[bsm], writes=[bsm])
                    S.op("dve", lambda e, L=L, sm=sm: e.tensor_scalar(out=sm[:, 4:8], in0=L[:, 0:4], scalar1=sm[:, 0:1], scalar2=None,
                                                                      op0=ALU.is_ge), reads=[bL, bsm], writes=[bsm])
                    S.op("dve", lambda e, sm=sm: e.tensor_scalar(out=sm[:, 4:8], in0=sm[:, 4:8], scalar1=1e30, scalar2=-1e30,
                                                                 op0=ALU.mult, op1=ALU.add), reads=[bsm], writes=[bsm])
                    S.op("dve", lambda e, L=L, Lm=Lm, sm=sm: e.tensor_tensor(
                        out=AP(Lm, 0, [[32, 128], [8, 4], [1, 8]]), in0=AP(L, 4, [[36, 128], [8, 4], [1, 8]]),
                        in1=AP(sm, 4, [[24, 128], [1, 4], [0, 8]]), op=ALU.add), reads=[bL, bsm], writes=[bLm])
                    S.op("dve", lambda e, Lm=Lm, sm=sm: e.max(out=sm[:, 8:16], in_=Lm[:]), reads=[bLm], writes=[bsm])
                    S.op("dve", lambda e, sm=sm: e.tensor_tensor(out=sm[:, 16:17], in0=sm[:, 9:10], in1=sm[:, 8:9], op=ALU.subtract),
                         reads=[bsm], writes=[bsm])
                    S.op("act", lambda e, sm=sm: e.activation(out=sm[:, 17:18], in_=sm[:, 16:17], func=AF.Exp), reads=[bsm], writes=[bsm])
                    S.op("dve", lambda e, sm=sm: e.tensor_scalar(out=sm[:, 18:19], in0=sm[:, 17:18], scalar1=1.0, scalar2=None,
                                                                 op0=ALU.add), reads=[bsm], writes=[bsm])
                    S.op("dve", lambda e, sm=sm: e.reciprocal(out=sm[:, 18:19], in_=sm[:, 18:19]), reads=[bsm], writes=[bsm])
                    S.op("dve", lambda e, sm=sm: e.tensor_tensor(out=sm[:, 18:19], in0=sm[:, 18:19], in1=sm[:, 3:4], op=ALU.mult),
                         reads=[bsm], writes=[bsm])
                    S.op("dve", lambda e, sm=sm: e.tensor_tensor(out=sm[:, 19:20], in0=sm[:, 18:19], in1=sm[:, 17:18], op=ALU.mult),
                         reads=[bsm], writes=[bsm])
                    S.op("dve", lambda e, Lm=Lm, sm=sm, e1=e1: e.tensor_scalar(out=e1[:], in0=Lm[:], scalar1=sm[:, 8:9], scalar2=sm[:, 18:19],
                                                                               op0=ALU.is_equal, op1=ALU.mult), reads=[bLm, bsm], writes=[be1])
                    S.op("dve", lambda e, Lm=Lm, sm=sm, tl=tl: e.tensor_scalar(out=comb[:, tl, :], in0=Lm[:], scalar1=sm[:, 9:10],
                                                                               scalar2=sm[:, 19:20], op0=ALU.is_equal, op1=ALU.mult),
                         reads=[bLm, bsm], writes=[bcomb[tl]])
                    S.op("dve", lambda e, e1=e1, tl=tl: e.tensor_tensor(out=comb[:, tl, :], in0=comb[:, tl, :], in1=e1[:], op=ALU.add),
                         reads=[be1, bcomb[tl]], writes=[bcomb[tl]])

                stage1(0)
                for tl in range(HT):
                    if tl + 1 < HT:
                        stage1(tl + 1)
                    stage2(tl)
                S.flush()
            with ExitStack() as p2:
                Wgr = sring(nc, p2, "c_wgu", [128, 8, 2, 512], BF16, 2)
                Wdr = sring(nc, p2, "c_wd", [128, 2, 2, 1024], BF16, 2)
                gur = pring(nc, p2, "c_gu", [128, 512], F32, 2)
                yr = pring(nc, p2, "c_y", [128, 1024], F32, 2)
                pATr = pring(nc, p2, "c_pAT", [128, 2, 128], BF16, 2)
                sgr = sring(nc, p2, "c_sg", [128, 256], F32, 4)
                Ar = sring(nc, p2, "c_A", [128, 256], BF16, 4)
                ATr = sring(nc, p2, "c_AT", [128, 2, 128], BF16, 4)
                units = []

                def stage_G(u):
                    (ep, tl, e2, Wg, bWg, Wd, bWd) = u["k"]
                    gu, bgu = gur.next()
                    for c in range(8):
                        S.op("pe", lambda e, c=c: e.matmul(
                            gu[:, :], lhsT=h2T[:, c, tl * 128:(tl + 1) * 128], rhs=Wg[:, c, e2, :],
                            start=(c == 0), stop=(c == 7)), reads=[bh2[tl], bWg], writes=[bgu])
                    sg, bsg = sgr.next()
                    A, bA = Ar.next()
                    ex = 2 * ep + e2
                    S.op("act", lambda e: e.activation(out=sg[:], in_=gu[:, 0:256], func=AF.Silu), reads=[bgu], writes=[bsg])
                    S.op("dve", lambda e: e.scalar_tensor_tensor(
                        out=A[:], in0=gu[:, 256:512], scalar=comb[:, tl, ex:ex + 1], in1=sg[:], op0=ALU.mult, op1=ALU.mult),
                        reads=[bgu, bsg, bcomb[tl]], writes=[bA])
                    u["A"] = (A, bA)

                def stage_T(u):
                    A, bA = u["A"]
                    pAT, bpAT = pATr.next()
                    for hh in range(2):
                        S.op("pe", lambda e, hh=hh: e.transpose(out=pAT[:, hh, :], in_=A[:, hh * 128:(hh + 1) * 128], identity=ident[:]),
                             reads=[bA, bid], writes=[bpAT])
                    AT, bAT = ATr.next()
                    S.op("act", lambda e: e.copy(out=AT[:], in_=pAT[:]), reads=[bpAT], writes=[bAT])
                    u["AT"] = (AT, bAT)

                ycur = {}

                def stage_D(u):
                    (ep, tl, e2, Wg, bWg, Wd, bWd) = u["k"]
                    AT, bAT = u["AT"]
                    if e2 == 0:
                        ycur[tl] = yr.next()
                    y, by = ycur[tl]
                    for nb in range(2):
                        for hh in range(2):
                            S.op("pe", lambda e, nb=nb, hh=hh: e.matmul(
                                y[:, nb * 512:(nb + 1) * 512], lhsT=AT[:, hh, :], rhs=Wd[:, e2, hh, nb * 512:(nb + 1) * 512],
                                start=(e2 == 0 and hh == 0), stop=(e2 == 1 and hh == 1)), reads=[bAT, bWd], writes=[by])
                    if e2 == 1:
                        if ep == 0:
                            S.op("dve", lambda e: e.tensor_copy(out=yacc[:, tl, :], in_=y[:]), reads=[by], writes=[byt[tl]])
                        else:
                            S.op("dve", lambda e: e.tensor_tensor(out=yacc[:, tl, :], in0=yacc[:, tl, :], in1=y[:], op=ALU.add),
                                 reads=[by, byt[tl]], writes=[byt[tl]])

                for ep in range(16):
                    Wg, bWg = Wgr.next()
                    Wd, bWd = Wdr.next()
                    for e2 in range(2):
                        ex = 2 * ep + e2
                        S.dma("pool", lambda e, Wg=Wg, ex=ex, e2=e2: e.dma_start(
                            out=Wg[:, :, e2, 0:256], in_=wg_ap[ex].rearrange("(c p) n -> p c n", p=128)),
                            reads=[wg_b], writes=[bWg])
                        S.dma("pool", lambda e, Wg=Wg, ex=ex, e2=e2: e.dma_start(
                            out=Wg[:, :, e2, 256:512], in_=wu_ap[ex].rearrange("(c p) n -> p c n", p=128)),
                            reads=[wu_b], writes=[bWg])
                        S.dma("pool", lambda e, Wd=Wd, ex=ex, e2=e2: e.dma_start(
                            out=Wd[:, e2, :, :], in_=wd_ap[ex].rearrange("(c p) n -> p c n", p=128)),
                            reads=[wd_b], writes=[bWd])
                    for tl in range(HT):
                        for e2 in range(2):
                            units.append({"k": (ep, tl, e2, Wg, bWg, Wd, bWd)})
                            n = len(units) - 1
                            stage_G(units[n])
                            if n >= 1:
                                stage_T(units[n - 1])
                            if n >= 2:
                                stage_D(units[n - 2])
                n = len(units)
                stage_T(units[n - 1])
                stage_D(units[n - 2])
                stage_D(units[n - 1])
                S.flush()
            with ExitStack() as p3:
                x1r = sring(nc, p3, "c3_x1", [128, 1024], F32, 2)
                tmpr = sring(nc, p3, "c3_tmp", [128, 1024], F32, 2)
                x2r = sring(nc, p3, "c3_x2", [128, 1024], F32, 2)
                sqr = sring(nc, p3, "c3_sq", [128, 1024], F32, 1)
                str_ = sring(nc, p3, "c3_st", [128, 2], F32, 2)
                for tl in range(HT):
                    t = half * HT + tl
                    x1, bx1 = x1r.next()
                    S.dma("sp", lambda e, x1=x1, t=t: e.dma_start(out=x1[:], in_=x1_ap[t * 128:(t + 1) * 128, :]),
                          reads=[x1_b], writes=[bx1])
                    tmp, btmp = tmpr.next()
                    x2, bx2 = x2r.next()
                    S.op("dve", lambda e, tmp=tmp, tl=tl: e.tensor_tensor(out=tmp[:], in0=yacc[:, tl, :], in1=g2b[:], op=ALU.mult),
                         reads=[byt[tl], bg2b], writes=[btmp])
                    S.op("dve", lambda e, tmp=tmp, x1=x1, x2=x2: e.tensor_tensor(out=x2[:], in0=tmp[:], in1=x1[:], op=ALU.add),
                         reads=[btmp, bx1], writes=[bx2])
                    if final:
                        sq, bsq = sqr.next()
                        st, bst = str_.next()
                        S.op("act", lambda e, sq=sq, x2=x2, st=st: e.activation(out=sq[:], in_=x2[:], func=AF.Square, accum_out=st[:, 0:1]),
                             reads=[bx2], writes=[bsq, bst])
                        S.op("act", lambda e, st=st: e.activation(out=st[:, 1:2], in_=st[:, 0:1], func=AF.Sqrt, scale=1.0 / D, bias=1e-6),
                             reads=[bst], writes=[bst])
                        S.op("dve", lambda e, st=st: e.reciprocal(out=st[:, 1:2], in_=st[:, 1:2]), reads=[bst], writes=[bst])
                        S.op("dve", lambda e, x2=x2, st=st, tmp=tmp: e.scalar_tensor_tensor(
                            out=tmp[:], in0=x2[:], scalar=st[:, 1:2], in1=fgb[:], op0=ALU.mult, op1=ALU.mult),
                            reads=[bx2, bst, bfgb], writes=[btmp])
                        S.dma("pool", lambda e, tmp=tmp, t=t: e.dma_start(out=xo_ap[t * 128:(t + 1) * 128, :], in_=tmp[:]),
                              reads=[btmp], writes=[xo_b])
                    else:
                        S.dma("pool", lambda e, x2=x2, t=t: e.dma_start(out=xo_ap[t * 128:(t + 1) * 128, :], in_=x2[:]),
                              reads=[bx2], writes=[xo_b])
                S.flush()


def make_dmask(j):
    m = np.zeros((128, 4, 128), np.float32)
    kk = np.arange(128)[:, None]
    qq = np.arange(128)[None, :]
    for u in range(4):
        if u == j:
            m[:, u, :] = np.where(kk > qq, NEG, 0.0)
        elif u > j:
            m[:, u, :] = NEG
    return m.astype(NPBF)


def declare_moe(k, l):
    k.din("moe_wr%d" % l, [1024, 36], F32)
    k.din("moe_br%d" % l, [128, 36], F32)
    k.din("moe_wg%d" % l, [32, 1024, 256], F32)
    k.din("moe_wu%d" % l, [32, 1024, 256], F32)
    k.din("moe_wd%d" % l, [32, 256, 1024], F32)


def moe_inputs(m, l, moe_w_group, moe_b_group, moe_w_expert, moe_b_expert, moe_w_gate, moe_w_up, moe_w_down):
    wr = np.concatenate([moe_w_group[l]] + [moe_w_expert[l, g] for g in range(4)], axis=1)
    br = np.concatenate([moe_b_group[l]] + [moe_b_expert[l, g] for g in range(4)], axis=0)
    m["moe_wr%d" % l] = np.ascontiguousarray(wr)
    m["moe_br%d" % l] = np.ascontiguousarray(np.broadcast_to(br[None, :], (128, 36)))
    m["moe_wg%d" % l] = moe_w_gate[l]
    m["moe_wu%d" % l] = moe_w_up[l]
    m["moe_wd%d" % l] = moe_w_down[l]


def build_L2(with_A1=False):
    nc = bass.Bass("TRN2", target_bir_lowering=False)
    with ExitStack() as es:
        k = K(nc, es)
        declare_common(k, [0, 1] if with_A1 else [0])
        k.din("x", [TOK, D], F32)
        k.din("QT0", [16, 65, TOK], BF16)
        k.din("KT0g", [4, 16, 64, TOK], BF16)
        k.din("V0g", [4, 8, 128, NT, 129], BF16)
        k.din("knm0g", [128, 4], F32)
        k.din("lam", [1, 256], F32)
        k.din("subg", [128, 128], F32)
        k.din("dmask", [128, 4, 128], BF16)
        k.din("diff_w_out", [D, D], F32)
        declare_moe(k, 0)
        k.dint("O0", [TOK, D], BF16)
        k.dint("x1s0", [TOK, D], F32)
        k.dout("xm0", [TOK, D], F32)
        cst = emit_consts(k, es)
        mod = emit_mod(k, es, cst, 0, "m0")
        phase_B0(k, cst)
        phase_C(k, cst, mod, 0, "O0", "x", "xm0", "diff_w_out", False)
    return nc


def phase_A1(k, cst, mod, cs, bcs, xin_name):
    nc, S = k.nc, k.S
    ident, bid = cst["ident"]
    AB, bAB = mod["AB"]
    x_ap, x_b = k.dram[xin_name]
    w_ap, w_b = k.dram["nsa_w_in"]
    QT_ap, QT_b = k.dram["QT1"]
    kc_ap, kc_b = k.dram["kcT1"]
    vc_ap, vc_b = k.dram["vcT1"]
    ks_ap, ks_b = k.dram["ksT1"]
    kw_ap, kw_b = k.dram["kwT1"]
    vs_ap, vs_b = k.dram["vs1"]
    vw_ap, vw_b = k.dram["vw1"]
    kn_ap, kn_b = k.dram["knm1"]
    gt_ap, gt_b = k.dram["gates1"]
    with ExitStack() as es:
        Win, bWin = sbt(nc, es, "a1win", [128, 8, 1840], BF16)
        load_cast_weight(k, w_ap, w_b, Win, bWin, 1840, piece=368)
        xr = sring(nc, es, "a1x", [128, 1024], F32, 2)
        hr = sring(nc, es, "a1hT", [128, 8, 128], BF16, 2)
        ntmp = norm_tmp(nc, es, "a1")
        ppr = pring(nc, es, "a1pp", [128, 1024], F32, 2)
        pTr = pring(nc, es, "a1pqt", [128, 16, 128], BF16, 1)
        Qsr = sring(nc, es, "a1qs", [128, 16, 65], BF16, 2)
        Rr = sring(nc, es, "a1r", [128, 8, 64], BF16, 2)
        Cr = sring(nc, es, "a1c", [128, 256], BF16, 2)
        R32r = sring(nc, es, "a1r32", [128, 512], F32, 2)
        QTr = sring(nc, es, "a1qts", [128, 16, 128], BF16, 2)
        KTr = sring(nc, es, "a1kts", [128, 6, 128], BF16, 2)
        Gr = sring(nc, es, "a1g", [128, 48], F32, 2)
        rtmp, brtmp = sbt(nc, es, "a1rt", [128, 4, 16, 8], F32)
        sqv, bsqv = sbt(nc, es, "a1sqv", [128, 16, 64], F32)
        nrm, bnrm = sbt(nc, es, "a1nrm", [128, 16], F32)
        knm, bknm = sbt(nc, es, "a1knm", [128, 4], F32)
        S.op("dve", lambda e: e.memset(knm[:], 0.0), writes=[bknm])
        hts = {}

        def normT(t):
            xt, bx = xr.next()
            S.dma("sp", lambda e: e.dma_start(out=xt[:], in_=x_ap[t * 128:(t + 1) * 128, :]), reads=[x_b], writes=[bx])
            hT, bhT = hr.next()
            emit_norm_T(k, cst, xt[:], bx, AB, bAB, 0, hT, bhT, ntmp)
            hts[t] = (hT, bhT)

        def qmm(t):
            hT, bhT = hts[t]
            pp, bpp = ppr.next()
            for nb in range(2):
                for c in range(8):
                    S.op("pe", lambda e, c=c, nb=nb: e.matmul(
                        pp[:, nb * 512:(nb + 1) * 512], lhsT=hT[:, c, :], rhs=Win[:, c, nb * 512:(nb + 1) * 512],
                        start=(c == 0), stop=(c == 7)), reads=[bhT, bWin], writes=[bpp])
            return pp, bpp

        def qpost(t, pp, bpp):
            src = lambda d0, d1: AP(pp, d0, [[1024, 128], [64, 16], [1, d1 - d0]])
            Qs, bQs = Qsr.next()
            dst = lambda d0, d1: AP(Qs, d0, [[1040, 128], [65, 16], [1, d1 - d0]])
            emit_rope(k, src, bpp, dst, bQs, 16, 65, cs, bcs, t, 16, rtmp, brtmp)
            S.op("act", lambda e: e.activation(out=dst(16, 64), in_=src(16, 64), func=AF.Copy, scale=0.125), reads=[bpp], writes=[bQs])
            S.op("dve", lambda e: e.tensor_tensor(out=sqv[:], in0=dst(0, 64), in1=dst(0, 64), op=ALU.mult), reads=[bQs], writes=[bsqv])
            S.op("dve", lambda e: e.tensor_reduce(out=nrm[:], in_=sqv[:], axis=AX.X, op=ALU.add), reads=[bsqv], writes=[bnrm])
            S.op("act", lambda e: e.activation(out=nrm[:], in_=nrm[:], func=AF.Sqrt), reads=[bnrm], writes=[bnrm])
            S.op("dve", lambda e: e.tensor_scalar(out=dst(64, 65), in0=AP(nrm, 0, [[16, 128], [1, 16], [1, 1]]),
                                                  scalar1=-1.0, scalar2=None, op0=ALU.mult), reads=[bnrm], writes=[bQs])
            return Qs, bQs

        def qtr(t, Qs, bQs):
            pT, bpT = pTr.next()
            for hc in range(16):
                S.op("pe", lambda e, hc=hc: e.transpose(out=pT[0:65, hc, :], in_=Qs[:, hc, :], identity=ident[:]),
                     reads=[bQs, bid], writes=[bpT])
            QTs, bQTs = QTr.next()
            S.op("act", lambda e: e.copy(out=QTs[0:65, :, :], in_=pT[0:65, :, :]), reads=[bpT], writes=[bQTs])
            S.dma("sp", lambda e: e.dma_start(out=QT_ap[:, :, t * 128:(t + 1) * 128].rearrange("h r q -> r h q"), in_=QTs[0:65, :, :]),
                  reads=[bQTs], writes=[QT_b])

        def kvmm(t):
            hT, bhT = hts[t]
            pp, bpp = ppr.next()
            for nb, (c0, c1) in enumerate(((1024, 1536), (1536, 1840))):
                for c in range(8):
                    S.op("pe", lambda e, c=c, nb=nb, c0=c0, c1=c1: e.matmul(
                        pp[:, nb * 512: nb * 512 + (c1 - c0)], lhsT=hT[:, c, :], rhs=Win[:, c, c0:c1],
                        start=(c == 0), stop=(c == 7)), reads=[bhT, bWin], writes=[bpp])
            return pp, bpp

        def kvpost(t, pp, bpp):
            Ct, bCt = Cr.next()
            S.op("act", lambda e: e.copy(out=Ct[:], in_=pp[:, 0:256]), reads=[bpp], writes=[bCt])
            R32, bR32 = R32r.next()
            S.op("act", lambda e: e.copy(out=R32[:], in_=pp[:, 256:768]), reads=[bpp], writes=[bR32])
            src = lambda d0, d1: AP(R32, d0, [[512, 128], [64, 8], [1, d1 - d0]])
            Rt, bRt = Rr.next()
            dst = lambda d0, d1: AP(Rt, d0, [[512, 128], [64, 8], [1, d1 - d0]])
            emit_rope(k, src, bR32, dst, bRt, 8, 64, cs, bcs, t, 0, rtmp, brtmp)
            S.op("act", lambda e: e.copy(out=dst(16, 64), in_=src(16, 64)), reads=[bR32], writes=[bRt])
            Gt, bGt = Gr.next()
            S.op("act", lambda e: e.activation(out=Gt[:], in_=pp[:, 768:816], func=AF.Sigmoid), reads=[bpp], writes=[bGt])
            S.dma("pool", lambda e: e.dma_start(out=gt_ap[t * 128:(t + 1) * 128, :], in_=Gt[:]), reads=[bGt], writes=[gt_b])
            S.op("dve", lambda e: e.tensor_tensor(out=sqv[:, 0:8, :], in0=Rt[:], in1=Rt[:], op=ALU.mult), reads=[bRt], writes=[bsqv])
            S.op("dve", lambda e: e.tensor_reduce(out=nrm[:, 0:8], in_=sqv[:, 0:8, :], axis=AX.X, op=ALU.add), reads=[bsqv], writes=[bnrm])
            S.op("dve", lambda e: e.tensor_reduce(out=knm[:, 2:3], in_=nrm[:, 0:2], axis=AX.X, op=ALU.max), reads=[bnrm], writes=[bknm])
            S.op("dve", lambda e: e.tensor_reduce(out=knm[:, 3:4], in_=nrm[:, 4:6], axis=AX.X, op=ALU.max), reads=[bnrm], writes=[bknm])
            S.op("dve", lambda e: e.tensor_tensor(out=knm[:, 0:2], in0=knm[:, 0:2], in1=knm[:, 2:4], op=ALU.max), reads=[bknm], writes=[bknm])
            return Ct, bCt, Rt, bRt

        def kvtr(t, Ct, bCt, Rt, bRt):
            pT, bpT = pTr.next()
            S.op("pe", lambda e: e.transpose(out=pT[:, 0, :], in_=Ct[:, 0:128], identity=ident[:]), reads=[bCt, bid], writes=[bpT])
            S.op("pe", lambda e: e.transpose(out=pT[:, 1, :], in_=Ct[:, 128:256], identity=ident[:]), reads=[bCt, bid], writes=[bpT])
            for ii, hh in enumerate((0, 1, 4, 5)):
                S.op("pe", lambda e, ii=ii, hh=hh: e.transpose(out=pT[0:64, 2 + ii, :], in_=Rt[:, hh, :], identity=ident[:]),
                     reads=[bRt, bid], writes=[bpT])
            KTs, bKTs = KTr.next()
            S.op("act", lambda e: e.copy(out=KTs[:, 0:2, :], in_=pT[:, 0:2, :]), reads=[bpT], writes=[bKTs])
            S.op("act", lambda e: e.copy(out=KTs[0:64, 2:6, :], in_=pT[0:64, 2:6, :]), reads=[bpT], writes=[bKTs])
            sl = slice(t * 128, (t + 1) * 128)
            S.dma("sp", lambda e: e.dma_start(out=kc_ap[:, sl], in_=KTs[:, 0, :]), reads=[bKTs], writes=[kc_b])
            S.dma("sp", lambda e: e.dma_start(out=vc_ap[:, sl], in_=KTs[:, 1, :]), reads=[bKTs], writes=[vc_b])
            S.dma("sp", lambda e: e.dma_start(out=ks_ap[:, :, sl].rearrange("g d q -> d g q"), in_=KTs[0:64, 2:4, :]), reads=[bKTs], writes=[ks_b])
            S.dma("sp", lambda e: e.dma_start(out=kw_ap[:, :, sl].rearrange("g d q -> d g q"), in_=KTs[0:64, 4:6, :]), reads=[bKTs], writes=[kw_b])
            S.dma("pool", lambda e: e.dma_start(out=vs_ap[sl, :].rearrange("p (g d) -> p g d", g=2), in_=Rt[:, 2:4, :]), reads=[bRt], writes=[vs_b])
            S.dma("pool", lambda e: e.dma_start(out=vw_ap[sl, :].rearrange("p (g d) -> p g d", g=2), in_=Rt[:, 6:8, :]), reads=[bRt], writes=[vw_b])

        normT(0)
        for t in range(NT):
            ppq = qmm(t)
            Qs = qpost(t, *ppq)
            ppkv = kvmm(t)
            kv = kvpost(t, *ppkv)
            qtr(t, *Qs)
            if t + 1 < NT:
                normT(t + 1)
            kvtr(t, *kv)
        S.dma("sp", lambda e: e.dma_start(out=kn_ap, in_=knm[:, 0:2]), reads=[bknm], writes=[kn_b])
        S.flush()


def emit_kmax_multi(k, es, cst, knm_name, tag, G):
    nc, S = k.nc, k.S
    kn_ap, kn_b = k.dram[knm_name]
    identf, bidf = cst["identf"]
    ones, bones = cst["ones"]
    kmx, bkmx = sbt(nc, es, "kmx" + tag, [128, G], F32)
    with ExitStack() as ps:
        a, ba = sbt(nc, ps, "kma" + tag, [128, G, 4], F32)
        m, bm = sbt(nc, ps, "kmm" + tag, [128, G], F32)
        r, br = sbt(nc, ps, "kmr" + tag, [1, G, 128], F32)
        s, bs = sbt(nc, ps, "kms" + tag, [1, G], F32)
        p1, bp1 = pst(nc, ps, "kmp" + tag, [128, 512], F32)
        S.dma("sp", lambda e: e.dma_start(out=a[:], in_=kn_ap.rearrange("p (g r) -> p g r", g=G)), reads=[kn_b], writes=[ba])
        S.op("dve", lambda e: e.tensor_reduce(out=m[:], in_=a[:], axis=AX.X, op=ALU.max), reads=[ba], writes=[bm])
        for g in range(G):
            S.op("pe", lambda e, g=g: e.transpose(out=p1[0:1, g * 128:(g + 1) * 128], in_=m[:, g:g + 1], identity=identf[:]),
                 reads=[bm, bidf], writes=[bp1])
        S.op("dve", lambda e: e.tensor_copy(out=r[:], in_=p1[0:1, 0:G * 128]), reads=[bp1], writes=[br])
        S.op("dve", lambda e: e.tensor_reduce(out=s[:], in_=r[:], axis=AX.X, op=ALU.max), reads=[br], writes=[bs])
        S.op("act", lambda e: e.activation(out=s[:], in_=s[:], func=AF.Sqrt, scale=1.02), reads=[bs], writes=[bs])
        S.op("pe", lambda e: e.matmul(p1[:, 384:384 + G], lhsT=ones[0:1, :], rhs=s[0:1, 0:G], start=True, stop=True),
             reads=[bs, bones, bp1], writes=[bp1])
        S.op("dve", lambda e: e.tensor_copy(out=kmx[:], in_=p1[:, 384:384 + G]), reads=[bp1], writes=[bkmx])
        S.flush()
    return kmx, bkmx


def load_global_T(k, dst, bdst, nrows, p0, src_ap, src_b, width):
    S = k.S
    for r in range(4):
        S.dma("sp", lambda e, r=r: e.dma_start(
            out=AP(dst, p0 * width + r * 128, [[width, nrows], [512, NT], [1, 128]]),
            in_=src_ap(r).rearrange("d (i p) -> d i p", p=128)), reads=[src_b], writes=[bdst])


def phase_B1(k, cst):
    nc, S = k.nc, k.S
    ident, bid = cst["ident"]
    ones, bones = cst["ones"]
    QT_ap, QT_b = k.dram["QT1"]
    kc_ap, kc_b = k.dram["kcT1g"]
    vc_ap, vc_b = k.dram["vcT1g"]
    ks_ap, ks_b = k.dram["ksT1g"]
    kw_ap, kw_b = k.dram["kwT1g"]
    vs_ap, vs_b = k.dram["vs1g"]
    vw_ap, vw_b = k.dram["vw1g"]
    gt_ap, gt_b = k.dram["gates1"]
    O_ap, O_b = k.dram["O1"]
    w1_ap, w1_b = k.dram["cmp_w1"]
    b1_ap, b1_b = k.dram["cmp_b1c"]
    w2_ap, w2_b = k.dram["cmp_w2"]
    pos_ap, pos_b = k.dram["cmp_posT"]
    em_ap, em_b = k.dram["emat"]
    dm_ap, dm_b = k.dram["dmaskb"]
    wm_ap, wm_b = k.dram["wmaskb"]
    cm_ap, cm_b = k.dram["cmaskb"]
    cb_ap, cb_b = k.dram["cmaskTb"]
    cf_ap, cf_b = k.dram["cforce"]
    W = SEQ
    with ExitStack() as es:
        kmx, bkmx = emit_kmax_multi(k, es, cst, "knm1g", "b1", 2)
        kcmpT, bkcmpT = sbt(nc, es, "b1kcmpT", [65, 2, 1024], BF16)
        vcmp, bvcmp = sbt(nc, es, "b1vcmp", [128, 8, 2, 65], BF16)
        gts, bgts = sbt(nc, es, "b1gts", [128, NT, 48], F32)
        S.dma("sp", lambda e: e.dma_start(out=gts[:], in_=gt_ap.rearrange("(t p) c -> p t c", p=128)), reads=[gt_b], writes=[bgts])
        S.op("dve", lambda e: e.memset(kcmpT[:], 0.0), writes=[bkcmpT])
        S.op("dve", lambda e: e.memset(vcmp[:], 0.0), writes=[bvcmp])
        S.op("dve", lambda e: e.memset(vcmp[:, :, :, 64:65], 1.0), writes=[bvcmp])
        with ExitStack() as ps:
            cT, bcT = sbt(nc, ps, "b1cT", [128, W], BF16)
            W1, bW1 = sbt(nc, ps, "b1W1", [128, 32, 256], BF16)
            W2, bW2 = sbt(nc, ps, "b1W2", [128, 2, 64], BF16)
            posT, bposT = sbt(nc, ps, "b1posT", [128, 32], BF16)
            b1c, bb1c = sbt(nc, ps, "b1b1c", [128, 4], F32)
            cbias, bcbias = sbt(nc, ps, "b1cbias", [128, 2], F32)
            Hd, bHd = sbt(nc, ps, "b1Hd", [128, 2, 1024], BF16)
            ur = sring(nc, ps, "b1u", [128, 512], F32, 2)
            tr_ = sring(nc, ps, "b1t", [128, 512], F32, 2)
            sqk, bsqk = sbt(nc, ps, "b1sqk", [64, 512], BF16)
            kn, bkn = sbt(nc, ps, "b1kn", [1, 8], F32)
            onesb, bonesb = sbt(nc, ps, "b1onesb", [128, 1], BF16)
            ph = pring(nc, ps, "b1ph", [128, 512], F32, 2)
            pk = pring(nc, ps, "b1pk", [128, 512], F32, 2)
            pb, bpb = pst(nc, ps, "b1pb", [128, 512], F32)
            S.op("dve", lambda e: e.memset(onesb[:], 1.0), writes=[bonesb])
            S.op("dve", lambda e: e.memset(Hd[:], 0.0), writes=[bHd])
            S.op("dve", lambda e: e.memset(kn[:], 0.0), writes=[bkn])
            S.dma("sp", lambda e: e.dma_start(out=b1c[:], in_=b1_ap), reads=[b1_b], writes=[bb1c])
            for kv in range(2):
                src_ap, src_b = (kc_ap, kc_b) if kv == 0 else (vc_ap, vc_b)
                load_global_T(k, cT, bcT, 128, 0, lambda r, src_ap=src_ap: src_ap[r], src_b, W)
                for half in range(2):
                    S.dma("pool", lambda e, kv=kv, half=half: e.dma_start(
                        out=W1[half * 64:(half + 1) * 64, :, :], in_=w1_ap[kv].rearrange("(j d) n -> d j n", d=64)),
                        reads=[w1_b], writes=[bW1])
                    S.dma("pool", lambda e, kv=kv, half=half: e.dma_start(
                        out=posT[half * 64:(half + 1) * 64, :], in_=pos_ap[kv]), reads=[pos_b], writes=[bposT])
                S.dma("pool", lambda e, kv=kv: e.dma_start(out=W2[:], in_=w2_ap[kv].rearrange("(c p) n -> p c n", p=128)),
                      reads=[w2_b], writes=[bW2])
                for half in range(2):
                    for j in range(32):
                        S.op("pe", lambda e, half=half, j=j: e.matmul(
                            pb[:, half:half + 1], lhsT=W1[0:64, j, half * 128:(half + 1) * 128], rhs=posT[0:64, j:j + 1],
                            start=(j == 0 and half == 0), stop=(j == 31), skip_group_check=True), reads=[bW1, bposT], writes=[bpb])
                S.op("dve", lambda e, kv=kv: e.tensor_tensor(out=cbias[:], in0=pb[:, 0:2], in1=b1c[:, kv * 2:kv * 2 + 2], op=ALU.add),
                     reads=[bpb, bb1c], writes=[bcbias])
                for g in range(2):
                    for half in range(2):
                        for ci, (n0, nn) in enumerate(((0, 512), (512, 511))):
                            pt, bpt = ph.next()
                            for j in range(32):
                                S.op("pe", lambda e, pt=pt, g=g, half=half, n0=n0, nn=nn, j=j: e.matmul(
                                    pt[:, 0:nn], lhsT=W1[g * 64:(g + 1) * 64, j, half * 128:(half + 1) * 128],
                                    rhs=AP(cT, g * 64 * W + 16 * n0 + j * 16 // 16 * 0 + j, [[W, 64], [16, nn]]),
                                    start=(j == 0), stop=(j == 31)), reads=[bW1, bcT], writes=[bpt])
                            u, bu = ur.next()
                            tt, btt = tr_.next()
                            S.op("act", lambda e, u=u, pt=pt, nn=nn, half=half: e.activation(
                                out=u[:, 0:nn], in_=pt[:, 0:nn], func=AF.Identity, bias=cbias[:, half:half + 1], scale=1.0),
                                reads=[bpt, bcbias], writes=[bu])
                            S.op("dve", lambda e, u=u, tt=tt, nn=nn: e.tensor_tensor(out=tt[:, 0:nn], in0=u[:, 0:nn], in1=u[:, 0:nn], op=ALU.mult),
                                 reads=[bu], writes=[btt])
                            S.op("dve", lambda e, tt=tt, nn=nn: e.tensor_scalar(out=tt[:, 0:nn], in0=tt[:, 0:nn], scalar1=0.044715, scalar2=1.0,
                                                                                op0=ALU.mult, op1=ALU.add), reads=[btt], writes=[btt])
                            S.op("dve", lambda e, u=u, tt=tt, nn=nn: e.tensor_tensor(out=tt[:, 0:nn], in0=tt[:, 0:nn], in1=u[:, 0:nn], op=ALU.mult),
                                 reads=[bu, btt], writes=[btt])
                            S.op("act", lambda e, tt=tt, nn=nn: e.activation(out=tt[:, 0:nn], in_=tt[:, 0:nn], func=AF.Tanh,
                                                                             scale=0.7978845608028654), reads=[btt], writes=[btt])
                            S.op("dve", lambda e, u=u, tt=tt, nn=nn: e.scalar_tensor_tensor(
                                out=tt[:, 0:nn], in0=tt[:, 0:nn], scalar=1.0, in1=u[:, 0:nn], op0=ALU.add, op1=ALU.mult),
                                reads=[bu, btt], writes=[btt])
                            S.op("act", lambda e, tt=tt, nn=nn, half=half, n0=n0: e.activation(
                                out=Hd[:, half, n0:n0 + nn], in_=tt[:, 0:nn], func=AF.Copy, scale=0.5), reads=[btt], writes=[bHd])
                    if kv == 0:
                        for ci, (n0, nn) in enumerate(((0, 512), (512, 511))):
                            pt, bpt = pk.next()
                            for half in range(2):
                                S.op("pe", lambda e, pt=pt, half=half, n0=n0, nn=nn: e.matmul(
                                    pt[0:64, 0:nn], lhsT=W2[:, half, :], rhs=Hd[:, half, n0:n0 + nn], start=(half == 0), stop=(half == 1)),
                                    reads=[bW2, bHd], writes=[bpt])
                            S.op("act", lambda e, pt=pt, g=g, n0=n0, nn=nn: e.copy(out=kcmpT[0:64, g, n0:n0 + nn], in_=pt[0:64, 0:nn]),
                                 reads=[bpt], writes=[bkcmpT])
                            S.op("dve", lambda e, g=g, n0=n0, nn=nn: e.tensor_tensor(
                                out=sqk[:, 0:nn], in0=kcmpT[0:64, g, n0:n0 + nn], in1=kcmpT[0:64, g, n0:n0 + nn], op=ALU.mult),
                                reads=[bkcmpT], writes=[bsqk])
                            pt2, bpt2 = pk.next()
                            S.op("pe", lambda e, pt2=pt2, nn=nn: e.matmul(pt2[0:1, 0:nn], lhsT=onesb[0:64, 0:1], rhs=sqk[:, 0:nn],
                                                                          start=True, stop=True), reads=[bsqk, bonesb], writes=[bpt2])
                            S.op("dve", lambda e, pt2=pt2, nn=nn, g=g, ci=ci: e.tensor_reduce(
                                out=kn[0:1, g * 2 + ci:g * 2 + ci + 1], in_=pt2[0:1, 0:nn], axis=AX.X, op=ALU.max), reads=[bpt2], writes=[bkn])
                    else:
                        for nt in range(8):
                            pt, bpt = pk.next()
                            for half in range(2):
                                S.op("pe", lambda e, pt=pt, half=half, nt=nt: e.matmul(
                                    pt[:, 0:64], lhsT=Hd[:, half, nt * 128:(nt + 1) * 128], rhs=W2[:, half, :], start=(half == 0), stop=(half == 1)),
                                    reads=[bW2, bHd], writes=[bpt])
                            S.op("act", lambda e, pt=pt, g=g, nt=nt: e.copy(out=vcmp[:, nt, g, 0:64], in_=pt[:, 0:64]),
                                 reads=[bpt], writes=[bvcmp])
            S.op("dve", lambda e: e.tensor_reduce(out=kn[0:1, 4:5], in_=kn[0:1, 0:4], axis=AX.X, op=ALU.max), reads=[bkn], writes=[bkn])
            S.op("act", lambda e: e.activation(out=kn[0:1, 4:5], in_=kn[0:1, 4:5], func=AF.Sqrt, scale=1.05), reads=[bkn], writes=[bkn])
            S.op("pe", lambda e: e.matmul(pb[:, 8:9], lhsT=ones[0:1, :], rhs=kn[0:1, 4:5], start=True, stop=True),
                 reads=[bkn, bones], writes=[bpb])
            S.op("dve", lambda e: e.tensor_copy(out=cbias[:, 0:1], in_=pb[:, 8:9]), reads=[bpb], writes=[bcbias])
            S.op("dve", lambda e: e.tensor_scalar(
                out=AP(kcmpT, 64 * 2048, [[2048, 1], [1, 2048]]), in0=AP(ones, 64 * 128, [[128, 1], [0, 2048]]),
                scalar1=cbias[64:65, 0:1], scalar2=None, op0=ALU.mult), reads=[bones, bcbias], writes=[bkcmpT])
            S.flush()
        emat, bemat = sbt(nc, es, "b1emat", [128, 64, 128], BF16)
        dmask, bdmask = sbt(nc, es, "b1dmask", [128, 4, 128], BF16)
        wmask, bwmask = sbt(nc, es, "b1wmask", [128, 8, 128], BF16)
        cmask, bcmask = sbt(nc, es, "b1cmask", [128, 8, 128], BF16)
        cmTb, bcmTb = sbt(nc, es, "b1cmTb", [128, 8, 128], BF16)
        for (tt_, bb_, ap_, b_) in ((emat, bemat, em_ap, em_b), (dmask, bdmask, dm_ap, dm_b), (wmask, bwmask, wm_ap, wm_b),
                                    (cmask, bcmask, cm_ap, cm_b), (cmTb, bcmTb, cb_ap, cb_b)):
            S.dma("sp", lambda e, tt_=tt_, ap_=ap_: e.dma_start(out=tt_[:], in_=ap_), reads=[b_], writes=[bb_])
        ksT, bksT = sbt(nc, es, "b1ksT", [65, W], BF16)
        kwT, bkwT = sbt(nc, es, "b1kwT", [65, W], BF16)
        vsS, bvsS = sbt(nc, es, "b1vs", [128, 128, 65], BF16)
        vwS, bvwS = sbt(nc, es, "b1vw", [128, 128, 65], BF16)
        Qr = sring(nc, es, "b1q", [65, 8, 128], BF16, 2)
        PTr = sring(nc, es, "b1pt", [128, 8, 128], BF16, 3)
        P2r = sring(nc, es, "b1p2", [128, 1024], F32, 2)
        Pn, bPn = sbt(nc, es, "b1pn", [128, 1024], F32)
        imp, bimp = sbt(nc, es, "b1imp", [128, 256], F32)
        imp2, bimp2 = sbt(nc, es, "b1imp2", [128, 256], F32)
        selA, bselA = sbt(nc, es, "b1selA", [128, 256], F32)
        selb, bselb = sbt(nc, es, "b1selb", [128, 256], F32)
        selT, bselT = sbt(nc, es, "b1selT", [128, 2, 128], BF16)
        m8, bm8 = sbt(nc, es, "b1m8", [128, 16], F32)
        l2, bl2 = sbt(nc, es, "b1l2", [128, 16], F32)
        cfr = sring(nc, es, "b1cf", [128, 512], F32, 2)
        obr = [sbt(nc, es, "b1ob%d" % i, [128, 8, 65], F32) for i in range(3)]
        wgt, bwgt = sbt(nc, es, "b1wgt", [128, 3, 8], F32)
        ot1, bot1 = sbt(nc, es, "b1ot1", [128, 8, 64], F32)
        ot2, bot2 = sbt(nc, es, "b1ot2", [128, 8, 64], F32)
        Otr = sring(nc, es, "b1o", [128, 8, 64], BF16, 2)
        STr = pring(nc, es, "b1st", [128, 1024], F32, 2)
        acc, bacc = pst(nc, es, "b1acc", [128, 1024], F32)
        pmisc, _ = pst(nc, es, "b1pmisc", [128, 512], F32)
        pm = Ring([(pmisc[:, 0:128], Buf()), (pmisc[:, 128:256], Buf())])
        psT, bpsT = pmisc[:, 256:512], Buf()
        identf, bidf = cst["identf"]
        S.op("pool", lambda e: e.memset(vsS[:, :, 64:65], 1.0), writes=[bvsS])
        S.op("pool", lambda e: e.memset(vwS[:, :, 64:65], 1.0), writes=[bvwS])
        S.op("dve", lambda e: e.tensor_scalar(out=AP(ksT, 64 * W, [[W, 1], [1, W]]), in0=AP(ones, 64 * 128, [[128, 1], [0, W]]),
                                              scalar1=kmx[64:65, 0:1], scalar2=None, op0=ALU.mult), reads=[bones, bkmx], writes=[bksT])
        S.op("dve", lambda e: e.tensor_scalar(out=AP(kwT, 64 * W, [[W, 1], [1, W]]), in0=AP(ones, 64 * 128, [[128, 1], [0, W]]),
                                              scalar1=kmx[64:65, 1:2], scalar2=None, op0=ALU.mult), reads=[bones, bkmx], writes=[bkwT])

        def attend(Qg, bQ, KT_t, bKT, Vt, bV, kts, maskfn, ob, bob):
            pend = []

            def pv(item):
                (PT, bPT, kt, first) = item
                for h in range(8):
                    S.op("pe", lambda e, h=h: e.matmul(
                        acc[:, (h // 4) * 512 + (h % 4) * 65:(h // 4) * 512 + (h % 4) * 65 + 65], lhsT=PT[:, h, :], rhs=Vt(kt),
                        start=(first and h % 4 == 0), stop=False, skip_group_check=True), reads=[bPT, bV], writes=[bacc])

            for idx, kt in enumerate(kts):
                ST, bST = STr.next()
                mks = maskfn(idx, kt)
                for hh in range(2):
                    S.op("pe", lambda e, ST=ST, kt=kt, hh=hh, mks=mks: e.matmul(
                        ST[:, hh * 512:(hh + 1) * 512], lhsT=KT_t[0:65, kt * 128:(kt + 1) * 128], rhs=Qg[0:65, hh * 4:(hh + 1) * 4, :],
                        start=True, stop=(len(mks) == 0)), reads=[bKT, bQ], writes=[bST])
                    for mi, (ml, mr, mb) in enumerate(mks):
                        S.op("pe", lambda e, ST=ST, hh=hh, ml=ml, mr=mr, mi=mi, mks=mks: e.matmul(
                            ST[:, hh * 512:(hh + 1) * 512], lhsT=ml, rhs=mr, start=False, stop=(mi == len(mks) - 1)),
                            reads=mb, writes=[bST])
                PT, bPT = PTr.next()
                S.op("act", lambda e, ST=ST, PT=PT: e.activation(out=PT[:], in_=ST[:], func=AF.Exp), reads=[bST], writes=[bPT])
                pend.append((PT, bPT, kt, idx == 0))
                if len(pend) > 1:
                    pv(pend.pop(0))
            while pend:
                pv(pend.pop(0))
            S.op("dve", lambda e: e.tensor_copy(out=ob[:, 0:4, :], in_=acc[:, 0:260]), reads=[bacc], writes=[bob])
            S.op("dve", lambda e: e.tensor_copy(out=ob[:, 4:8, :], in_=acc[:, 512:772]), reads=[bacc], writes=[bob])

        def bc4(t_, off, pstride):
            return AP(t_, off, [[pstride, 128], [0, 4], [1, 128]])

        for g in range(2):
            load_global_T(k, ksT, bksT, 64, 0, lambda r, g=g: ks_ap[r, g], ks_b, W)
            load_global_T(k, kwT, bkwT, 64, 0, lambda r, g=g: kw_ap[r, g], kw_b, W)
            for r in range(4):
                S.dma("sp", lambda e, r=r, g=g: e.dma_start(
                    out=AP(vsS, r * 65, [[128 * 65, 128], [4 * 65, NT], [1, 64]]),
                    in_=vs_ap[r, :, g * 64:(g + 1) * 64].rearrange("(i p) d -> p i d", p=128)), reads=[vs_b], writes=[bvsS])
                S.dma("sp", lambda e, r=r, g=g: e.dma_start(
                    out=AP(vwS, r * 65, [[128 * 65, 128], [4 * 65, NT], [1, 64]]),
                    in_=vw_ap[r, :, g * 64:(g + 1) * 64].rearrange("(i p) d -> p i d", p=128)), reads=[vw_b], writes=[bvwS])
            for i in range(NT):
                Qg, bQ = Qr.next()
                S.dma("sp", lambda e, Qg=Qg, g=g, i=i: e.dma_start(
                    out=Qg[:, :, :], in_=QT_ap[8 * g:8 * g + 8, :, i * 128:(i + 1) * 128].rearrange("h r q -> r h q")),
                    reads=[QT_b], writes=[bQ])
                cf, bcf = cfr.next()
                S.dma("sp", lambda e, cf=cf, i=i: e.dma_start(out=cf[:], in_=cf_ap[i]), reads=[cf_b], writes=[bcf])
                nts = i // 4 + 1
                ncol = nts * 128
                attend(Qg, bQ, AP(kcmpT, g * 1024, [[2048, 65], [1, 1024]]), bkcmpT, lambda kt, g=g: vcmp[:, kt, g, :], bvcmp,
                       list(range(nts)),
                       lambda idx, kt, i=i, nts=nts: ([(ident[:], bc4(cmask, ((i % 4) * 2 + (nts - 1 - kt)) * 128, 1024), [bid, bcmask])]
                                                      if kt >= nts - 2 else []),
                       obr[0][0], obr[0][1])
                for h in range(8):
                    S2, bS2 = STr.next()
                    for c0 in range(0, ncol, 512):
                        c1 = min(ncol, c0 + 512)
                        lastc = (c1 == ncol)
                        S.op("pe", lambda e, S2=S2, h=h, c0=c0, c1=c1, g=g, lastc=lastc: e.matmul(
                            S2[:, c0:c1], lhsT=Qg[0:65, h, :], rhs=kcmpT[0:65, g, c0:c1], start=True, stop=not lastc),
                            reads=[bQ, bkcmpT], writes=[bS2])
                    S.op("pe", lambda e, S2=S2, i=i, ncol=ncol: e.matmul(
                        S2[:, ncol - 128:ncol], lhsT=ident[:], rhs=cmTb[:, (i % 4) * 2, :], start=False, stop=True),
                        reads=[bid, bcmTb], writes=[bS2])
                    if nts >= 2:
                        S.op("pe", lambda e, S2=S2, i=i, ncol=ncol: e.matmul(
                            S2[:, ncol - 256:ncol - 128], lhsT=ident[:], rhs=cmTb[:, (i % 4) * 2 + 1, :], start=False, stop=True),
                            reads=[bid, bcmTb], writes=[bS2])
                    P2, bP2 = P2r.next()
                    S.op("act", lambda e, S2=S2, P2=P2, ncol=ncol, h=h: e.activation(
                        out=P2[:, 0:ncol], in_=S2[:, 0:ncol], func=AF.Exp, accum_out=l2[:, h:h + 1]), reads=[bS2], writes=[bP2, bl2])
                    S.op("dve", lambda e, h=h: e.tensor_scalar(out=l2[:, 8 + h:9 + h], in0=l2[:, h:h + 1], scalar1=1e-30, scalar2=None,
                                                               op0=ALU.max), reads=[bl2], writes=[bl2])
                    S.op("dve", lambda e, h=h: e.reciprocal(out=l2[:, 8 + h:9 + h], in_=l2[:, 8 + h:9 + h]), reads=[bl2], writes=[bl2])
                    if h == 0:
                        S.op("dve", lambda e, P2=P2, ncol=ncol, h=h: e.tensor_scalar(
                            out=Pn[:, 0:ncol], in0=P2[:, 0:ncol], scalar1=l2[:, 8 + h:9 + h], scalar2=None, op0=ALU.mult),
                            reads=[bP2, bl2], writes=[bPn])
                    else:
                        S.op("dve", lambda e, P2=P2, ncol=ncol, h=h: e.scalar_tensor_tensor(
                            out=Pn[:, 0:ncol], in0=P2[:, 0:ncol], scalar=l2[:, 8 + h:9 + h], in1=Pn[:, 0:ncol], op0=ALU.mult, op1=ALU.add),
                            reads=[bP2, bl2, bPn], writes=[bPn])
                ns = ncol // 4
                S.op("dve", lambda e: e.memset(imp[:], 0.0), writes=[bimp])
                S.op("dve", lambda e, ns=ns: e.tensor_reduce(out=imp[:, 0:ns], in_=AP(Pn, 0, [[1024, 128], [4, ns], [1, 4]]), axis=AX.X, op=ALU.add),
                     reads=[bPn], writes=[bimp])
                S.op("dve", lambda e, ns=ns: e.tensor_tensor(out=imp[:, 1:ns], in0=imp[:, 1:ns], in1=AP(Pn, 3, [[1024, 128], [4, ns - 1]]), op=ALU.add),
                     reads=[bPn, bimp], writes=[bimp])
                S.op("dve", lambda e, cf=cf: e.tensor_tensor(out=imp[:], in0=imp[:], in1=cf[:, 0:256], op=ALU.mult), reads=[bimp, bcf], writes=[bimp])
                S.op("dve", lambda e, cf=cf: e.tensor_tensor(out=imp[:], in0=imp[:], in1=cf[:, 256:512], op=ALU.add), reads=[bimp, bcf], writes=[bimp])
                S.op("dve", lambda e: e.max(out=m8[:, 0:8], in_=imp[:]), reads=[bimp], writes=[bm8])
                S.op("dve", lambda e: e.match_replace(out=imp2[:], in_to_replace=m8[:, 0:8], in_values=imp[:], imm_value=-30000.0),
                     reads=[bimp, bm8], writes=[bimp2])
                S.op("dve", lambda e: e.max(out=m8[:, 8:16], in_=imp2[:]), reads=[bimp2], writes=[bm8])
                S.op("dve", lambda e: e.tensor_scalar(out=selA[:], in0=imp[:], scalar1=m8[:, 15:16], scalar2=None, op0=ALU.is_ge),
                     reads=[bimp, bm8], writes=[bselA])
                S.op("dve", lambda e: e.tensor_scalar(out=imp2[:], in0=imp[:], scalar1=-5000.0, scalar2=None, op0=ALU.is_gt),
                     reads=[bimp], writes=[bimp2])
                S.op("dve", lambda e: e.tensor_tensor(out=selb[:], in0=selA[:], in1=imp2[:], op=ALU.mult), reads=[bselA, bimp2], writes=[bselb])
                wk = [4 * (i - 1) + u for u in range(8) if 4 * (i - 1) + u >= 0]
                attend(Qg, bQ, kwT, bkwT, lambda kt: vwS[:, kt, :], bvwS, wk,
                       lambda idx, kt, i=i: [(ident[:], bc4(wmask, (kt - 4 * (i - 1)) * 128, 1024), [bid, bwmask])],
                       obr[2][0], obr[2][1])
                for c in range(2):
                    S.op("pe", lambda e, c=c: e.transpose(out=psT[:, c * 128:(c + 1) * 128], in_=selb[:, c * 128:(c + 1) * 128], identity=identf[:]),
                         reads=[bselb, bidf], writes=[bpsT])
                S.op("dve", lambda e: e.tensor_scalar(out=selT[:].rearrange("p a b -> p (a b)"), in0=psT, scalar1=-1.0, scalar2=30000.0,
                                                      op0=ALU.add, op1=ALU.mult), reads=[bpsT], writes=[bselT])

                def selmask(idx, kt, i=i):
                    mks = [(emat[:, kt % 64, :], bc4(selT, (kt // 64) * 128, 256), [bemat, bselT])]
                    if kt >= 4 * i:
                        mks.append((ident[:], bc4(dmask, (kt - 4 * i) * 128, 512), [bid, bdmask]))
                    return mks

                attend(Qg, bQ, ksT, bksT, lambda kt: vsS[:, kt, :], bvsS, list(range(4 * i + 4)), selmask, obr[1][0], obr[1][1])
                for br in range(3):
                    ob, bob = obr[br]
                    S.op("dve", lambda e, ob=ob, br=br: e.tensor_scalar(out=wgt[:, br, :], in0=AP(ob, 64, [[520, 128], [65, 8]]), scalar1=1e-30,
                                                                        scalar2=None, op0=ALU.max), reads=[bob], writes=[bwgt])
                S.op("dve", lambda e: e.reciprocal(out=wgt[:], in_=wgt[:]), reads=[bwgt], writes=[bwgt])
                S.op("dve", lambda e, g=g, i=i: e.tensor_tensor(out=wgt[:], in0=wgt[:],
                                                               in1=AP(gts, i * 48 + g * 24, [[NT * 48, 128], [1, 3], [3, 8]]), op=ALU.mult),
                     reads=[bwgt, bgts], writes=[bwgt])
                for br in range(3):
                    ob, bob = obr[br]
                    dstt, bdstt = (ot1, bot1) if br == 0 else (ot2, bot2)
                    S.op("dve", lambda e, ob=ob, br=br, dstt=dstt: e.tensor_tensor(
                        out=dstt[:], in0=ob[:, :, 0:64], in1=AP(wgt, br * 8, [[24, 128], [1, 8], [0, 64]]), op=ALU.mult),
                        reads=[bob, bwgt], writes=[bdstt])
                    if br > 0:
                        S.op("dve", lambda e: e.tensor_tensor(out=ot1[:], in0=ot1[:], in1=ot2[:], op=ALU.add), reads=[bot1, bot2], writes=[bot1])
                Ot, bOt = Otr.next()
                S.op("act", lambda e, Ot=Ot: e.copy(out=Ot[:], in_=ot1[:]), reads=[bot1], writes=[bOt])
                S.dma("pool", lambda e, Ot=Ot, g=g, i=i: e.dma_start(
                    out=O_ap[i * 128:(i + 1) * 128, g * 512:(g + 1) * 512].rearrange("p (h d) -> p h d", h=8), in_=Ot[:]),
                    reads=[bOt], writes=[O_b])
        S.flush()


def make_nsa_masks(j):
    kk = np.arange(128)[:, None]
    qq = np.arange(128)[None, :]
    dm = np.zeros((128, 4, 128), np.float32)
    for u in range(4):
        if u < j:
            dm[:, u, :] = 1.0
        elif u == j:
            dm[:, u, :] = (kk <= qq)
    wm = np.zeros((128, 8, 128), np.float32)
    for u in range(8):
        off = u - 4 - j
        if off == -4:
            wm[:, u, :] = (kk > qq)
        elif -4 < off < 0:
            wm[:, u, :] = 1.0
        elif off == 0:
            wm[:, u, :] = (kk <= qq)
    cm = np.zeros((128, 8, 128), np.float32)
    for m in range(4):
        for w in range(2):
            nloc = kk - 128 * w
            cm[:, m * 2 + w, :] = (16 * nloc + 31 <= 128 * (4 * m + j) + qq)
    cmT = np.where(cm.transpose(2, 1, 0) > 0.5, 0.0, NEG)
    cf = np.zeros((NT, 128, 512), np.float32)
    ss = np.arange(256)[None, :]
    for i in range(NT):
        gq = 4 * i + j
        qblk = 2 * gq + (np.arange(128)[:, None] >= 64)
        forced = (ss == 0) | (ss == qblk) | (ss == qblk - 1)
        causal = ss <= qblk
        cf[i, :, 0:256] = (causal & ~forced)
        cf[i, :, 256:512] = np.where(forced, 1e4, np.where(causal, 0.0, -1e4))
    em = np.zeros((128, 64, 128), np.float32)
    for m in range(64):
        em[2 * m, m, 0:64] = 1.0
        em[2 * m + 1, m, 64:128] = 1.0
    nb = lambda m01: np.where(m01 > 0.5, 0.0, NEG).astype(NPBF)
    return {"dmaskb": nb(dm), "wmaskb": nb(wm), "cmaskb": nb(cm),
            "cmaskTb": np.ascontiguousarray(cmT).astype(NPBF), "cforce": cf, "emat": em.astype(NPBF)}


def declare_A1_outputs(k, kind):
    f = k.dout if kind == "out" else k.dint
    f("QT1", [16, 65, TOK], BF16)
    f("kcT1", [128, TOK], BF16)
    f("vcT1", [128, TOK], BF16)
    f("ksT1", [2, 64, TOK], BF16)
    f("kwT1", [2, 64, TOK], BF16)
    f("vs1", [TOK, 128], BF16)
    f("vw1", [TOK, 128], BF16)
    f("knm1", [128, 2], F32)
    f("gates1", [TOK, 48], F32)


def build_L2():
    nc = bass.Bass("TRN2", target_bir_lowering=False)
    with ExitStack() as es:
        k = K(nc, es)
        declare_common(k, [0, 1])
        k.din("x", [TOK, D], F32)
        k.din("QT0", [16, 65, TOK], BF16)
        k.din("KT0g", [4, 16, 64, TOK], BF16)
        k.din("V0g", [4, 8, 128, NT, 129], BF16)
        k.din("knm0g", [128, 4], F32)
        k.din("lam", [1, 256], F32)
        k.din("subg", [128, 128], F32)
        k.din("dmask", [128, 4, 128], BF16)
        k.din("diff_w_out", [D, D], F32)
        k.din("nsa_w_in", [D, 1840], F32)
        declare_moe(k, 0)
        k.dint("O0", [TOK, D], BF16)
        k.dint("x1s0", [TOK, D], F32)
        k.dout("xm0", [TOK, D], F32)
        declare_A1_outputs(k, "out")
        cst = emit_consts(k, es)
        mod0 = emit_mod(k, es, cst, 0, "m0")
        phase_B0(k, cst)
        phase_C(k, cst, mod0, 0, "O0", "x", "xm0", "diff_w_out", False)
        mod1 = emit_mod(k, es, cst, 1, "m1")
        cs, bcs = emit_rope_tables(k, es, "r")
        phase_A1(k, cst, mod1, cs, bcs, "xm0")
    return nc


def build_L3():
    nc = bass.Bass("TRN2", target_bir_lowering=False)
    with ExitStack() as es:
        k = K(nc, es)
        declare_common(k, [1])
        k.din("xm0", [TOK, D], F32)
        k.din("QT1", [16, 65, TOK], BF16)
        k.din("kcT1g", [4, 128, TOK], BF16)
        k.din("vcT1g", [4, 128, TOK], BF16)
        k.din("ksT1g", [4, 2, 64, TOK], BF16)
        k.din("kwT1g", [4, 2, 64, TOK], BF16)
        k.din("vs1g", [4, TOK, 128], BF16)
        k.din("vw1g", [4, TOK, 128], BF16)
        k.din("knm1g", [128, 8], F32)
        k.din("gates1", [TOK, 48], F32)
        k.din("cmp_w1", [2, 2048, 256], F32)
        k.din("cmp_b1c", [128, 4], F32)
        k.din("cmp_w2", [2, 256, 64], F32)
        k.din("cmp_posT", [2, 64, 32], F32)
        k.din("emat", [128, 64, 128], BF16)
        k.din("dmaskb", [128, 4, 128], BF16)
        k.din("wmaskb", [128, 8, 128], BF16)
        k.din("cmaskb", [128, 8, 128], BF16)
        k.din("cmaskTb", [128, 8, 128], BF16)
        k.din("cforce", [NT, 128, 512], F32)
        k.din("nsa_w_out", [D, D], F32)
        k.din("final_g", [128, D], F32)
        declare_moe(k, 1)
        k.dint("O1", [TOK, D], BF16)
        k.dint("x1s1", [TOK, D], F32)
        k.dout("out", [TOK, D], F32)
        cst = emit_consts(k, es)
        mod1 = emit_mod(k, es, cst, 1, "m1")
        phase_B1(k, cst)
        phase_C(k, cst, mod1, 1, "O1", "xm0", "out", "nsa_w_out", True)
    return nc


_NC_CACHE = {}


def _get(name, fn):
    if name not in _NC_CACHE:
        _NC_CACHE[name] = fn()
    return _NC_CACHE[name]


def kernel(x, c, positions, ada_w, ada_b, norm_g, final_g, diff_w_in, diff_w_out, diff_lambda, diff_subln_g,
           nsa_w_in, nsa_w_out, nsa_cmp_pos, nsa_cmp_w1, nsa_cmp_b1, nsa_cmp_w2,
           moe_w_group, moe_b_group, moe_w_expert, moe_b_expert, moe_w_gate, moe_w_up, moe_w_down):
    A = lambda a: np.ascontiguousarray(np.asarray(a))
    x, c, positions, ada_w, ada_b, norm_g, final_g = map(A, (x, c, positions, ada_w, ada_b, norm_g, final_g))
    moe = tuple(map(A, (moe_w_group, moe_b_group, moe_w_expert, moe_b_expert, moe_w_gate, moe_w_up, moe_w_down)))
    cores = list(range(8))
    maps = []
    for core in cores:
        b, j = core // 4, core % 4
        m = common_inputs(core, x, c, positions, ada_w, ada_b, norm_g, [0])
        m["x"] = shard_rows(x[b], j)
        m["diff_w_in"] = A(diff_w_in)[0]
        maps.append(m)
    r1 = run_bass_kernel_spmd(_get("L1", build_L1), maps, core_ids=cores).results
    maps = []
    for core in cores:
        b, j = core // 4, core % 4
        m = common_inputs(core, x, c, positions, ada_w, ada_b, norm_g, [0, 1])
        m["x"] = shard_rows(x[b], j)
        m["QT0"] = r1[core]["QT0"]
        m["KT0g"] = np.stack([r1[4 * b + r]["KT0"] for r in range(4)])
        m["V0g"] = np.stack([r1[4 * b + r]["V0"] for r in range(4)])
        m["knm0g"] = np.concatenate([r1[4 * b + r]["knm0"] for r in range(4)], axis=1)
        m["lam"] = A(diff_lambda)[0].reshape(1, 256)
        m["subg"] = A(np.broadcast_to(A(diff_subln_g)[0][None, :], (128, 128)))
        m["dmask"] = make_dmask(j)
        m["diff_w_out"] = A(diff_w_out)[0]
        m["nsa_w_in"] = A(nsa_w_in)[0]
        moe_inputs(m, 0, *moe)
        maps.append(m)
    r2 = run_bass_kernel_spmd(_get("L2", build_L2), maps, core_ids=cores).results
    maps = []
    w1 = A(nsa_cmp_w1)[0]
    b1 = A(nsa_cmp_b1)[0]
    for core in cores:
        b, j = core // 4, core % 4
        m = common_inputs(core, x, c, positions, ada_w, ada_b, norm_g, [1])
        m["xm0"] = r2[core]["xm0"]
        m["QT1"] = r2[core]["QT1"]
        for nm in ("kcT1", "vcT1", "ksT1", "kwT1", "vs1", "vw1"):
            m[nm + "g"] = np.stack([r2[4 * b + r][nm] for r in range(4)])
        kn = np.stack([r2[4 * b + r]["knm1"] for r in range(4)], axis=2)
        m["knm1g"] = A(kn.reshape(128, 8))
        m["gates1"] = r2[core]["gates1"]
        m["cmp_w1"] = w1
        m["cmp_b1c"] = A(np.concatenate([col_layout(b1[0]), col_layout(b1[1])], axis=1))
        m["cmp_w2"] = A(nsa_cmp_w2)[0]
        m["cmp_posT"] = A(A(nsa_cmp_pos)[0].transpose(0, 2, 1))
        m.update(make_nsa_masks(j))
        m["nsa_w_out"] = A(nsa_w_out)[0]
        m["final_g"] = A(np.broadcast_to(final_g[None, :], (128, D)))
        moe_inputs(m, 1, *moe)
        maps.append(m)
    r3 = run_bass_kernel_spmd(_get("L3", build_L3), maps, core_ids=cores).results
    out = np.empty((2, SEQ // 128, 128, D), np.float32)
    for core in cores:
        b, j = core // 4, core % 4
        out[b, j::4] = np.asarray(r3[core]["out"]).reshape(NT, 128, D)
    return out.reshape(2, SEQ, D)
```

```python
import math
import numpy as np
import ml_dtypes
from contextlib import ExitStack
import concourse.bass as bass
import concourse.mybir as mybir
from concourse.bass_utils import run_bass_kernel_spmd

F32 = mybir.dt.float32
BF16 = mybir.dt.bfloat16
I32 = mybir.dt.int32
ALU = mybir.AluOpType
AF = mybir.ActivationFunctionType
AX = mybir.AxisListType
NPBF = ml_dtypes.bfloat16

D = 1024
SEQ = 16384
NT = 32
TOK = NT * 128
NEG = -30000.0


class Buf:
    __slots__ = ("w", "r")

    def __init__(self):
        self.w = None
        self.r = {}


class Sched:
    ENG = ("pe", "act", "dve", "pool", "sp")

    def __init__(self, nc, es, n_dma_sems=32):
        self.nc = nc
        self.sems = {}
        self.count = {}
        for e in self.ENG:
            self.sems[e] = es.enter_context(nc.semaphore("s_" + e))
            self.count[e] = 0
        self.dma_sems = []
        for i in range(n_dma_sems):
            k = "d%d" % i
            self.sems[k] = es.enter_context(nc.semaphore("s_" + k))
            self.count[k] = 0
            self.dma_sems.append(k)
        self.dma_rr = 0
        self.dma_rr_sw = 0
        self.n_hw = n_dma_sems - 8
        self.seen = {e: {} for e in self.ENG}
        self.prog = {e: [] for e in self.ENG}

    def _need(self, eng, tok):
        k, v = tok
        if self.seen[eng].get(k, 0) >= v:
            return
        self.seen[eng][k] = v
        self.prog[eng].append(("w", k, v))

    def _deps(self, eng, reads, writes):
        for b in reads:
            if b.w is not None and not (eng == "pe" and b.w[0] == "pe"):
                self._need(eng, b.w)
        for b in writes:
            if b.w is not None and b.w[0] != eng:
                self._need(eng, b.w)
            for k, v in b.r.items():
                if k != eng:
                    self._need(eng, (k, v))

    def _mark(self, tok, reads, writes):
        k, v = tok
        for b in reads:
            if b.r.get(k, 0) < v:
                b.r[k] = v
        for b in writes:
            b.w = tok
            b.r = {}

    def op(self, eng, fn, reads=(), writes=()):
        self._deps(eng, reads, writes)
        self.count[eng] += 1
        tok = (eng, self.count[eng])
        self.prog[eng].append(("i", fn, eng, 1))
        self._mark(tok, reads, writes)
        return tok

    def dma(self, eng, fn, reads=(), writes=()):
        self._deps(eng, reads, writes)
        if eng == "pool":
            k = self.dma_sems[self.n_hw + self.dma_rr_sw]
            self.dma_rr_sw = (self.dma_rr_sw + 1) % (len(self.dma_sems) - self.n_hw)
        else:
            k = self.dma_sems[self.dma_rr]
            self.dma_rr = (self.dma_rr + 1) % self.n_hw
        if self.count[k] > 0:
            self._need(eng, (k, self.count[k]))
        self.count[k] += 16
        tok = (k, self.count[k])
        self.prog[eng].append(("i", fn, k, 16))
        self._mark(tok, reads, writes)
        return tok

    def barrier(self):
        for e in self.ENG:
            for k, c in self.count.items():
                if c > 0 and k != e:
                    self._need(e, (k, c))

    def flush(self):
        self.barrier()
        nc = self.nc
        prog = self.prog
        sems = self.sems

        def run(e, items):
            for it in items:
                if it[0] == "w":
                    e.wait_ge(sems[it[1]], it[2])
                else:
                    it[1](e).then_inc(sems[it[2]], it[3])

        with nc.Block() as block:
            @block.tensor
            def _(e):
                run(e, prog["pe"])

            @block.scalar
            def _(e):
                run(e, prog["act"])

            @block.vector
            def _(e):
                run(e, prog["dve"])

            @block.gpsimd
            def _(e):
                run(e, prog["pool"])

            @block.sync
            def _(e):
                run(e, prog["sp"])
        self.prog = {e: [] for e in self.ENG}


class Ring:
    def __init__(self, items):
        self.items = items
        self.i = 0

    def next(self):
        it = self.items[self.i]
        self.i = (self.i + 1) % len(self.items)
        return it


class K:
    def __init__(self, nc, es):
        self.nc = nc
        self.es = es
        self.S = Sched(nc, es)
        self.dram = {}

    def din(self, name, shape, dt):
        t = self.nc.dram_tensor(name, list(shape), dt, kind="ExternalInput")
        self.dram[name] = (t.ap(), Buf())
        return self.dram[name]

    def dout(self, name, shape, dt):
        t = self.nc.dram_tensor(name, list(shape), dt, kind="ExternalOutput")
        self.dram[name] = (t.ap(), Buf())
        return self.dram[name]

    def dint(self, name, shape, dt):
        t = self.nc.dram_tensor(name, list(shape), dt, kind="Internal")
        self.dram[name] = (t.ap(), Buf())
        return self.dram[name]


_UID = [0]


def _uname(name):
    _UID[0] += 1
    return "%s_%d" % (name, _UID[0])


def sbt(nc, es, name, shape, dt):
    return es.enter_context(nc.sbuf_tensor(_uname(name), list(shape), dt)), Buf()


def pst(nc, es, name, shape, dt):
    return es.enter_context(nc.psum_tensor(_uname(name), list(shape), dt)), Buf()


def sring(nc, es, name, shape, dt, n):
    return Ring([sbt(nc, es, "%s%d" % (name, i), shape, dt) for i in range(n)])


def pring(nc, es, name, shape, dt, n):
    return Ring([pst(nc, es, "%s%d" % (name, i), shape, dt) for i in range(n)])


def AP(t, off, dims):
    return bass.AP(t, off, [list(d) for d in dims])


def emit_consts(k, es):
    nc, S = k.nc, k.S
    c = {}
    identf, bidf = sbt(nc, es, "identf", [128, 128], F32)
    ident, bid = sbt(nc, es, "ident", [128, 128], BF16)
    ones, bones = sbt(nc, es, "ones", [128, 128], F32)
    S.op("pool", lambda e: e.memset(identf[:], 1.0), writes=[bidf])
    S.op("pool", lambda e: e.affine_select(out=identf[:], in_=identf[:], pattern=[[-1, 128]],
                                           compare_op=ALU.is_equal, fill=0.0, base=0, channel_multiplier=1),
         reads=[bidf], writes=[bidf])
    S.op("pool", lambda e: e.tensor_copy(out=ident[:], in_=identf[:]), reads=[bidf], writes=[bid])
    S.op("pool", lambda e: e.memset(ones[:], 1.0), writes=[bones])
    c["identf"] = (identf, bidf)
    c["ident"] = (ident, bid)
    c["ones"] = (ones, bones)
    return c


def emit_mod(k, es, cst, layer, tag):
    nc, S = k.nc, k.S
    c_ap, c_b = k.dram["c"]
    w_ap, w_b = k.dram["ada_w%d" % layer]
    b_ap, b_b = k.dram["ada_b%d" % layer]
    g_ap, g_b = k.dram["ng%d" % layer]
    ones, bones = cst["ones"]
    out = {}
    modcol, bmc = sbt(nc, es, "modcol" + tag, [128, 48], F32)
    gcol, bgc = sbt(nc, es, "gcol" + tag, [128, 16], F32)
    AB, bAB = sbt(nc, es, "AB" + tag, [128, 32], F32)
    gbs = {nm: sbt(nc, es, nm + tag, [128, 1024], F32) for nm in ("g1b", "g2b")}
    with ExitStack() as ps:
        cact, bcact = sbt(nc, ps, "cact" + tag, [128, 8], F32)
        csig, bcsig = sbt(nc, ps, "csig" + tag, [128, 8], F32)
        row, brow = sbt(nc, ps, "modrow" + tag, [1, 6144], F32)
        brow_t, bbrow = sbt(nc, ps, "modb" + tag, [1, 6144], F32)
        wr = sring(nc, ps, "modw" + tag, [128, 8, 512], F32, 2)
        pr = pring(nc, ps, "modp" + tag, [128, 512], F32, 2)
        pcol, bpcol = pst(nc, ps, "modpc" + tag, [128, 512], F32)
        S.dma("sp", lambda e: e.dma_start(out=cact[:], in_=c_ap), reads=[c_b], writes=[bcact])
        S.dma("sp", lambda e: e.dma_start(out=brow_t[:], in_=b_ap), reads=[b_b], writes=[bbrow])
        S.op("act", lambda e: e.activation(out=csig[:], in_=cact[:], func=AF.Sigmoid), reads=[bcact], writes=[bcsig])
        S.op("dve", lambda e: e.tensor_tensor(out=cact[:], in0=cact[:], in1=csig[:], op=ALU.mult),
             reads=[bcact, bcsig], writes=[bcact])
        for nb in range(12):
            wt, bw = wr.next()
            pt, bp = pr.next()
            S.dma("sp", lambda e, wt=wt, nb=nb: e.dma_start(
                out=wt[:], in_=w_ap[:, nb * 512:(nb + 1) * 512].rearrange("(c p) n -> p c n", p=128)),
                reads=[w_b], writes=[bw])
            for kc in range(8):
                S.op("pe", lambda e, pt=pt, wt=wt, kc=kc: e.matmul(
                    pt[0:1, :], lhsT=cact[:, kc:kc + 1], rhs=wt[:, kc, :], start=(kc == 0), stop=(kc == 7)),
                    reads=[bcact, bw], writes=[bp])
            S.op("dve", lambda e, pt=pt, nb=nb: e.tensor_tensor(
                out=row[0:1, nb * 512:(nb + 1) * 512], in0=pt[0:1, :], in1=brow_t[0:1, nb * 512:(nb + 1) * 512],
                op=ALU.add), reads=[bp, bbrow], writes=[brow])
        for kk in range(48):
            S.op("pe", lambda e, kk=kk: e.matmul(pcol[:, kk:kk + 1], lhsT=row[0:1, kk * 128:(kk + 1) * 128],
                                                 rhs=ones[0:1, 0:1], start=True, stop=True),
                 reads=[brow, bones], writes=[bpcol])
        S.op("dve", lambda e: e.tensor_copy(out=modcol[:], in_=pcol[:, 0:48]), reads=[bpcol], writes=[bmc])
        S.dma("sp", lambda e: e.dma_start(out=gcol[:], in_=g_ap), reads=[g_b], writes=[bgc])
        S.op("dve", lambda e: e.scalar_tensor_tensor(out=AB[:, 0:8], in0=modcol[:, 8:16], scalar=1.0, in1=gcol[:, 0:8],
                                                     op0=ALU.add, op1=ALU.mult), reads=[bmc, bgc], writes=[bAB])
        S.op("dve", lambda e: e.tensor_copy(out=AB[:, 8:16], in_=modcol[:, 0:8]), reads=[bmc], writes=[bAB])
        S.op("dve", lambda e: e.scalar_tensor_tensor(out=AB[:, 16:24], in0=modcol[:, 32:40], scalar=1.0, in1=gcol[:, 8:16],
                                                     op0=ALU.add, op1=ALU.mult), reads=[bmc, bgc], writes=[bAB])
        S.op("dve", lambda e: e.tensor_copy(out=AB[:, 24:32], in_=modcol[:, 24:32]), reads=[bmc], writes=[bAB])
        out["AB"] = (AB, bAB)
        for nm, ch in (("g1b", 2), ("g2b", 5)):
            gb, bgb = gbs[nm]
            for hf in range(2):
                pt, bp = pr.next()
                S.op("pe", lambda e, pt=pt, ch=ch, hf=hf: e.matmul(
                    pt[:, :], lhsT=ones[0:1, :], rhs=row[0:1, ch * 1024 + hf * 512: ch * 1024 + (hf + 1) * 512],
                    start=True, stop=True), reads=[bones, brow], writes=[bp])
                S.op("act", lambda e, pt=pt, gb=gb, hf=hf: e.copy(out=gb[:, hf * 512:(hf + 1) * 512], in_=pt[:, :]),
                     reads=[bp], writes=[bgb])
            out[nm] = (gb, bgb)
        S.flush()
    return out


def emit_rope_tables(k, es, tag):
    nc, S = k.nc, k.S
    pos_ap, pos_b = k.dram["pos"]
    inv_ap, inv_b = k.dram["invf"]
    cs, bcs = sbt(nc, es, "ropecs" + tag, [128, NT, 32], F32)
    with ExitStack() as ps:
        posi, bpi = sbt(nc, ps, "posi" + tag, [128, NT], I32)
        posf, bpf = sbt(nc, ps, "posf" + tag, [128, NT], F32)
        invf, binv = sbt(nc, ps, "invf" + tag, [128, 8], F32)
        ang, bang = sbt(nc, ps, "ang" + tag, [128, NT, 8], F32)
        red, bred = sbt(nc, ps, "red" + tag, [128, NT, 16], F32)
        S.dma("sp", lambda e: e.dma_start(out=posi[:], in_=pos_ap), reads=[pos_b], writes=[bpi])
        S.dma("sp", lambda e: e.dma_start(out=invf[:], in_=inv_ap), reads=[inv_b], writes=[binv])
        S.op("dve", lambda e: e.tensor_copy(out=posf[:], in_=posi[:]), reads=[bpi], writes=[bpf])
        S.op("dve", lambda e: e.tensor_tensor(out=ang[:], in0=AP(posf, 0, [[NT, 128], [1, NT], [0, 8]]),
                                              in1=AP(invf, 0, [[8, 128], [0, NT], [1, 8]]), op=ALU.mult),
             reads=[bpf, binv], writes=[bang])
        twopi = 2.0 * math.pi
        ki, bki = sbt(nc, ps, "ropeki" + tag, [128, NT, 16], I32)
        kf, bkf = sbt(nc, ps, "ropekf" + tag, [128, NT, 16], F32)
        S.op("dve", lambda e: e.tensor_scalar(out=red[:, :, 0:8], in0=ang[:], scalar1=0.5 * math.pi, scalar2=None,
                                              op0=ALU.add), reads=[bang], writes=[bred])
        S.op("dve", lambda e: e.tensor_copy(out=red[:, :, 8:16], in_=ang[:]), reads=[bang], writes=[bred])
        S.op("dve", lambda e: e.tensor_scalar(out=kf[:], in0=red[:], scalar1=1.0 / twopi, scalar2=None, op0=ALU.mult),
             reads=[bred], writes=[bkf])
        S.op("dve", lambda e: e.tensor_copy(out=ki[:], in_=kf[:]), reads=[bkf], writes=[bki])
        S.op("dve", lambda e: e.tensor_copy(out=kf[:], in_=ki[:]), reads=[bki], writes=[bkf])
        S.op("dve", lambda e: e.scalar_tensor_tensor(out=red[:], in0=kf[:], scalar=-twopi, in1=red[:],
                                                     op0=ALU.mult, op1=ALU.add), reads=[bkf, bred], writes=[bred])
        S.op("dve", lambda e: e.tensor_scalar(out=kf[:], in0=red[:], scalar1=math.pi, scalar2=-twopi,
                                              op0=ALU.is_gt, op1=ALU.mult), reads=[bred], writes=[bkf])
        S.op("dve", lambda e: e.tensor_tensor(out=red[:], in0=red[:], in1=kf[:], op=ALU.add),
             reads=[bred, bkf], writes=[bred])
        S.op("dve", lambda e: e.tensor_scalar(out=kf[:], in0=red[:], scalar1=-math.pi, scalar2=twopi,
                                              op0=ALU.is_lt, op1=ALU.mult), reads=[bred], writes=[bkf])
        S.op("dve", lambda e: e.tensor_tensor(out=red[:], in0=red[:], in1=kf[:], op=ALU.add),
             reads=[bred, bkf], writes=[bred])
        S.op("dve", lambda e: e.tensor_scalar(out=red[:], in0=red[:], scalar1=-3.1415925, scalar2=3.1415925,
                                              op0=ALU.max, op1=ALU.min), reads=[bred], writes=[bred])
        S.op("act", lambda e: e.activation(out=cs[:, :, 0:16], in_=red[:], func=AF.Sin), reads=[bred], writes=[bcs])
        S.op("dve", lambda e: e.tensor_scalar(out=cs[:, :, 16:32], in0=cs[:, :, 0:16], scalar1=0.125, scalar2=None,
                                              op0=ALU.mult), reads=[bcs], writes=[bcs])
        S.flush()
    return cs, bcs


def emit_norm_T_a(k, cst, xt, bx, tmp):
    S = k.S
    sq, bsq = tmp["sq"].next()
    st, bst = tmp["st"].next()
    xs, bxs = tmp["xs"].next()
    S.op("act", lambda e: e.activation(out=sq[:], in_=xt, func=AF.Square, accum_out=st[:, 0:1]),
         reads=[bx], writes=[bsq, bst])
    S.op("act", lambda e: e.activation(out=st[:, 1:2], in_=st[:, 0:1], func=AF.Sqrt, scale=1.0 / D, bias=1e-6),
         reads=[bst], writes=[bst])
    S.op("dve", lambda e: e.reciprocal(out=st[:, 1:2], in_=st[:, 1:2]), reads=[bst], writes=[bst])
    S.op("act", lambda e: e.activation(out=xs[:], in_=xt, func=AF.Copy, scale=st[:, 1:2]),
         reads=[bx, bst], writes=[bxs])
    return xs, bxs


def emit_norm_T_b(k, cst, xs, bxs, AB, bAB, abcol, hT, bhT, tmp):
    S = k.S
    ident, bid = cst["ident"]
    pT, bpT = tmp["pT"].next()
    for c in range(8):
        S.op("pe", lambda e, c=c: e.transpose(out=pT[:, c, :], in_=xs[:, c * 128:(c + 1) * 128], identity=ident[:]),
             reads=[bxs, bid], writes=[bpT])
    for c in range(8):
        S.op("dve", lambda e, c=c: e.tensor_scalar(out=hT[:, c, :], in0=pT[:, c, :], scalar1=AB[:, abcol + c:abcol + c + 1],
                                                   scalar2=AB[:, abcol + 8 + c:abcol + 9 + c], op0=ALU.mult, op1=ALU.add),
             reads=[bpT, bAB], writes=[bhT])


def emit_norm_T(k, cst, xt, bx, AB, bAB, abcol, hT, bhT, tmp):
    xs, bxs = emit_norm_T_a(k, cst, xt, bx, tmp)
    emit_norm_T_b(k, cst, xs, bxs, AB, bAB, abcol, hT, bhT, tmp)


def norm_tmp(nc, es, tag):
    return {
        "sq": sring(nc, es, "nsq" + tag, [128, 1024], F32, 1),
        "st": sring(nc, es, "nst" + tag, [128, 2], F32, 2),
        "xs": sring(nc, es, "nxs" + tag, [128, 1024], BF16, 2),
        "pT": pring(nc, es, "npT" + tag, [128, 8, 128], BF16, 1),
    }


def emit_rope(k, src, bsrc, dst, bdst, nh, dstw, cs, bcs, t, coff, tmp, btmp):
    S = k.S
    cosb = AP(cs, t * 32 + coff, [[NT * 32, 128], [0, nh], [1, 8]])
    sinb = AP(cs, t * 32 + coff + 8, [[NT * 32, 128], [0, nh], [1, 8]])
    x1 = src(0, 8)
    x2 = src(8, 16)
    S.op("dve", lambda e: e.tensor_tensor(out=tmp[:, 0, 0:nh, :], in0=x1, in1=cosb, op=ALU.mult),
         reads=[bsrc, bcs], writes=[btmp])
    S.op("dve", lambda e: e.tensor_tensor(out=tmp[:, 1, 0:nh, :], in0=x2, in1=sinb, op=ALU.mult),
         reads=[bsrc, bcs], writes=[btmp])
    S.op("dve", lambda e: e.tensor_tensor(out=tmp[:, 2, 0:nh, :], in0=x2, in1=cosb, op=ALU.mult),
         reads=[bsrc, bcs], writes=[btmp])
    S.op("dve", lambda e: e.tensor_tensor(out=tmp[:, 3, 0:nh, :], in0=x1, in1=sinb, op=ALU.mult),
         reads=[bsrc, bcs], writes=[btmp])
    S.op("dve", lambda e: e.tensor_tensor(out=dst(0, 8), in0=tmp[:, 0, 0:nh, :], in1=tmp[:, 1, 0:nh, :], op=ALU.subtract),
         reads=[btmp], writes=[bdst])
    S.op("dve", lambda e: e.tensor_tensor(out=dst(8, 16), in0=tmp[:, 2, 0:nh, :], in1=tmp[:, 3, 0:nh, :], op=ALU.add),
         reads=[btmp], writes=[bdst])


def load_cast_weight(k, w_ap, w_b, wt, bw, ncols, piece=512):
    S = k.S
    for c0 in range(0, ncols, piece):
        c1 = min(ncols, c0 + piece)
        S.dma("pool", lambda e, c0=c0, c1=c1: e.dma_start(
            out=wt[:, :, c0:c1], in_=w_ap[:, c0:c1].rearrange("(c p) n -> p c n", p=128)),
            reads=[w_b], writes=[bw])


def phase_A0(k, cst, mod, cs, bcs):
    nc, S = k.nc, k.S
    ident, bid = cst["ident"]
    AB, bAB = mod["AB"]
    x_ap, x_b = k.dram["x"]
    w_ap, w_b = k.dram["diff_w_in"]
    QT_ap, QT_b = k.dram["QT0"]
    KT_ap, KT_b = k.dram["KT0"]
    V_ap, V_b = k.dram["V0"]
    kn_ap, kn_b = k.dram["knm0"]
    with ExitStack() as es:
        Win, bWin = sbt(nc, es, "a0win", [128, 8, 3072], BF16)
        load_cast_weight(k, w_ap, w_b, Win, bWin, 3072)
        xr = sring(nc, es, "a0x", [128, 1024], F32, 2)
        hr = sring(nc, es, "a0hT", [128, 8, 128], BF16, 2)
        ntmp = norm_tmp(nc, es, "a0")
        ppr = pring(nc, es, "a0pp", [128, 1024], F32, 2)
        pTr = pring(nc, es, "a0pqt", [128, 16, 128], BF16, 1)
        Qsr = sring(nc, es, "a0qs", [128, 16, 65], BF16, 2)
        Ksr = sring(nc, es, "a0ks", [128, 16, 64], BF16, 2)
        Vsr = sring(nc, es, "a0vs", [128, 8, 129], BF16, 2)
        for (Vs_, bVs_) in Vsr.items:
            S.op("pool", lambda e, Vs_=Vs_: e.memset(Vs_[:, :, 128:129], 1.0), writes=[bVs_])
        QTr = sring(nc, es, "a0qts", [128, 16, 128], BF16, 2)
        KTr = sring(nc, es, "a0kts", [128, 16, 128], BF16, 2)
        rtmp, brtmp = sbt(nc, es, "a0rt", [128, 4, 16, 8], F32)
        sqv, bsqv = sbt(nc, es, "a0sqv", [128, 16, 64], F32)
        nrm, bnrm = sbt(nc, es, "a0nrm", [128, 16], F32)
        knm, bknm = sbt(nc, es, "a0knm", [128, 2], F32)
        S.op("dve", lambda e: e.memset(knm[:], 0.0), writes=[bknm])
        hts = {}

        def normT(t):
            xt, bx = xr.next()
            S.dma("sp", lambda e: e.dma_start(out=xt[:], in_=x_ap[t * 128:(t + 1) * 128, :]), reads=[x_b], writes=[bx])
            hT, bhT = hr.next()
            emit_norm_T(k, cst, xt[:], bx, AB, bAB, 0, hT, bhT, ntmp)
            hts[t] = (hT, bhT)

        def mm(t, sec):
            hT, bhT = hts[t]
            pp, bpp = ppr.next()
            for nb in range(2):
                for c in range(8):
                    S.op("pe", lambda e, c=c, nb=nb: e.matmul(
                        pp[:, nb * 512:(nb + 1) * 512], lhsT=hT[:, c, :],
                        rhs=Win[:, c, sec * 1024 + nb * 512: sec * 1024 + (nb + 1) * 512],
                        start=(c == 0), stop=(c == 7)), reads=[bhT, bWin], writes=[bpp])
            return pp, bpp

        def qpost(t, pp, bpp):
            src = lambda d0, d1: AP(pp, d0, [[1024, 128], [64, 16], [1, d1 - d0]])
            Qs, bQs = Qsr.next()
            dst = lambda d0, d1: AP(Qs, d0, [[1040, 128], [65, 16], [1, d1 - d0]])
            emit_rope(k, src, bpp, dst, bQs, 16, 65, cs, bcs, t, 16, rtmp, brtmp)
            S.op("act", lambda e: e.activation(out=dst(16, 64), in_=src(16, 64), func=AF.Copy, scale=0.125), reads=[bpp], writes=[bQs])
            S.op("dve", lambda e: e.tensor_tensor(out=sqv[:], in0=dst(0, 64), in1=dst(0, 64), op=ALU.mult), reads=[bQs], writes=[bsqv])
            S.op("dve", lambda e: e.tensor_reduce(out=nrm[:], in_=sqv[:], axis=AX.X, op=ALU.add), reads=[bsqv], writes=[bnrm])
            S.op("act", lambda e: e.activation(out=nrm[:], in_=nrm[:], func=AF.Sqrt), reads=[bnrm], writes=[bnrm])
            S.op("dve", lambda e: e.tensor_scalar(out=dst(64, 65), in0=AP(nrm, 0, [[16, 128], [1, 16], [1, 1]]),
                                                  scalar1=-1.0, scalar2=None, op0=ALU.mult), reads=[bnrm], writes=[bQs])
            return Qs, bQs

        def qtr(t, Qs, bQs):
            pT, bpT = pTr.next()
            for hc in range(16):
                S.op("pe", lambda e, hc=hc: e.transpose(out=pT[0:65, hc, :], in_=Qs[:, hc, :], identity=ident[:]),
                     reads=[bQs, bid], writes=[bpT])
            QTs, bQTs = QTr.next()
            S.op("act", lambda e: e.copy(out=QTs[0:65, :, :], in_=pT[0:65, :, :]), reads=[bpT], writes=[bQTs])
            S.dma("sp", lambda e: e.dma_start(out=QT_ap[:, :, t * 128:(t + 1) * 128].rearrange("h r q -> r h q"), in_=QTs[0:65, :, :]),
                  reads=[bQTs], writes=[QT_b])

        def kpost(t, pp, bpp):
            src = lambda d0, d1: AP(pp, d0, [[1024, 128], [64, 16], [1, d1 - d0]])
            Ks, bKs = Ksr.next()
            dst = lambda d0, d1: AP(Ks, d0, [[1024, 128], [64, 16], [1, d1 - d0]])
            emit_rope(k, src, bpp, dst, bKs, 16, 64, cs, bcs, t, 0, rtmp, brtmp)
            S.op("act", lambda e: e.copy(out=dst(16, 64), in_=src(16, 64)), reads=[bpp], writes=[bKs])
            S.op("dve", lambda e: e.tensor_tensor(out=sqv[:], in0=dst(0, 64), in1=dst(0, 64), op=ALU.mult), reads=[bKs], writes=[bsqv])
            S.op("dve", lambda e: e.tensor_reduce(out=nrm[:], in_=sqv[:], axis=AX.X, op=ALU.add), reads=[bsqv], writes=[bnrm])
            S.op("dve", lambda e: e.tensor_reduce(out=knm[:, 1:2], in_=nrm[:], axis=AX.X, op=ALU.max), reads=[bnrm], writes=[bknm])
            S.op("dve", lambda e: e.tensor_tensor(out=knm[:, 0:1], in0=knm[:, 0:1], in1=knm[:, 1:2], op=ALU.max), reads=[bknm], writes=[bknm])
            return Ks, bKs

        def ktr(t, Ks, bKs):
            pT, bpT = pTr.next()
            for hc in range(16):
                S.op("pe", lambda e, hc=hc: e.transpose(out=pT[0:64, hc, :], in_=Ks[:, hc, :], identity=ident[:]),
                     reads=[bKs, bid], writes=[bpT])
            KTs, bKTs = KTr.next()
            S.op("act", lambda e: e.copy(out=KTs[0:64, :, :], in_=pT[0:64, :, :]), reads=[bpT], writes=[bKTs])
            S.dma("sp", lambda e: e.dma_start(out=KT_ap[:, :, t * 128:(t + 1) * 128].rearrange("h r q -> r h q"), in_=KTs[0:64, :, :]),
                  reads=[bKTs], writes=[KT_b])

        def vpost(t, pp, bpp):
            Vs, bVs = Vsr.next()
            S.op("act", lambda e: e.copy(out=Vs[:, :, 0:128], in_=AP(pp, 0, [[1024, 128], [128, 8], [1, 128]])), reads=[bpp], writes=[bVs])
            S.dma("sp", lambda e: e.dma_start(out=V_ap[:, :, t, :].rearrange("h p v -> p h v"), in_=Vs[:]), reads=[bVs], writes=[V_b])

        normT(0)
        for t in range(NT):
            ppq = mm(t, 0)
            Qs = qpost(t, *ppq)
            ppk = mm(t, 1)
            Ks = kpost(t, *ppk)
            qtr(t, *Qs)
            if t + 1 < NT:
                normT(t + 1)
            ppv = mm(t, 2)
            vpost(t, *ppv)
            ktr(t, *Ks)
        S.dma("sp", lambda e: e.dma_start(out=kn_ap, in_=knm[:, 0:1]), reads=[bknm], writes=[kn_b])
        S.flush()


def core_tiles(j):
    return [4 * i + j for i in range(NT)]


def shard_rows(a_b, j):
    a = a_b.reshape(SEQ // 128, 128, *a_b.shape[1:])
    return np.ascontiguousarray(a[j::4].reshape(TOK, *a_b.shape[1:]))


def col_layout(v):
    return np.ascontiguousarray(v.reshape(-1, 128).T)


INVF = (500000.0 ** (-np.arange(0, 16, 2, dtype=np.float32) / 16)).astype(np.float32)


def common_inputs(core, x, c, positions, ada_w, ada_b, norm_g, layers):
    b, j = core // 4, core % 4
    m = {
        "c": col_layout(c[b]),
        "pos": np.ascontiguousarray(shard_rows(positions[b], j).reshape(NT, 128).T),
        "invf": np.ascontiguousarray(np.broadcast_to(INVF[None, :], (128, 8))),
    }
    for l in layers:
        m["ada_w%d" % l] = ada_w[l]
        m["ada_b%d" % l] = ada_b[l][None, :]
        m["ng%d" % l] = np.concatenate([col_layout(norm_g[l, 0]), col_layout(norm_g[l, 1])], axis=1)
    return m


def declare_common(k, layers):
    k.din("c", [128, 8], F32)
    k.din("pos", [128, NT], I32)
    k.din("invf", [128, 8], F32)
    for l in layers:
        k.din("ada_w%d" % l, [1024, 6144], F32)
        k.din("ada_b%d" % l, [1, 6144], F32)
        k.din("ng%d" % l, [128, 16], F32)


def build_L1():
    nc = bass.Bass("TRN2", target_bir_lowering=False)
    with ExitStack() as es:
        k = K(nc, es)
        declare_common(k, [0])
        k.din("x", [TOK, D], F32)
        k.din("diff_w_in", [D, 3072], F32)
        k.dout("QT0", [16, 65, TOK], BF16)
        k.dout("KT0", [16, 64, TOK], BF16)
        k.dout("V0", [8, 128, NT, 129], BF16)
        k.dout("knm0", [128, 1], F32)
        cst = emit_consts(k, es)
        mod = emit_mod(k, es, cst, 0, "m0")
        cs, bcs = emit_rope_tables(k, es, "r")
        phase_A0(k, cst, mod, cs, bcs)
    return nc


def emit_kmax(k, es, cst, knm_name, tag):
    nc, S = k.nc, k.S
    kn_ap, kn_b = k.dram[knm_name]
    identf, bidf = cst["identf"]
    ones, bones = cst["ones"]
    kmx, bkmx = sbt(nc, es, "kmx" + tag, [128, 1], F32)
    with ExitStack() as ps:
        a, ba = sbt(nc, ps, "kma" + tag, [128, 4], F32)
        m, bm = sbt(nc, ps, "kmm" + tag, [128, 1], F32)
        r, br = sbt(nc, ps, "kmr" + tag, [1, 128], F32)
        s, bs = sbt(nc, ps, "kms" + tag, [1, 1], F32)
        p1, bp1 = pst(nc, ps, "kmp" + tag, [128, 512], F32)
        S.dma("sp", lambda e: e.dma_start(out=a[:], in_=kn_ap), reads=[kn_b], writes=[ba])
        S.op("dve", lambda e: e.tensor_reduce(out=m[:], in_=a[:], axis=AX.X, op=ALU.max), reads=[ba], writes=[bm])
        S.op("pe", lambda e: e.transpose(out=p1[0:1, 0:128], in_=m[:, 0:1], identity=identf[:]),
             reads=[bm, bidf], writes=[bp1])
        S.op("dve", lambda e: e.tensor_copy(out=r[:], in_=p1[0:1, 0:128]), reads=[bp1], writes=[br])
        S.op("dve", lambda e: e.tensor_reduce(out=s[:], in_=r[:], axis=AX.X, op=ALU.max), reads=[br], writes=[bs])
        S.op("act", lambda e: e.activation(out=s[:], in_=s[:], func=AF.Sqrt, scale=1.02), reads=[bs], writes=[bs])
        S.op("pe", lambda e: e.matmul(p1[:, 256:257], lhsT=ones[0:1, :], rhs=s[0:1, 0:1], start=True, stop=True),
             reads=[bs, bones, bp1], writes=[bp1])
        S.op("dve", lambda e: e.tensor_copy(out=kmx[:], in_=p1[:, 256:257]), reads=[bp1], writes=[bkmx])
        S.flush()
    return kmx, bkmx


def phase_B0(k, cst):
    nc, S = k.nc, k.S
    ident, bid = cst["ident"]
    ones, bones = cst["ones"]
    QT_ap, QT_b = k.dram["QT0"]
    KT_ap, KT_b = k.dram["KT0g"]
    V_ap, V_b = k.dram["V0g"]
    O_ap, O_b = k.dram["O0"]
    lam_ap, lam_b = k.dram["lam"]
    sg_ap, sg_b = k.dram["subg"]
    dm_ap, dm_b = k.dram["dmask"]
    lam_init = 0.8 - 0.6 * math.exp(-0.3 * 0)
    with ExitStack() as es:
        kmx, bkmx = emit_kmax(k, es, cst, "knm0g", "b0")
        nlam, bnlam = sbt(nc, es, "b0nlam", [128, 1], F32)
        gsub, bgsub = sbt(nc, es, "b0gsub", [128, 128], F32)
        dmask, bdmask = sbt(nc, es, "b0dmask", [128, 4, 128], BF16)
        S.dma("sp", lambda e: e.dma_start(out=gsub[:], in_=sg_ap), reads=[sg_b], writes=[bgsub])
        S.dma("sp", lambda e: e.dma_start(out=dmask[:], in_=dm_ap), reads=[dm_b], writes=[bdmask])
        with ExitStack() as ps:
            lt, blt = sbt(nc, ps, "b0lt", [1, 256], F32)
            lp, blp = sbt(nc, ps, "b0lp", [1, 2, 64], F32)
            ls, bls = sbt(nc, ps, "b0ls", [1, 2], F32)
            pl, bpl = pst(nc, ps, "b0pl", [128, 512], F32)
            S.dma("sp", lambda e: e.dma_start(out=lt[:], in_=lam_ap), reads=[lam_b], writes=[blt])
            S.op("dve", lambda e: e.tensor_tensor(out=lp[:], in0=AP(lt, 0, [[256, 1], [128, 2], [1, 64]]),
                                                  in1=AP(lt, 64, [[256, 1], [128, 2], [1, 64]]), op=ALU.mult),
                 reads=[blt], writes=[blp])
            S.op("dve", lambda e: e.tensor_reduce(out=ls[:], in_=lp[:], axis=AX.X, op=ALU.add), reads=[blp], writes=[bls])
            S.op("act", lambda e: e.activation(out=ls[:], in_=ls[:], func=AF.Exp), reads=[bls], writes=[bls])
            S.op("dve", lambda e: e.scalar_tensor_tensor(out=ls[0:1, 0:1], in0=ls[0:1, 1:2], scalar=-lam_init, in1=ls[0:1, 0:1],
                                                         op0=ALU.add, op1=ALU.subtract), reads=[bls], writes=[bls])
            S.op("pe", lambda e: e.matmul(pl[:, 0:1], lhsT=ones[0:1, :], rhs=ls[0:1, 0:1], start=True, stop=True),
                 reads=[bls, bones], writes=[bpl])
            S.op("dve", lambda e: e.tensor_copy(out=nlam[:], in_=pl[:, 0:1]), reads=[bpl], writes=[bnlam])
            S.flush()
        NCH = 16
        Qr = sring(nc, es, "b0q", [65, 2, 512], BF16, 3)
        Kr = sring(nc, es, "b0k", [65, 2, 4, 512], BF16, 5)
        Vr = sring(nc, es, "b0v", [128, 4, 4, 129], BF16, 5)
        Pr = sring(nc, es, "b0p", [128, 512], BF16, 5)
        STr = pring(nc, es, "b0st", [128, 512], F32, 4)
        accs = [pst(nc, es, "b0acc%d" % a, [128, 512], F32) for a in range(4)]
        rl, brl = sbt(nc, es, "b0rl", [128, 4], F32)
        o1r = sring(nc, es, "b0o1", [128, 128], F32, 2)
        o2r = sring(nc, es, "b0o2", [128, 128], F32, 2)
        sqr = sring(nc, es, "b0sq", [128, 128], F32, 2)
        str_ = sring(nc, es, "b0stt", [128, 2], F32, 2)
        Or = sring(nc, es, "b0o", [128, 128], BF16, 3)
        for (Kc, bK) in Kr.items:
            S.op("dve", lambda e, Kc=Kc: e.tensor_scalar(
                out=AP(Kc, 64 * 4096, [[4096, 1], [1, 4096]]),
                in0=AP(ones, 64 * 128, [[128, 1], [0, 4096]]), scalar1=kmx[64:65, 0:1], scalar2=None,
                op0=ALU.mult), reads=[bones, bkmx], writes=[bK])
        LA = 3
        pend = []

        def finish_tile(h, I, a):
            acc, bacc = accs[a]
            o1, bo1 = o1r.next()
            o2, bo2 = o2r.next()
            sq, bsq = sqr.next()
            stt, bstt = str_.next()
            Ot, bOt = Or.next()
            S.op("dve", lambda e: e.reciprocal(out=rl[:, 0:2], in_=AP(acc, 128, [[512, 128], [256, 2]])),
                 reads=[bacc], writes=[brl])
            S.op("dve", lambda e: e.tensor_tensor(out=rl[:, 2:3], in0=rl[:, 1:2], in1=nlam[:], op=ALU.mult),
                 reads=[brl, bnlam], writes=[brl])
            S.op("dve", lambda e: e.tensor_scalar(out=o1[:], in0=acc[:, 256:384], scalar1=rl[:, 2:3], scalar2=None, op0=ALU.mult),
                 reads=[bacc, brl], writes=[bo1])
            S.op("dve", lambda e: e.scalar_tensor_tensor(out=o2[:], in0=acc[:, 0:128], scalar=rl[:, 0:1], in1=o1[:],
                                                         op0=ALU.mult, op1=ALU.add), reads=[bacc, brl, bo1], writes=[bo2])
            S.op("act", lambda e: e.activation(out=sq[:], in_=o2[:], func=AF.Square, accum_out=stt[:, 0:1]),
                 reads=[bo2], writes=[bsq, bstt])
            f2 = (1.0 - lam_init) ** 2
            S.op("act", lambda e: e.activation(out=stt[:, 1:2], in_=stt[:, 0:1], func=AF.Sqrt, scale=1.0 / (128 * f2), bias=1e-6 / f2),
                 reads=[bstt], writes=[bstt])
            S.op("dve", lambda e: e.reciprocal(out=stt[:, 1:2], in_=stt[:, 1:2]), reads=[bstt], writes=[bstt])
            S.op("dve", lambda e: e.scalar_tensor_tensor(out=Ot[:], in0=o2[:], scalar=stt[:, 1:2], in1=gsub[:], op0=ALU.mult, op1=ALU.mult),
                 reads=[bo2, bstt, bgsub], writes=[bOt])
            tl = 4 * I + a
            S.dma("pool", lambda e: e.dma_start(out=O_ap[tl * 128:(tl + 1) * 128, h * 128:(h + 1) * 128], in_=Ot[:]),
                  reads=[bOt], writes=[O_b])

        def emit_pv(item):
            (h, I, kt, comp, a0, PT, bPT, Vc, bV, r, ip, diag, u) = item
            for a in range(a0, 4):
                acc, bacc = accs[a]
                last = (kt == 16 * I + 4 * a + 3)
                S.op("pe", lambda e, acc=acc, a=a, last=last: e.matmul(
                    acc[:, comp * 256: comp * 256 + 129], lhsT=PT[:, a * 128:(a + 1) * 128], rhs=Vc[:, r, ip, :],
                    start=(kt == 0 and comp == 0), stop=last, skip_group_check=True), reads=[bPT, bV], writes=[bacc])
            if diag and u == 3 and comp == 1:
                finish_tile(h, I, a0)

        for h in range(8):
            for I in range(8):
                Qc, bQ = Qr.next()
                S.dma("sp", lambda e, Qc=Qc, h=h, I=I: e.dma_start(
                    out=Qc[:, :, :], in_=QT_ap[2 * h:2 * h + 2, :, I * 512:(I + 1) * 512].rearrange("c r q -> r c q")),
                    reads=[QT_b], writes=[bQ])
                for ch in range(I + 1):
                    Kc, bK = Kr.next()
                    Vc, bV = Vr.next()
                    for r in range(4):
                        for comp in range(2):
                            S.dma("sp", lambda e, Kc=Kc, h=h, ch=ch, r=r, comp=comp: e.dma_start(
                                out=Kc[0:64, comp, r, :], in_=KT_ap[r, 2 * h + comp, :, ch * 512:(ch + 1) * 512]),
                                reads=[KT_b], writes=[bK])
                        S.dma("sp", lambda e, Vc=Vc, h=h, ch=ch, r=r: e.dma_start(
                            out=Vc[:, r, :, :], in_=V_ap[r, h, :, 4 * ch:4 * ch + 4, :]), reads=[V_b], writes=[bV])
                    diag = (ch == I)
                    for kl in range(NCH):
                        kt = 16 * ch + kl
                        ip, r = kl // 4, kl % 4
                        a0 = kl // 4 if diag else 0
                        u = kl % 4
                        for comp in range(2):
                            ST, bST = STr.next()
                            ksl = lambda Kc=Kc, comp=comp, r=r, ip=ip: Kc[:, comp, r, ip * 128:(ip + 1) * 128]
                            if diag:
                                S.op("pe", lambda e, ST=ST, ksl=ksl, Qc=Qc, comp=comp, a0=a0: e.matmul(
                                    ST[:, a0 * 128:(a0 + 1) * 128], lhsT=ksl(), rhs=Qc[:, comp, a0 * 128:(a0 + 1) * 128],
                                    start=True, stop=False), reads=[bK, bQ], writes=[bST])
                                S.op("pe", lambda e, ST=ST, a0=a0, u=u: e.matmul(
                                    ST[:, a0 * 128:(a0 + 1) * 128], lhsT=ident[:], rhs=dmask[:, u, :],
                                    start=False, stop=True), reads=[bid, bdmask], writes=[bST])
                                if a0 < 3:
                                    S.op("pe", lambda e, ST=ST, ksl=ksl, Qc=Qc, comp=comp, a0=a0: e.matmul(
                                        ST[:, (a0 + 1) * 128:512], lhsT=ksl(), rhs=Qc[:, comp, (a0 + 1) * 128:512],
                                        start=True, stop=True), reads=[bK, bQ], writes=[bST])
                            else:
                                S.op("pe", lambda e, ST=ST, ksl=ksl, Qc=Qc, comp=comp: e.matmul(
                                    ST[:, :], lhsT=ksl(), rhs=Qc[:, comp, :], start=True, stop=True),
                                    reads=[bK, bQ], writes=[bST])
                            PT, bPT = Pr.next()
                            S.op("act", lambda e, ST=ST, PT=PT, a0=a0: e.activation(
                                out=PT[:, a0 * 128:512], in_=ST[:, a0 * 128:512], func=AF.Exp), reads=[bST], writes=[bPT])
                            pend.append((h, I, kt, comp, a0, PT, bPT, Vc, bV, r, ip, diag, u))
                            if len(pend) > LA:
                                emit_pv(pend.pop(0))
        while pend:
            emit_pv(pend.pop(0))
        S.flush()


def phase_C(k, cst, mod, layer, o_name, xin_name, xout_name, wout_name, final):
    nc, S = k.nc, k.S
    ident, bid = cst["ident"]
    AB, bAB = mod["AB"]
    g1b, bg1b = mod["g1b"]
    g2b, bg2b = mod["g2b"]
    O_ap, O_b = k.dram[o_name]
    xi_ap, xi_b = k.dram[xin_name]
    xo_ap, xo_b = k.dram[xout_name]
    x1_ap, x1_b = k.dram["x1s%d" % layer]
    wo_ap, wo_b = k.dram[wout_name]
    wr_ap, wr_b = k.dram["moe_wr%d" % layer]
    br_ap, br_b = k.dram["moe_br%d" % layer]
    wg_ap, wg_b = k.dram["moe_wg%d" % layer]
    wu_ap, wu_b = k.dram["moe_wu%d" % layer]
    wd_ap, wd_b = k.dram["moe_wd%d" % layer]
    HT = 16
    with ExitStack() as es:
        yacc, byacc = sbt(nc, es, "c_yacc", [128, HT, 1024], F32)
        byt = [Buf() for _ in range(HT)]
        h2T, _ = sbt(nc, es, "c_h2T", [128, 8, HT * 128], BF16)
        bh2 = [Buf() for _ in range(HT)]
        comb, _ = sbt(nc, es, "c_comb", [128, HT, 32], F32)
        bcomb = [Buf() for _ in range(HT)]
        if final:
            fgb, bfgb = sbt(nc, es, "c_fgb", [128, 1024], F32)
            fg_ap, fg_b = k.dram["final_g"]
            S.dma("sp", lambda e: e.dma_start(out=fgb[:], in_=fg_ap), reads=[fg_b], writes=[bfgb])
        for half in range(2):
            with ExitStack() as p1:
                Wout, bWout = sbt(nc, p1, "c_wout", [128, 8, 1024], BF16)
                load_cast_weight(k, wo_ap, wo_b, Wout, bWout, 1024)
                Wr, bWr = sbt(nc, p1, "c_wr", [128, 8, 36], BF16)
                S.dma("pool", lambda e: e.dma_start(out=Wr[:], in_=wr_ap.rearrange("(c p) n -> p c n", p=128)),
                      reads=[wr_b], writes=[bWr])
                brt, bbrt = sbt(nc, p1, "c_br", [128, 36], F32)
                S.dma("sp", lambda e: e.dma_start(out=brt[:], in_=br_ap), reads=[br_b], writes=[bbrt])
                xr = sring(nc, p1, "c_x", [128, 1024], F32, 2)
                Otr = sring(nc, p1, "c_o", [128, 1024], BF16, 2)
                OTr = sring(nc, p1, "c_oT", [128, 8, 128], BF16, 2)
                x1r = sring(nc, p1, "c_x1", [128, 1024], F32, 2)
                tmpr = sring(nc, p1, "c_tmp", [128, 1024], F32, 1)
                ntmp = norm_tmp(nc, p1, "c")
                ppr = pring(nc, p1, "c_pp", [128, 1024], F32, 2)
                pOT = pring(nc, p1, "c_pOT", [128, 8, 128], BF16, 1)
                plg = pring(nc, p1, "c_plg", [128, 512], F32, 2)
                Lr = sring(nc, p1, "c_L", [128, 36], F32, 2)
                Lmr = sring(nc, p1, "c_Lm", [128, 32], F32, 2)
                smr = sring(nc, p1, "c_sm", [128, 24], F32, 2)
                e1r = sring(nc, p1, "c_e1", [128, 32], F32, 2)
                xss = {}

                def stage1(tl):
                        t = half * HT + tl
                        xt, bx = xr.next()
                        Ot, bOt = Otr.next()
                        S.dma("sp", lambda e, xt=xt, t=t: e.dma_start(out=xt[:], in_=xi_ap[t * 128:(t + 1) * 128, :]),
                              reads=[xi_b], writes=[bx])
                        S.dma("sp", lambda e, Ot=Ot, t=t: e.dma_start(out=Ot[:], in_=O_ap[t * 128:(t + 1) * 128, :]),
                              reads=[O_b], writes=[bOt])
                        pT, bpT = pOT.next()
                        for c in range(8):
                            S.op("pe", lambda e, pT=pT, Ot=Ot, c=c: e.transpose(out=pT[:, c, :], in_=Ot[:, c * 128:(c + 1) * 128],
                                                                              identity=ident[:]), reads=[bOt, bid], writes=[bpT])
                        OT, bOT = OTr.next()
                        S.op("act", lambda e, OT=OT, pT=pT: e.copy(out=OT[:], in_=pT[:]), reads=[bpT], writes=[bOT])
                        pp, bpp = ppr.next()
                        for nb in range(2):
                            for c in range(8):
                                S.op("pe", lambda e, pp=pp, OT=OT, c=c, nb=nb: e.matmul(
                                    pp[:, nb * 512:(nb + 1) * 512], lhsT=OT[:, c, :], rhs=Wout[:, c, nb * 512:(nb + 1) * 512],
                                    start=(c == 0), stop=(c == 7)), reads=[bOT, bWout], writes=[bpp])
                        tmp, btmp = tmpr.next()
                        x1, bx1 = x1r.next()
                        S.op("dve", lambda e, tmp=tmp, pp=pp: e.tensor_tensor(out=tmp[:], in0=pp[:], in1=g1b[:], op=ALU.mult),
                             reads=[bpp, bg1b], writes=[btmp])
                        S.op("dve", lambda e, tmp=tmp, x1=x1, xt=xt: e.tensor_tensor(out=x1[:], in0=tmp[:], in1=xt[:], op=ALU.add),
                             reads=[btmp, bx], writes=[bx1])
                        S.dma("pool", lambda e, x1=x1, t=t: e.dma_start(out=x1_ap[t * 128:(t + 1) * 128, :], in_=x1[:]),
                              reads=[bx1], writes=[x1_b])
                        xss[tl] = emit_norm_T_a(k, cst, x1[:], bx1, ntmp)

                def stage2(tl):
                    xs, bxs = xss[tl]
                    hT = AP(h2T, tl * 128, [[8 * HT * 128, 128], [HT * 128, 8], [1, 128]])
                    emit_norm_T_b(k, cst, xs, bxs, AB, bAB, 16, hT, bh2[tl], ntmp)
                    pl, bpl = plg.next()
                    for c in range(8):
                        S.op("pe", lambda e, pl=pl, tl=tl, c=c: e.matmul(
                            pl[:, 0:36], lhsT=h2T[:, c, tl * 128:(tl + 1) * 128], rhs=Wr[:, c, :],
                            start=(c == 0), stop=(c == 7)), reads=[bh2[tl], bWr], writes=[bpl])
                    L, bL = Lr.next()
                    Lm, bLm = Lmr.next()
                    sm, bsm = smr.next()
                    e1, be1 = e1r.next()
                    S.op("dve", lambda e, L=L, pl=pl: e.tensor_tensor(out=L[:], in0=pl[:, 0:36], in1=brt[:], op=ALU.add),
                         reads=[bpl, bbrt], writes=[bL])
                    S.op("dve", lambda e, L=L, sm=sm: e.tensor_reduce(out=sm[:, 0:1], in_=L[:, 0:4], axis=AX.X, op=ALU.max),
                         reads=[bL], writes=[bsm])
                    S.op("dve", lambda e, sm=sm: e.tensor_scalar(out=sm[:, 1:2], in0=sm[:, 0:1], scalar1=-1.0, scalar2=None,
                                                                 op0=ALU.mult), reads=[bsm], writes=[bsm])
                    S.op("act", lambda e, L=L, sm=sm: e.activation(out=sm[:, 20:24], in_=L[:, 0:4], func=AF.Exp, bias=sm[:, 1:2],
                                                                   scale=1.0, accum_out=sm[:, 2:3]), reads=[bL, bsm], writes=[bsm])
                    S.op("dve", lambda e, sm=sm: e.reciprocal(out=sm[:, 3:4], in_=sm[:, 2:3]), reads=[bsm], writes=[bsm])
                    S.op("dve", lambda e, L=L, sm=sm: e.tensor_scalar(out=sm[:, 4:8], in0=L[:, 0:4], scalar1=sm[:, 0:1], scalar2=None,
                                                                      op0=ALU.is_ge), reads=[bL, bsm], writes=[bsm])
                    S.op("dve", lambda e, sm=sm: e.tensor_scalar(out=sm[:, 4:8], in0=sm[:, 4:8], scalar1=1e30, scalar2=-1e30,
                                                                 op0=ALU.mult, op1=ALU.add), reads=[bsm], writes=[bsm])
                    S.op("dve", lambda e, L=L, Lm=Lm, sm=sm: e.tensor_tensor(
                        out=AP(Lm, 0, [[32, 128], [8, 4], [1, 8]]), in0=AP(L, 4, [[36, 128], [8, 4], [1, 8]]),
                        in1=AP(sm, 4, [[24, 128], [1, 4], [0, 8]]), op=ALU.add), reads=[bL, bsm], writes=[bLm])
                    S.op("dve", lambda e, Lm=Lm, sm=sm: e.max(out=sm[:, 8:16], in_=Lm[:]), reads=[bLm], writes=[bsm])
                    S.op("dve", lambda e, sm=sm: e.tensor_tensor(out=sm[:, 16:17], in0=sm[:, 9:10], in1=sm[:, 8:9], op=ALU.subtract),
                         reads=[bsm], writes=[bsm])
                    S.op("act", lambda e, sm=sm: e.activation(out=sm[:, 17:18], in_=sm[:, 16:17], func=AF.Exp), reads=[bsm], writes=[bsm])
                    S.op("dve", lambda e, sm=sm: e.tensor_scalar(out=sm[:, 18:19], in0=sm[:, 17:18], scalar1=1.0, scalar2=None,
                                                                 op0=ALU.add), reads=[bsm], writes=[bsm])
                    S.op("dve", lambda e, sm=sm: e.reciprocal(out=sm[:, 18:19], in_=sm[:, 18:19]), reads=[bsm], writes=[bsm])
                    S.op("dve", lambda e, sm=sm: e.tensor_tensor(out=sm[:, 18:19], in0=sm[:, 18:19], in1=sm[:, 3:4], op=ALU.mult),
                         reads=[bsm], writes=[bsm])
                    S.op("dve", lambda e, sm=sm: e.tensor_tensor(out=sm[:, 19:20], in0=sm[:, 18:19], in1=sm[:, 17:18], op=ALU.mult),
                         reads=[bsm], writes=[bsm])
                    S.op("dve", lambda e, Lm=Lm, sm=sm, e1=e1: e.tensor_scalar(out=e1[:], in0=Lm[:], scalar1=sm[:, 8:9], scalar2=sm[:, 18:19],
                                                                               op0=ALU.is_equal, op1=ALU.mult), reads=[bLm, bsm], writes=[be1])
                    S.op("dve", lambda e, Lm=Lm, sm=sm, tl=tl: e.tensor_scalar(out=comb[:, tl, :], in0=Lm[:], scalar1=sm[:, 9:10],
                                                                               scalar2=sm[:, 19:20], op0=ALU.is_equal, op1=ALU.mult),
                         reads=[bLm, bsm], writes=[bcomb[tl]])
                    S.op("dve", lambda e, e1=e1, tl=tl: e.tensor_tensor(out=comb[:, tl, :], in0=comb[:, tl, :], in1=e1[:], op=ALU.add),
                         reads=[be1, bcomb[tl]], writes=[bcomb[tl]])

                stage1(0)
                for tl in range(HT):
                    if tl + 1 < HT:
                        stage1(tl + 1)
                    stage2(tl)
                S.flush()
            with ExitStack() as p2:
                Wgr = sring(nc, p2, "c_wgu", [128, 8, 2, 512], BF16, 2)
                Wdr = sring(nc, p2, "c_wd", [128, 2, 2, 1024], BF16, 2)
                gur = pring(nc, p2, "c_gu", [128, 512], F32, 2)
                yr = pring(nc, p2, "c_y", [128, 1024], F32, 2)
                pATr = pring(nc, p2, "c_pAT", [128, 2, 128], BF16, 2)
                sgr = sring(nc, p2, "c_sg", [128, 256], F32, 4)
                Ar = sring(nc, p2, "c_A", [128, 256], BF16, 4)
                ATr = sring(nc, p2, "c_AT", [128, 2, 128], BF16, 4)
                units = []

                def stage_G(u):
                    (ep, tl, e2, Wg, bWg, Wd, bWd) = u["k"]
                    gu, bgu = gur.next()
                    for c in range(8):
                        S.op("pe", lambda e, c=c: e.matmul(
                            gu[:, :], lhsT=h2T[:, c, tl * 128:(tl + 1) * 128], rhs=Wg[:, c, e2, :],
                            start=(c == 0), stop=(c == 7)), reads=[bh2[tl], bWg], writes=[bgu])
                    sg, bsg = sgr.next()
                    A, bA = Ar.next()
                    ex = 2 * ep + e2
                    S.op("act", lambda e: e.activation(out=sg[:], in_=gu[:, 0:256], func=AF.Silu), reads=[bgu], writes=[bsg])
                    S.op("dve", lambda e: e.scalar_tensor_tensor(
                        out=A[:], in0=gu[:, 256:512], scalar=comb[:, tl, ex:ex + 1], in1=sg[:], op0=ALU.mult, op1=ALU.mult),
                        reads=[bgu, bsg, bcomb[tl]], writes=[bA])
                    u["A"] = (A, bA)

                def stage_T(u):
                    A, bA = u["A"]
                    pAT, bpAT = pATr.next()
                    for hh in range(2):
                        S.op("pe", lambda e, hh=hh: e.transpose(out=pAT[:, hh, :], in_=A[:, hh * 128:(hh + 1) * 128], identity=ident[:]),
                             reads=[bA, bid], writes=[bpAT])
                    AT, bAT = ATr.next()
                    S.op("act", lambda e: e.copy(out=AT[:], in_=pAT[:]), reads=[bpAT], writes=[bAT])
                    u["AT"] = (AT, bAT)

                ycur = {}

                def stage_D(u):
                    (ep, tl, e2, Wg, bWg, Wd, bWd) = u["k"]
                    AT, bAT = u["AT"]
                    if e2 == 0:
                        ycur[tl] = yr.next()
                    y, by = ycur[tl]
                    for nb in range(2):
                        for hh in range(2):
                            S.op("pe", lambda e, nb=nb, hh=hh: e.matmul(
                                y[:, nb * 512:(nb + 1) * 512], lhsT=AT[:, hh, :], rhs=Wd[:, e2, hh, nb * 512:(nb + 1) * 512],
                                start=(e2 == 0 and hh == 0), stop=(e2 == 1 and hh == 1)), reads=[bAT, bWd], writes=[by])
                    if e2 == 1:
                        if ep == 0:
                            S.op("dve", lambda e: e.tensor_copy(out=yacc[:, tl, :], in_=y[:]), reads=[by], writes=[byt[tl]])
                        else:
                            S.op("dve", lambda e: e.tensor_tensor(out=yacc[:, tl, :], in0=yacc[:, tl, :], in1=y[:], op=ALU.add),
                                 reads=[by, byt[tl]], writes=[byt[tl]])

                for ep in range(16):
                    Wg, bWg = Wgr.next()
                    Wd, bWd = Wdr.next()
                    for e2 in range(2):
                        ex = 2 * ep + e2
                        S.dma("pool", lambda e, Wg=Wg, ex=ex, e2=e2: e.dma_start(
                            out=Wg[:, :, e2, 0:256], in_=wg_ap[ex].rearrange("(c p) n -> p c n", p=128)),
                            reads=[wg_b], writes=[bWg])
                        S.dma("pool", lambda e, Wg=Wg, ex=ex, e2=e2: e.dma_start(
                            out=Wg[:, :, e2, 256:512], in_=wu_ap[ex].rearrange("(c p) n -> p c n", p=128)),
                            reads=[wu_b], writes=[bWg])
                        S.dma("pool", lambda e, Wd=Wd, ex=ex, e2=e2: e.dma_start(
                            out=Wd[:, e2, :, :], in_=wd_ap[ex].rearrange("(c p) n -> p c n", p=128)),
                            reads=[wd_b], writes=[bWd])
                    for tl in range(HT):
                        for e2 in range(2):
                            units.append({"k": (ep, tl, e2, Wg, bWg, Wd, bWd)})
                            n = len(units) - 1
                            stage_G(units[n])
                            if n >= 1:
                                stage_T(units[n - 1])
                            if n >= 2:
                                stage_D(units[n - 2])
                n = len(units)
                stage_T(units[n - 1])
                stage_D(units[n - 2])
                stage_D(units[n - 1])
                S.flush()
            with ExitStack() as p3:
                x1r = sring(nc, p3, "c3_x1", [128, 1024], F32, 2)
                tmpr = sring(nc, p3, "c3_tmp", [128, 1024], F32, 2)
                x2r = sring(nc, p3, "c3_x2", [128, 1024], F32, 2)
                sqr = sring(nc, p3, "c3_sq", [128, 1024], F32, 1)
                str_ = sring(nc, p3, "c3_st", [128, 2], F32, 2)
                for tl in range(HT):
                    t = half * HT + tl
                    x1, bx1 = x1r.next()
                    S.dma("sp", lambda e, x1=x1, t=t: e.dma_start(out=x1[:], in_=x1_ap[t * 128:(t + 1) * 128, :]),
                          reads=[x1_b], writes=[bx1])
                    tmp, btmp = tmpr.next()
                    x2, bx2 = x2r.next()
                    S.op("dve", lambda e, tmp=tmp, tl=tl: e.tensor_tensor(out=tmp[:], in0=yacc[:, tl, :], in1=g2b[:], op=ALU.mult),
                         reads=[byt[tl], bg2b], writes=[btmp])
                    S.op("dve", lambda e, tmp=tmp, x1=x1, x2=x2: e.tensor_tensor(out=x2[:], in0=tmp[:], in1=x1[:], op=ALU.add),
                         reads=[btmp, bx1], writes=[bx2])
                    if final:
                        sq, bsq = sqr.next()
                        st, bst = str_.next()
                        S.op("act", lambda e, sq=sq, x2=x2, st=st: e.activation(out=sq[:], in_=x2[:], func=AF.Square, accum_out=st[:, 0:1]),
                             reads=[bx2], writes=[bsq, bst])
                        S.op("act", lambda e, st=st: e.activation(out=st[:, 1:2], in_=st[:, 0:1], func=AF.Sqrt, scale=1.0 / D, bias=1e-6),
                             reads=[bst], writes=[bst])
                        S.op("dve", lambda e, st=st: e.reciprocal(out=st[:, 1:2], in_=st[:, 1:2]), reads=[bst], writes=[bst])
                        S.op("dve", lambda e, x2=x2, st=st, tmp=tmp: e.scalar_tensor_tensor(
                            out=tmp[:], in0=x2[:], scalar=st[:, 1:2], in1=fgb[:], op0=ALU.mult, op1=ALU.mult),
                            reads=[bx2, bst, bfgb], writes=[btmp])
                        S.dma("pool", lambda e, tmp=tmp, t=t: e.dma_start(out=xo_ap[t * 128:(t + 1) * 128, :], in_=tmp[:]),
                              reads=[btmp], writes=[xo_b])
                    else:
                        S.dma("pool", lambda e, x2=x2, t=t: e.dma_start(out=xo_ap[t * 128:(t + 1) * 128, :], in_=x2[:]),
                              reads=[bx2], writes=[xo_b])
                S.flush()


def make_dmask(j):
    m = np.zeros((128, 4, 128), np.float32)
    kk = np.arange(128)[:, None]
    qq = np.arange(128)[None, :]
    for u in range(4):
        if u == j:
            m[:, u, :] = np.where(kk > qq, NEG, 0.0)
        elif u > j:
            m[:, u, :] = NEG
    return m.astype(NPBF)


def declare_moe(k, l):
    k.din("moe_wr%d" % l, [1024, 36], F32)
    k.din("moe_br%d" % l, [128, 36], F32)
    k.din("moe_wg%d" % l, [32, 1024, 256], F32)
    k.din("moe_wu%d" % l, [32, 1024, 256], F32)
    k.din("moe_wd%d" % l, [32, 256, 1024], F32)


def moe_inputs(m, l, moe_w_group, moe_b_group, moe_w_expert, moe_b_expert, moe_w_gate, moe_w_up, moe_w_down):
    wr = np.concatenate([moe_w_group[l]] + [moe_w_expert[l, g] for g in range(4)], axis=1)
    br = np.concatenate([moe_b_group[l]] + [moe_b_expert[l, g] for g in range(4)], axis=0)
    m["moe_wr%d" % l] = np.ascontiguousarray(wr)
    m["moe_br%d" % l] = np.ascontiguousarray(np.broadcast_to(br[None, :], (128, 36)))
    m["moe_wg%d" % l] = moe_w_gate[l]
    m["moe_wu%d" % l] = moe_w_up[l]
    m["moe_wd%d" % l] = moe_w_down[l]


def build_L2(with_A1=False):
    nc = bass.Bass("TRN2", target_bir_lowering=False)
    with ExitStack() as es:
        k = K(nc, es)
        declare_common(k, [0, 1] if with_A1 else [0])
        k.din("x", [TOK, D], F32)
        k.din("QT0", [16, 65, TOK], BF16)
        k.din("KT0g", [4, 16, 64, TOK], BF16)
        k.din("V0g", [4, 8, 128, NT, 129], BF16)
        k.din("knm0g", [128, 4], F32)
        k.din("lam", [1, 256], F32)
        k.din("subg", [128, 128], F32)
        k.din("dmask", [128, 4, 128], BF16)
        k.din("diff_w_out", [D, D], F32)
        declare_moe(k, 0)
        k.dint("O0", [TOK, D], BF16)
        k.dint("x1s0", [TOK, D], F32)
        k.dout("xm0", [TOK, D], F32)
        cst = emit_consts(k, es)
        mod = emit_mod(k, es, cst, 0, "m0")
        phase_B0(k, cst)
        phase_C(k, cst, mod, 0, "O0", "x", "xm0", "diff_w_out", False)
    return nc


def phase_A1(k, cst, mod, cs, bcs, xin_name):
    nc, S = k.nc, k.S
    ident, bid = cst["ident"]
    AB, bAB = mod["AB"]
    x_ap, x_b = k.dram[xin_name]
    w_ap, w_b = k.dram["nsa_w_in"]
    QT_ap, QT_b = k.dram["QT1"]
    kc_ap, kc_b = k.dram["kcT1"]
    vc_ap, vc_b = k.dram["vcT1"]
    ks_ap, ks_b = k.dram["ksT1"]
    kw_ap, kw_b = k.dram["kwT1"]
    vs_ap, vs_b = k.dram["vs1"]
    vw_ap, vw_b = k.dram["vw1"]
    kn_ap, kn_b = k.dram["knm1"]
    gt_ap, gt_b = k.dram["gates1"]
    with ExitStack() as es:
        Win, bWin = sbt(nc, es, "a1win", [128, 8, 1840], BF16)
        load_cast_weight(k, w_ap, w_b, Win, bWin, 1840, piece=368)
        xr = sring(nc, es, "a1x", [128, 1024], F32, 2)
        hr = sring(nc, es, "a1hT", [128, 8, 128], BF16, 2)
        ntmp = norm_tmp(nc, es, "a1")
        ppr = pring(nc, es, "a1pp", [128, 1024], F32, 2)
        pTr = pring(nc, es, "a1pqt", [128, 16, 128], BF16, 1)
        Qsr = sring(nc, es, "a1qs", [128, 16, 65], BF16, 2)
        Rr = sring(nc, es, "a1r", [128, 8, 64], BF16, 2)
        Cr = sring(nc, es, "a1c", [128, 256], BF16, 2)
        R32r = sring(nc, es, "a1r32", [128, 512], F32, 2)
        QTr = sring(nc, es, "a1qts", [128, 16, 128], BF16, 2)
        KTr = sring(nc, es, "a1kts", [128, 6, 128], BF16, 2)
        Gr = sring(nc, es, "a1g", [128, 48], F32, 2)
        rtmp, brtmp = sbt(nc, es, "a1rt", [128, 4, 16, 8], F32)
        sqv, bsqv = sbt(nc, es, "a1sqv", [128, 16, 64], F32)
        nrm, bnrm = sbt(nc, es, "a1nrm", [128, 16], F32)
        knm, bknm = sbt(nc, es, "a1knm", [128, 4], F32)
        S.op("dve", lambda e: e.memset(knm[:], 0.0), writes=[bknm])
        hts = {}

        def normT(t):
            xt, bx = xr.next()
            S.dma("sp", lambda e: e.dma_start(out=xt[:], in_=x_ap[t * 128:(t + 1) * 128, :]), reads=[x_b], writes=[bx])
            hT, bhT = hr.next()
            emit_norm_T(k, cst, xt[:], bx, AB, bAB, 0, hT, bhT, ntmp)
            hts[t] = (hT, bhT)

        def qmm(t):
            hT, bhT = hts[t]
            pp, bpp = ppr.next()
            for nb in range(2):
                for c in range(8):
                    S.op("pe", lambda e, c=c, nb=nb: e.matmul(
                        pp[:, nb * 512:(nb + 1) * 512], lhsT=hT[:, c, :], rhs=Win[:, c, nb * 512:(nb + 1) * 512],
                        start=(c == 0), stop=(c == 7)), reads=[bhT, bWin], writes=[bpp])
            return pp, bpp

        def qpost(t, pp, bpp):
            src = lambda d0, d1: AP(pp, d0, [[1024, 128], [64, 16], [1, d1 - d0]])
            Qs, bQs = Qsr.next()
            dst = lambda d0, d1: AP(Qs, d0, [[1040, 128], [65, 16], [1, d1 - d0]])
            emit_rope(k, src, bpp, dst, bQs, 16, 65, cs, bcs, t, 16, rtmp, brtmp)
            S.op("act", lambda e: e.activation(out=dst(16, 64), in_=src(16, 64), func=AF.Copy, scale=0.125), reads=[bpp], writes=[bQs])
            S.op("dve", lambda e: e.tensor_tensor(out=sqv[:], in0=dst(0, 64), in1=dst(0, 64), op=ALU.mult), reads=[bQs], writes=[bsqv])
            S.op("dve", lambda e: e.tensor_reduce(out=nrm[:], in_=sqv[:], axis=AX.X, op=ALU.add), reads=[bsqv], writes=[bnrm])
            S.op("act", lambda e: e.activation(out=nrm[:], in_=nrm[:], func=AF.Sqrt), reads=[bnrm], writes=[bnrm])
            S.op("dve", lambda e: e.tensor_scalar(out=dst(64, 65), in0=AP(nrm, 0, [[16, 128], [1, 16], [1, 1]]),
                                                  scalar1=-1.0, scalar2=None, op0=ALU.mult), reads=[bnrm], writes=[bQs])
            return Qs, bQs

        def qtr(t, Qs, bQs):
            pT, bpT = pTr.next()
            for hc in range(16):
                S.op("pe", lambda e, hc=hc: e.transpose(out=pT[0:65, hc, :], in_=Qs[:, hc, :], identity=ident[:]),
                     reads=[bQs, bid], writes=[bpT])
            QTs, bQTs = QTr.next()
            S.op("act", lambda e: e.copy(out=QTs[0:65, :, :], in_=pT[0:65, :, :]), reads=[bpT], writes=[bQTs])
            S.dma("sp", lambda e: e.dma_start(out=QT_ap[:, :, t * 128:(t + 1) * 128].rearrange("h r q -> r h q"), in_=QTs[0:65, :, :]),
                  reads=[bQTs], writes=[QT_b])

        def kvmm(t):
            hT, bhT = hts[t]
            pp, bpp = ppr.next()
            for nb, (c0, c1) in enumerate(((1024, 1536), (1536, 1840))):
                for c in range(8):
                    S.op("pe", lambda e, c=c, nb=nb, c0=c0, c1=c1: e.matmul(
                        pp[:, nb * 512: nb * 512 + (c1 - c0)], lhsT=hT[:, c, :], rhs=Win[:, c, c0:c1],
                        start=(c == 0), stop=(c == 7)), reads=[bhT, bWin], writes=[bpp])
            return pp, bpp

        def kvpost(t, pp, bpp):
            Ct, bCt = Cr.next()
            S.op("act", lambda e: e.copy(out=Ct[:], in_=pp[:, 0:256]), reads=[bpp], writes=[bCt])
            R32, bR32 = R32r.next()
            S.op("act", lambda e: e.copy(out=R32[:], in_=pp[:, 256:768]), reads=[bpp], writes=[bR32])
            src = lambda d0, d1: AP(R32, d0, [[512, 128], [64, 8], [1, d1 - d0]])
            Rt, bRt = Rr.next()
            dst = lambda d0, d1: AP(Rt, d0, [[512, 128], [64, 8], [1, d1 - d0]])
            emit_rope(k, src, bR32, dst, bRt, 8, 64, cs, bcs, t, 0, rtmp, brtmp)
            S.op("act", lambda e: e.copy(out=dst(16, 64), in_=src(16, 64)), reads=[bR32], writes=[bRt])
            Gt, bGt = Gr.next()
            S.op("act", lambda e: e.activation(out=Gt[:], in_=pp[:, 768:816], func=AF.Sigmoid), reads=[bpp], writes=[bGt])
            S.dma("pool", lambda e: e.dma_start(out=gt_ap[t * 128:(t + 1) * 128, :], in_=Gt[:]), reads=[bGt], writes=[gt_b])
            S.op("dve", lambda e: e.tensor_tensor(out=sqv[:, 0:8, :], in0=Rt[:], in1=Rt[:], op=ALU.mult), reads=[bRt], writes=[bsqv])
            S.op("dve", lambda e: e.tensor_reduce(out=nrm[:, 0:8], in_=sqv[:, 0:8, :], axis=AX.X, op=ALU.add), reads=[bsqv], writes=[bnrm])
            S.op("dve", lambda e: e.tensor_reduce(out=knm[:, 2:3], in_=nrm[:, 0:2], axis=AX.X, op=ALU.max), reads=[bnrm], writes=[bknm])
            S.op("dve", lambda e: e.tensor_reduce(out=knm[:, 3:4], in_=nrm[:, 4:6], axis=AX.X, op=ALU.max), reads=[bnrm], writes=[bknm])
            S.op("dve", lambda e: e.tensor_tensor(out=knm[:, 0:2], in0=knm[:, 0:2], in1=knm[:, 2:4], op=ALU.max), reads=[bknm], writes=[bknm])
            return Ct, bCt, Rt, bRt

        def kvtr(t, Ct, bCt, Rt, bRt):
            pT, bpT = pTr.next()
            S.op("pe", lambda e: e.transpose(out=pT[:, 0, :], in_=Ct[:, 0:128], identity=ident[:]), reads=[bCt, bid], writes=[bpT])
            S.op("pe", lambda e: e.transpose(out=pT[:, 1, :], in_=Ct[:, 128:256], identity=ident[:]), reads=[bCt, bid], writes=[bpT])
            for ii, hh in enumerate((0, 1, 4, 5)):
                S.op("pe", lambda e, ii=ii, hh=hh: e.transpose(out=pT[0:64, 2 + ii, :], in_=Rt[:, hh, :], identity=ident[:]),
                     reads=[bRt, bid], writes=[bpT])
            KTs, bKTs = KTr.next()
            S.op("act", lambda e: e.copy(out=KTs[:, 0:2, :], in_=pT[:, 0:2, :]), reads=[bpT], writes=[bKTs])
            S.op("act", lambda e: e.copy(out=KTs[0:64, 2:6, :], in_=pT[0:64, 2:6, :]), reads=[bpT], writes=[bKTs])
            sl = slice(t * 128, (t + 1) * 128)
            S.dma("sp", lambda e: e.dma_start(out=kc_ap[:, sl], in_=KTs[:, 0, :]), reads=[bKTs], writes=[kc_b])
            S.dma("sp", lambda e: e.dma_start(out=vc_ap[:, sl], in_=KTs[:, 1, :]), reads=[bKTs], writes=[vc_b])
            S.dma("sp", lambda e: e.dma_start(out=ks_ap[:, :, sl].rearrange("g d q -> d g q"), in_=KTs[0:64, 2:4, :]), reads=[bKTs], writes=[ks_b])
            S.dma("sp", lambda e: e.dma_start(out=kw_ap[:, :, sl].rearrange("g d q -> d g q"), in_=KTs[0:64, 4:6, :]), reads=[bKTs], writes=[kw_b])
            S.dma("pool", lambda e: e.dma_start(out=vs_ap[sl, :].rearrange("p (g d) -> p g d", g=2), in_=Rt[:, 2:4, :]), reads=[bRt], writes=[vs_b])
            S.dma("pool", lambda e: e.dma_start(out=vw_ap[sl, :].rearrange("p (g d) -> p g d", g=2), in_=Rt[:, 6:8, :]), reads=[bRt], writes=[vw_b])

        normT(0)
        for t in range(NT):
            ppq = qmm(t)
            Qs = qpost(t, *ppq)
            ppkv = kvmm(t)
            kv = kvpost(t, *ppkv)
            qtr(t, *Qs)
            if t + 1 < NT:
                normT(t + 1)
            kvtr(t, *kv)
        S.dma("sp", lambda e: e.dma_start(out=kn_ap, in_=knm[:, 0:2]), reads=[bknm], writes=[kn_b])
        S.flush()


def emit_kmax_multi(k, es, cst, knm_name, tag, G):
    nc, S = k.nc, k.S
    kn_ap, kn_b = k.dram[knm_name]
    identf, bidf = cst["identf"]
    ones, bones = cst["ones"]
    kmx, bkmx = sbt(nc, es, "kmx" + tag, [128, G], F32)
    with ExitStack() as ps:
        a, ba = sbt(nc, ps, "kma" + tag, [128, G, 4], F32)
        m, bm = sbt(nc, ps, "kmm" + tag, [128, G], F32)
        r, br = sbt(nc, ps, "kmr" + tag, [1, G, 128], F32)
        s, bs = sbt(nc, ps, "kms" + tag, [1, G], F32)
        p1, bp1 = pst(nc, ps, "kmp" + tag, [128, 512], F32)
        S.dma("sp", lambda e: e.dma_start(out=a[:], in_=kn_ap.rearrange("p (g r) -> p g r", g=G)), reads=[kn_b], writes=[ba])
        S.op("dve", lambda e: e.tensor_reduce(out=m[:], in_=a[:], axis=AX.X, op=ALU.max), reads=[ba], writes=[bm])
        for g in range(G):
            S.op("pe", lambda e, g=g: e.transpose(out=p1[0:1, g * 128:(g + 1) * 128], in_=m[:, g:g + 1], identity=identf[:]),
                 reads=[bm, bidf], writes=[bp1])
        S.op("dve", lambda e: e.tensor_copy(out=r[:], in_=p1[0:1, 0:G * 128]), reads=[bp1], writes=[br])
        S.op("dve", lambda e: e.tensor_reduce(out=s[:], in_=r[:], axis=AX.X, op=ALU.max), reads=[br], writes=[bs])
        S.op("act", lambda e: e.activation(out=s[:], in_=s[:], func=AF.Sqrt, scale=1.02), reads=[bs], writes=[bs])
        S.op("pe", lambda e: e.matmul(p1[:, 384:384 + G], lhsT=ones[0:1, :], rhs=s[0:1, 0:G], start=True, stop=True),
             reads=[bs, bones, bp1], writes=[bp1])
        S.op("dve", lambda e: e.tensor_copy(out=kmx[:], in_=p1[:, 384:384 + G]), reads=[bp1], writes=[bkmx])
        S.flush()
    return kmx, bkmx


def load_global_T(k, dst, bdst, nrows, p0, src_ap, src_b, width):
    S = k.S
    for r in range(4):
        S.dma("sp", lambda e, r=r: e.dma_start(
            out=AP(dst, p0 * width + r * 128, [[width, nrows], [512, NT], [1, 128]]),
            in_=src_ap(r).rearrange("d (i p) -> d i p", p=128)), reads=[src_b], writes=[bdst])


def phase_B1(k, cst):
    nc, S = k.nc, k.S
    ident, bid = cst["ident"]
    ones, bones = cst["ones"]
    QT_ap, QT_b = k.dram["QT1"]
    kc_ap, kc_b = k.dram["kcT1g"]
    vc_ap, vc_b = k.dram["vcT1g"]
    ks_ap, ks_b = k.dram["ksT1g"]
    kw_ap, kw_b = k.dram["kwT1g"]
    vs_ap, vs_b = k.dram["vs1g"]
    vw_ap, vw_b = k.dram["vw1g"]
    gt_ap, gt_b = k.dram["gates1"]
    O_ap, O_b = k.dram["O1"]
    w1_ap, w1_b = k.dram["cmp_w1"]
    b1_ap, b1_b = k.dram["cmp_b1c"]
    w2_ap, w2_b = k.dram["cmp_w2"]
    pos_ap, pos_b = k.dram["cmp_posT"]
    em_ap, em_b = k.dram["emat"]
    dm_ap, dm_b = k.dram["dmaskb"]
    wm_ap, wm_b = k.dram["wmaskb"]
    cm_ap, cm_b = k.dram["cmaskb"]
    cb_ap, cb_b = k.dram["cmaskTb"]
    cf_ap, cf_b = k.dram["cforce"]
    W = SEQ
    with ExitStack() as es:
        kmx, bkmx = emit_kmax_multi(k, es, cst, "knm1g", "b1", 2)
        kcmpT, bkcmpT = sbt(nc, es, "b1kcmpT", [65, 2, 1024], BF16)
        vcmp, bvcmp = sbt(nc, es, "b1vcmp", [128, 8, 2, 65], BF16)
        gts, bgts = sbt(nc, es, "b1gts", [128, NT, 48], F32)
        S.dma("sp", lambda e: e.dma_start(out=gts[:], in_=gt_ap.rearrange("(t p) c -> p t c", p=128)), reads=[gt_b], writes=[bgts])
        S.op("dve", lambda e: e.memset(kcmpT[:], 0.0), writes=[bkcmpT])
        S.op("dve", lambda e: e.memset(vcmp[:], 0.0), writes=[bvcmp])
        S.op("dve", lambda e: e.memset(vcmp[:, :, :, 64:65], 1.0), writes=[bvcmp])
        with ExitStack() as ps:
            cT, bcT = sbt(nc, ps, "b1cT", [128, W], BF16)
            W1, bW1 = sbt(nc, ps, "b1W1", [128, 32, 256], BF16)
            W2, bW2 = sbt(nc, ps, "b1W2", [128, 2, 64], BF16)
            posT, bposT = sbt(nc, ps, "b1posT", [128, 32], BF16)
            b1c, bb1c = sbt(nc, ps, "b1b1c", [128, 4], F32)
            cbias, bcbias = sbt(nc, ps, "b1cbias", [128, 2], F32)
            Hd, bHd = sbt(nc, ps, "b1Hd", [128, 2, 1024], BF16)
            ur = sring(nc, ps, "b1u", [128, 512], F32, 2)
            tr_ = sring(nc, ps, "b1t", [128, 512], F32, 2)
            sqk, bsqk = sbt(nc, ps, "b1sqk", [64, 512], BF16)
            kn, bkn = sbt(nc, ps, "b1kn", [1, 8], F32)
            onesb, bonesb = sbt(nc, ps, "b1onesb", [128, 1], BF16)
            ph = pring(nc, ps, "b1ph", [128, 512], F32, 2)
            pk = pring(nc, ps, "b1pk", [128, 512], F32, 2)
            pb, bpb = pst(nc, ps, "b1pb", [128, 512], F32)
            S.op("dve", lambda e: e.memset(onesb[:], 1.0), writes=[bonesb])
            S.op("dve", lambda e: e.memset(Hd[:], 0.0), writes=[bHd])
            S.op("dve", lambda e: e.memset(kn[:], 0.0), writes=[bkn])
            S.dma("sp", lambda e: e.dma_start(out=b1c[:], in_=b1_ap), reads=[b1_b], writes=[bb1c])
            for kv in range(2):
                src_ap, src_b = (kc_ap, kc_b) if kv == 0 else (vc_ap, vc_b)
                load_global_T(k, cT, bcT, 128, 0, lambda r, src_ap=src_ap: src_ap[r], src_b, W)
                for half in range(2):
                    S.dma("pool", lambda e, kv=kv, half=half: e.dma_start(
                        out=W1[half * 64:(half + 1) * 64, :, :], in_=w1_ap[kv].rearrange("(j d) n -> d j n", d=64)),
                        reads=[w1_b], writes=[bW1])
                    S.dma("pool", lambda e, kv=kv, half=half: e.dma_start(
                        out=posT[half * 64:(half + 1) * 64, :], in_=pos_ap[kv]), reads=[pos_b], writes=[bposT])
                S.dma("pool", lambda e, kv=kv: e.dma_start(out=W2[:], in_=w2_ap[kv].rearrange("(c p) n -> p c n", p=128)),
                      reads=[w2_b], writes=[bW2])
                for half in range(2):
                    for j in range(32):
                        S.op("pe", lambda e, half=half, j=j: e.matmul(
                            pb[:, half:half + 1], lhsT=W1[0:64, j, half * 128:(half + 1) * 128], rhs=posT[0:64, j:j + 1],
                            start=(j == 0 and half == 0), stop=(j == 31), skip_group_check=True), reads=[bW1, bposT], writes=[bpb])
                S.op("dve", lambda e, kv=kv: e.tensor_tensor(out=cbias[:], in0=pb[:, 0:2], in1=b1c[:, kv * 2:kv * 2 + 2], op=ALU.add),
                     reads=[bpb, bb1c], writes=[bcbias])
                for g in range(2):
                    for half in range(2):
                        for ci, (n0, nn) in enumerate(((0, 512), (512, 511))):
                            pt, bpt = ph.next()
                            for j in range(32):
                                S.op("pe", lambda e, pt=pt, g=g, half=half, n0=n0, nn=nn, j=j: e.matmul(
                                    pt[:, 0:nn], lhsT=W1[g * 64:(g + 1) * 64, j, half * 128:(half + 1) * 128],
                                    rhs=AP(cT, g * 64 * W + 16 * n0 + j * 16 // 16 * 0 + j, [[W, 64], [16, nn]]),
                                    start=(j == 0), stop=(j == 31)), reads=[bW1, bcT], writes=[bpt])
                            u, bu = ur.next()
                            tt, btt = tr_.next()
                            S.op("act", lambda e, u=u, pt=pt, nn=nn, half=half: e.activation(
                                out=u[:, 0:nn], in_=pt[:, 0:nn], func=AF.Identity, bias=cbias[:, half:half + 1], scale=1.0),
                                reads=[bpt, bcbias], writes=[bu])
                            S.op("dve", lambda e, u=u, tt=tt, nn=nn: e.tensor_tensor(out=tt[:, 0:nn], in0=u[:, 0:nn], in1=u[:, 0:nn], op=ALU.mult),
                                 reads=[bu], writes=[btt])
                            S.op("dve", lambda e, tt=tt, nn=nn: e.tensor_scalar(out=tt[:, 0:nn], in0=tt[:, 0:nn], scalar1=0.044715, scalar2=1.0,
                                                                                op0=ALU.mult, op1=ALU.add), reads=[btt], writes=[btt])
                            S.op("dve", lambda e, u=u, tt=tt, nn=nn: e.tensor_tensor(out=tt[:, 0:nn], in0=tt[:, 0:nn], in1=u[:, 0:nn], op=ALU.mult),
                                 reads=[bu, btt], writes=[btt])
                            S.op("act", lambda e, tt=tt, nn=nn: e.activation(out=tt[:, 0:nn], in_=tt[:, 0:nn], func=AF.Tanh,
                                                                             scale=0.7978845608028654), reads=[btt], writes=[btt])
                            S.op("dve", lambda e, u=u, tt=tt, nn=nn: e.scalar_tensor_tensor(
                                out=tt[:, 0:nn], in0=tt[:, 0:nn], scalar=1.0, in1=u[:, 0:nn], op0=ALU.add, op1=ALU.mult),
                                reads=[bu, btt], writes=[btt])
                            S.op("act", lambda e, tt=tt, nn=nn, half=half, n0=n0: e.activation(
                                out=Hd[:, half, n0:n0 + nn], in_=tt[:, 0:nn], func=AF.Copy, scale=0.5), reads=[btt], writes=[bHd])
                    if kv == 0:
                        for ci, (n0, nn) in enumerate(((0, 512), (512, 511))):
                            pt, bpt = pk.next()
                            for half in range(2):
                                S.op("pe", lambda e, pt=pt, half=half, n0=n0, nn=nn: e.matmul(
                                    pt[0:64, 0:nn], lhsT=W2[:, half, :], rhs=Hd[:, half, n0:n0 + nn], start=(half == 0), stop=(half == 1)),
                                    reads=[bW2, bHd], writes=[bpt])
                            S.op("act", lambda e, pt=pt, g=g, n0=n0, nn=nn: e.copy(out=kcmpT[0:64, g, n0:n0 + nn], in_=pt[0:64, 0:nn]),
                                 reads=[bpt], writes=[bkcmpT])
                            S.op("dve", lambda e, g=g, n0=n0, nn=nn: e.tensor_tensor(
                                out=sqk[:, 0:nn], in0=kcmpT[0:64, g, n0:n0 + nn], in1=kcmpT[0:64, g, n0:n0 + nn], op=ALU.mult),
                                reads=[bkcmpT], writes=[bsqk])
                            pt2, bpt2 = pk.next()
                            S.op("pe", lambda e, pt2=pt2, nn=nn: e.matmul(pt2[0:1, 0:nn], lhsT=onesb[0:64, 0:1], rhs=sqk[:, 0:nn],
                                                                          start=True, stop=True), reads=[bsqk, bonesb], writes=[bpt2])
                            S.op("dve", lambda e, pt2=pt2, nn=nn, g=g, ci=ci: e.tensor_reduce(
                                out=kn[0:1, g * 2 + ci:g * 2 + ci + 1], in_=pt2[0:1, 0:nn], axis=AX.X, op=ALU.max), reads=[bpt2], writes=[bkn])
                    else:
                        for nt in range(8):
                            pt, bpt = pk.next()
                            for half in range(2):
                                S.op("pe", lambda e, pt=pt, half=half, nt=nt: e.matmul(
                                    pt[:, 0:64], lhsT=Hd[:, half, nt * 128:(nt + 1) * 128], rhs=W2[:, half, :], start=(half == 0), stop=(half == 1)),
                                    reads=[bW2, bHd], writes=[bpt])
                            S.op("act", lambda e, pt=pt, g=g, nt=nt: e.copy(out=vcmp[:, nt, g, 0:64], in_=pt[:, 0:64]),
                                 reads=[bpt], writes=[bvcmp])
            S.op("dve", lambda e: e.tensor_reduce(out=kn[0:1, 4:5], in_=kn[0:1, 0:4], axis=AX.X, op=ALU.max), reads=[bkn], writes=[bkn])
            S.op("act", lambda e: e.activation(out=kn[0:1, 4:5], in_=kn[0:1, 4:5], func=AF.Sqrt, scale=1.05), reads=[bkn], writes=[bkn])
            S.op("pe", lambda e: e.matmul(pb[:, 8:9], lhsT=ones[0:1, :], rhs=kn[0:1, 4:5], start=True, stop=True),
                 reads=[bkn, bones], writes=[bpb])
            S.op("dve", lambda e: e.tensor_copy(out=cbias[:, 0:1], in_=pb[:, 8:9]), reads=[bpb], writes=[bcbias])
            S.op("dve", lambda e: e.tensor_scalar(
                out=AP(kcmpT, 64 * 2048, [[2048, 1], [1, 2048]]), in0=AP(ones, 64 * 128, [[128, 1], [0, 2048]]),
                scalar1=cbias[64:65, 0:1], scalar2=None, op0=ALU.mult), reads=[bones, bcbias], writes=[bkcmpT])
            S.flush()
        emat, bemat = sbt(nc, es, "b1emat", [128, 64, 128], BF16)
        dmask, bdmask = sbt(nc, es, "b1dmask", [128, 4, 128], BF16)
        wmask, bwmask = sbt(nc, es, "b1wmask", [128, 8, 128], BF16)
        cmask, bcmask = sbt(nc, es, "b1cmask", [128, 8, 128], BF16)
        cmTb, bcmTb = sbt(nc, es, "b1cmTb", [128, 8, 128], BF16)
        for (tt_, bb_, ap_, b_) in ((emat, bemat, em_ap, em_b), (dmask, bdmask, dm_ap, dm_b), (wmask, bwmask, wm_ap, wm_b),
                                    (cmask, bcmask, cm_ap, cm_b), (cmTb, bcmTb, cb_ap, cb_b)):
            S.dma("sp", lambda e, tt_=tt_, ap_=ap_: e.dma_start(out=tt_[:], in_=ap_), reads=[b_], writes=[bb_])
        ksT, bksT = sbt(nc, es, "b1ksT", [65, W], BF16)
        kwT, bkwT = sbt(nc, es, "b1kwT", [65, W], BF16)
        vsS, bvsS = sbt(nc, es, "b1vs", [128, 128, 65], BF16)
        vwS, bvwS = sbt(nc, es, "b1vw", [128, 128, 65], BF16)
        Qr = sring(nc, es, "b1q", [65, 8, 128], BF16, 2)
        PTr = sring(nc, es, "b1pt", [128, 8, 128], BF16, 3)
        P2r = sring(nc, es, "b1p2", [128, 1024], F32, 2)
        Pn, bPn = sbt(nc, es, "b1pn", [128, 1024], F32)
        imp, bimp = sbt(nc, es, "b1imp", [128, 256], F32)
        imp2, bimp2 = sbt(nc, es, "b1imp2", [128, 256], F32)
        selA, bselA = sbt(nc, es, "b1selA", [128, 256], F32)
        selb, bselb = sbt(nc, es, "b1selb", [128, 256], F32)
        selT, bselT = sbt(nc, es, "b1selT", [128, 2, 128], BF16)
        m8, bm8 = sbt(nc, es, "b1m8", [128, 16], F32)
        l2, bl2 = sbt(nc, es, "b1l2", [128, 16], F32)
        cfr = sring(nc, es, "b1cf", [128, 512], F32, 2)
        obr = [sbt(nc, es, "b1ob%d" % i, [128, 8, 65], F32) for i in range(3)]
        wgt, bwgt = sbt(nc, es, "b1wgt", [128, 3, 8], F32)
        ot1, bot1 = sbt(nc, es, "b1ot1", [128, 8, 64], F32)
        ot2, bot2 = sbt(nc, es, "b1ot2", [128, 8, 64], F32)
        Otr = sring(nc, es, "b1o", [128, 8, 64], BF16, 2)
        STr = pring(nc, es, "b1st", [128, 1024], F32, 2)
        acc, bacc = pst(nc, es, "b1acc", [128, 1024], F32)
        pmisc, _ = pst(nc, es, "b1pmisc", [128, 512], F32)
        pm = Ring([(pmisc[:, 0:128], Buf()), (pmisc[:, 128:256], Buf())])
        psT, bpsT = pmisc[:, 256:512], Buf()
        identf, bidf = cst["identf"]
        S.op("pool", lambda e: e.memset(vsS[:, :, 64:65], 1.0), writes=[bvsS])
        S.op("pool", lambda e: e.memset(vwS[:, :, 64:65], 1.0), writes=[bvwS])
        S.op("dve", lambda e: e.tensor_scalar(out=AP(ksT, 64 * W, [[W, 1], [1, W]]), in0=AP(ones, 64 * 128, [[128, 1], [0, W]]),
                                              scalar1=kmx[64:65, 0:1], scalar2=None, op0=ALU.mult), reads=[bones, bkmx], writes=[bksT])
        S.op("dve", lambda e: e.tensor_scalar(out=AP(kwT, 64 * W, [[W, 1], [1, W]]), in0=AP(ones, 64 * 128, [[128, 1], [0, W]]),
                                              scalar1=kmx[64:65, 1:2], scalar2=None, op0=ALU.mult), reads=[bones, bkmx], writes=[bkwT])

        def attend(Qg, bQ, KT_t, bKT, Vt, bV, kts, maskfn, ob, bob):
            pend = []

            def pv(item):
                (PT, bPT, kt, first) = item
                for h in range(8):
                    S.op("pe", lambda e, h=h: e.matmul(
                        acc[:, (h // 4) * 512 + (h % 4) * 65:(h // 4) * 512 + (h % 4) * 65 + 65], lhsT=PT[:, h, :], rhs=Vt(kt),
                        start=(first and h % 4 == 0), stop=False, skip_group_check=True), reads=[bPT, bV], writes=[bacc])

            for idx, kt in enumerate(kts):
                ST, bST = STr.next()
                mks = maskfn(idx, kt)
                for hh in range(2):
                    S.op("pe", lambda e, ST=ST, kt=kt, hh=hh, mks=mks: e.matmul(
                        ST[:, hh * 512:(hh + 1) * 512], lhsT=KT_t[0:65, kt * 128:(kt + 1) * 128], rhs=Qg[0:65, hh * 4:(hh + 1) * 4, :],
                        start=True, stop=(len(mks) == 0)), reads=[bKT, bQ], writes=[bST])
                    for mi, (ml, mr, mb) in enumerate(mks):
                        S.op("pe", lambda e, ST=ST, hh=hh, ml=ml, mr=mr, mi=mi, mks=mks: e.matmul(
                            ST[:, hh * 512:(hh + 1) * 512], lhsT=ml, rhs=mr, start=False, stop=(mi == len(mks) - 1)),
                            reads=mb, writes=[bST])
                PT, bPT = PTr.next()
                S.op("act", lambda e, ST=ST, PT=PT: e.activation(out=PT[:], in_=ST[:], func=AF.Exp), reads=[bST], writes=[bPT])
                pend.append((PT, bPT, kt, idx == 0))
                if len(pend) > 1:
                    pv(pend.pop(0))
            while pend:
                pv(pend.pop(0))
            S.op("dve", lambda e: e.tensor_copy(out=ob[:, 0:4, :], in_=acc[:, 0:260]), reads=[bacc], writes=[bob])
            S.op("dve", lambda e: e.tensor_copy(out=ob[:, 4:8, :], in_=acc[:, 512:772]), reads=[bacc], writes=[bob])

        def bc4(t_, off, pstride):
            return AP(t_, off, [[pstride, 128], [0, 4], [1, 128]])

        for g in range(2):
            load_global_T(k, ksT, bksT, 64, 0, lambda r, g=g: ks_ap[r, g], ks_b, W)
            load_global_T(k, kwT, bkwT, 64, 0, lambda r, g=g: kw_ap[r, g], kw_b, W)
            for r in range(4):
                S.dma("sp", lambda e, r=r, g=g: e.dma_start(
                    out=AP(vsS, r * 65, [[128 * 65, 128], [4 * 65, NT], [1, 64]]),
                    in_=vs_ap[r, :, g * 64:(g + 1) * 64].rearrange("(i p) d -> p i d", p=128)), reads=[vs_b], writes=[bvsS])
                S.dma("sp", lambda e, r=r, g=g: e.dma_start(
                    out=AP(vwS, r * 65, [[128 * 65, 128], [4 * 65, NT], [1, 64]]),
                    in_=vw_ap[r, :, g * 64:(g + 1) * 64].rearrange("(i p) d -> p i d", p=128)), reads=[vw_b], writes=[bvwS])
            for i in range(NT):
                Qg, bQ = Qr.next()
                S.dma("sp", lambda e, Qg=Qg, g=g, i=i: e.dma_start(
                    out=Qg[:, :, :], in_=QT_ap[8 * g:8 * g + 8, :, i * 128:(i + 1) * 128].rearrange("h r q -> r h q")),
                    reads=[QT_b], writes=[bQ])
                cf, bcf = cfr.next()
                S.dma("sp", lambda e, cf=cf, i=i: e.dma_start(out=cf[:], in_=cf_ap[i]), reads=[cf_b], writes=[bcf])
                nts = i // 4 + 1
                ncol = nts * 128
                attend(Qg, bQ, AP(kcmpT, g * 1024, [[2048, 65], [1, 1024]]), bkcmpT, lambda kt, g=g: vcmp[:, kt, g, :], bvcmp,
                       list(range(nts)),
                       lambda idx, kt, i=i, nts=nts: ([(ident[:], bc4(cmask, ((i % 4) * 2 + (nts - 1 - kt)) * 128, 1024), [bid, bcmask])]
                                                      if kt >= nts - 2 else []),
                       obr[0][0], obr[0][1])
                for h in range(8):
                    S2, bS2 = STr.next()
                    for c0 in range(0, ncol, 512):
                        c1 = min(ncol, c0 + 512)
                        lastc = (c1 == ncol)
                        S.op("pe", lambda e, S2=S2, h=h, c0=c0, c1=c1, g=g, lastc=lastc: e.matmul(
                            S2[:, c0:c1], lhsT=Qg[0:65, h, :], rhs=kcmpT[0:65, g, c0:c1], start=True, stop=not lastc),
                            reads=[bQ, bkcmpT], writes=[bS2])
                    S.op("pe", lambda e, S2=S2, i=i, ncol=ncol: e.matmul(
                        S2[:, ncol - 128:ncol], lhsT=ident[:], rhs=cmTb[:, (i % 4) * 2, :], start=False, stop=True),
                        reads=[bid, bcmTb], writes=[bS2])
                    if nts >= 2:
                        S.op("pe", lambda e, S2=S2, i=i, ncol=ncol: e.matmul(
                            S2[:, ncol - 256:ncol - 128], lhsT=ident[:], rhs=cmTb[:, (i % 4) * 2 + 1, :], start=False, stop=True),
                            reads=[bid, bcmTb], writes=[bS2])
                    P2, bP2 = P2r.next()
                    S.op("act", lambda e, S2=S2, P2=P2, ncol=ncol, h=h: e.activation(
                        out=P2[:, 0:ncol], in_=S2[:, 0:ncol], func=AF.Exp, accum_out=l2[:, h:h + 1]), reads=[bS2], writes=[bP2, bl2])
                    S.op("dve", lambda e, h=h: e.tensor_scalar(out=l2[:, 8 + h:9 + h], in0=l2[:, h:h + 1], scalar1=1e-30, scalar2=None,
                                                               op0=ALU.max), reads=[bl2], writes=[bl2])
                    S.op("dve", lambda e, h=h: e.reciprocal(out=l2[:, 8 + h:9 + h], in_=l2[:, 8 + h:9 + h]), reads=[bl2], writes=[bl2])
                    if h == 0:
                        S.op("dve", lambda e, P2=P2, ncol=ncol, h=h: e.tensor_scalar(
                            out=Pn[:, 0:ncol], in0=P2[:, 0:ncol], scalar1=l2[:, 8 + h:9 + h], scalar2=None, op0=ALU.mult),
                            reads=[bP2, bl2], writes=[bPn])
                    else:
                        S.op("dve", lambda e, P2=P2, ncol=ncol, h=h: e.scalar_tensor_tensor(
                            out=Pn[:, 0:ncol], in0=P2[:, 0:ncol], scalar=l2[:, 8 + h:9 + h], in1=Pn[:, 0:ncol], op0=ALU.mult, op1=ALU.add),
                            reads=[bP2, bl2, bPn], writes=[bPn])
                ns = ncol // 4
                S.op("dve", lambda e: e.memset(imp[:], 0.0), writes=[bimp])
                S.op("dve", lambda e, ns=ns: e.tensor_reduce(out=imp[:, 0:ns], in_=AP(Pn, 0, [[1024, 128], [4, ns], [1, 4]]), axis=AX.X, op=ALU.add),
                     reads=[bPn], writes=[bimp])
                S.op("dve", lambda e, ns=ns: e.tensor_tensor(out=imp[:, 1:ns], in0=imp[:, 1:ns], in1=AP(Pn, 3, [[1024, 128], [4, ns - 1]]), op=ALU.add),
                     reads=[bPn, bimp], writes=[bimp])
                S.op("dve", lambda e, cf=cf: e.tensor_tensor(out=imp[:], in0=imp[:], in1=cf[:, 0:256], op=ALU.mult), reads=[bimp, bcf], writes=[bimp])
                S.op("dve", lambda e, cf=cf: e.tensor_tensor(out=imp[:], in0=imp[:], in1=cf[:, 256:512], op=ALU.add), reads=[bimp, bcf], writes=[bimp])
                S.op("dve", lambda e: e.max(out=m8[:, 0:8], in_=imp[:]), reads=[bimp], writes=[bm8])
                S.op("dve", lambda e: e.match_replace(out=imp2[:], in_to_replace=m8[:, 0:8], in_values=imp[:], imm_value=-30000.0),
                     reads=[bimp, bm8], writes=[bimp2])
                S.op("dve", lambda e: e.max(out=m8[:, 8:16], in_=imp2[:]), reads=[bimp2], writes=[bm8])
                S.op("dve", lambda e: e.tensor_scalar(out=selA[:], in0=imp[:], scalar1=m8[:, 15:16], scalar2=None, op0=ALU.is_ge),
                     reads=[bimp, bm8], writes=[bselA])
                S.op("dve", lambda e: e.tensor_scalar(out=imp2[:], in0=imp[:], scalar1=-5000.0, scalar2=None, op0=ALU.is_gt),
                     reads=[bimp], writes=[bimp2])
                S.op("dve", lambda e: e.tensor_tensor(out=selb[:], in0=selA[:], in1=imp2[:], op=ALU.mult), reads=[bselA, bimp2], writes=[bselb])
                wk = [4 * (i - 1) + u for u in range(8) if 4 * (i - 1) + u >= 0]
                attend(Qg, bQ, kwT, bkwT, lambda kt: vwS[:, kt, :], bvwS, wk,
                       lambda idx, kt, i=i: [(ident[:], bc4(wmask, (kt - 4 * (i - 1)) * 128, 1024), [bid, bwmask])],
                       obr[2][0], obr[2][1])
                for c in range(2):
                    S.op("pe", lambda e, c=c: e.transpose(out=psT[:, c * 128:(c + 1) * 128], in_=selb[:, c * 128:(c + 1) * 128], identity=identf[:]),
                         reads=[bselb, bidf], writes=[bpsT])
                S.op("dve", lambda e: e.tensor_scalar(out=selT[:].rearrange("p a b -> p (a b)"), in0=psT, scalar1=-1.0, scalar2=30000.0,
                                                      op0=ALU.add, op1=ALU.mult), reads=[bpsT], writes=[bselT])

                def selmask(idx, kt, i=i):
                    mks = [(emat[:, kt % 64, :], bc4(selT, (kt // 64) * 128, 256), [bemat, bselT])]
                    if kt >= 4 * i:
                        mks.append((ident[:], bc4(dmask, (kt - 4 * i) * 128, 512), [bid, bdmask]))
                    return mks

                attend(Qg, bQ, ksT, bksT, lambda kt: vsS[:, kt, :], bvsS, list(range(4 * i + 4)), selmask, obr[1][0], obr[1][1])
                for br in range(3):
                    ob, bob = obr[br]
                    S.op("dve", lambda e, ob=ob, br=br: e.tensor_scalar(out=wgt[:, br, :], in0=AP(ob, 64, [[520, 128], [65, 8]]), scalar1=1e-30,
                                                                        scalar2=None, op0=ALU.max), reads=[bob], writes=[bwgt])
                S.op("dve", lambda e: e.reciprocal(out=wgt[:], in_=wgt[:]), reads=[bwgt], writes=[bwgt])
                S.op("dve", lambda e, g=g, i=i: e.tensor_tensor(out=wgt[:], in0=wgt[:],
                                                               in1=AP(gts, i * 48 + g * 24, [[NT * 48, 128], [1, 3], [3, 8]]), op=ALU.mult),
                     reads=[bwgt, bgts], writes=[bwgt])
                for br in range(3):
                    ob, bob = obr[br]
                    dstt, bdstt = (ot1, bot1) if br == 0 else (ot2, bot2)
                    S.op("dve", lambda e, ob=ob, br=br, dstt=dstt: e.tensor_tensor(
                        out=dstt[:], in0=ob[:, :, 0:64], in1=AP(wgt, br * 8, [[24, 128], [1, 8], [0, 64]]), op=ALU.mult),
                        reads=[bob, bwgt], writes=[bdstt])
                    if br > 0:
                        S.op("dve", lambda e: e.tensor_tensor(out=ot1[:], in0=ot1[:], in1=ot2[:], op=ALU.add), reads=[bot1, bot2], writes=[bot1])
                Ot, bOt = Otr.next()
                S.op("act", lambda e, Ot=Ot: e.copy(out=Ot[:], in_=ot1[:]), reads=[bot1], writes=[bOt])
                S.dma("pool", lambda e, Ot=Ot, g=g, i=i: e.dma_start(
                    out=O_ap[i * 128:(i + 1) * 128, g * 512:(g + 1) * 512].rearrange("p (h d) -> p h d", h=8), in_=Ot[:]),
                    reads=[bOt], writes=[O_b])
        S.flush()


def make_nsa_masks(j):
    kk = np.arange(128)[:, None]
    qq = np.arange(128)[None, :]
    dm = np.zeros((128, 4, 128), np.float32)
    for u in range(4):
        if u < j:
            dm[:, u, :] = 1.0
        elif u == j:
            dm[:, u, :] = (kk <= qq)
    wm = np.zeros((128, 8, 128), np.float32)
    for u in range(8):
        off = u - 4 - j
        if off == -4:
            wm[:, u, :] = (kk > qq)
        elif -4 < off < 0:
            wm[:, u, :] = 1.0
        elif off == 0:
            wm[:, u, :] = (kk <= qq)
    cm = np.zeros((128, 8, 128), np.float32)
    for m in range(4):
        for w in range(2):
            nloc = kk - 128 * w
            cm[:, m * 2 + w, :] = (16 * nloc + 31 <= 128 * (4 * m + j) + qq)
    cmT = np.where(cm.transpose(2, 1, 0) > 0.5, 0.0, NEG)
    cf = np.zeros((NT, 128, 512), np.float32)
    ss = np.arange(256)[None, :]
    for i in range(NT):
        gq = 4 * i + j
        qblk = 2 * gq + (np.arange(128)[:, None] >= 64)
        forced = (ss == 0) | (ss == qblk) | (ss == qblk - 1)
        causal = ss <= qblk
        cf[i, :, 0:256] = (causal & ~forced)
        cf[i, :, 256:512] = np.where(forced, 1e4, np.where(causal, 0.0, -1e4))
    em = np.zeros((128, 64, 128), np.float32)
    for m in range(64):
        em[2 * m, m, 0:64] = 1.0
        em[2 * m + 1, m, 64:128] = 1.0
    nb = lambda m01: np.where(m01 > 0.5, 0.0, NEG).astype(NPBF)
    return {"dmaskb": nb(dm), "wmaskb": nb(wm), "cmaskb": nb(cm),
            "cmaskTb": np.ascontiguousarray(cmT).astype(NPBF), "cforce": cf, "emat": em.astype(NPBF)}


def declare_A1_outputs(k, kind):
    f = k.dout if kind == "out" else k.dint
    f("QT1", [16, 65, TOK], BF16)
    f("kcT1", [128, TOK], BF16)
    f("vcT1", [128, TOK], BF16)
    f("ksT1", [2, 64, TOK], BF16)
    f("kwT1", [2, 64, TOK], BF16)
    f("vs1", [TOK, 128], BF16)
    f("vw1", [TOK, 128], BF16)
    f("knm1", [128, 2], F32)
    f("gates1", [TOK, 48], F32)


def build_L2():
    nc = bass.Bass("TRN2", target_bir_lowering=False)
    with ExitStack() as es:
        k = K(nc, es)
        declare_common(k, [0, 1])
        k.din("x", [TOK, D], F32)
        k.din("QT0", [16, 65, TOK], BF16)
        k.din("KT0g", [4, 16, 64, TOK], BF16)
        k.din("V0g", [4, 8, 128, NT, 129], BF16)
        k.din("knm0g", [128, 4], F32)
        k.din("lam", [1, 256], F32)
        k.din("subg", [128, 128], F32)
        k.din("dmask", [128, 4, 128], BF16)
        k.din("diff_w_out", [D, D], F32)
        k.din("nsa_w_in", [D, 1840], F32)
        declare_moe(k, 0)
        k.dint("O0", [TOK, D], BF16)
        k.dint("x1s0", [TOK, D], F32)
        k.dout("xm0", [TOK, D], F32)
        declare_A1_outputs(k, "out")
        cst = emit_consts(k, es)
        mod0 = emit_mod(k, es, cst, 0, "m0")
        phase_B0(k, cst)
        phase_C(k, cst, mod0, 0, "O0", "x", "xm0", "diff_w_out", False)
        mod1 = emit_mod(k, es, cst, 1, "m1")
        cs, bcs = emit_rope_tables(k, es, "r")
        phase_A1(k, cst, mod1, cs, bcs, "xm0")
    return nc


def build_L3():
    nc = bass.Bass("TRN2", target_bir_lowering=False)
    with ExitStack() as es:
        k = K(nc, es)
        declare_common(k, [1])
        k.din("xm0", [TOK, D], F32)
        k.din("QT1", [16, 65, TOK], BF16)
        k.din("kcT1g", [4, 128, TOK], BF16)
        k.din("vcT1g", [4, 128, TOK], BF16)
        k.din("ksT1g", [4, 2, 64, TOK], BF16)
        k.din("kwT1g", [4, 2, 64, TOK], BF16)
        k.din("vs1g", [4, TOK, 128], BF16)
        k.din("vw1g", [4, TOK, 128], BF16)
        k.din("knm1g", [128, 8], F32)
        k.din("gates1", [TOK, 48], F32)
        k.din("cmp_w1", [2, 2048, 256], F32)
        k.din("cmp_b1c", [128, 4], F32)
        k.din("cmp_w2", [2, 256, 64], F32)
        k.din("cmp_posT", [2, 64, 32], F32)
        k.din("emat", [128, 64, 128], BF16)
        k.din("dmaskb", [128, 4, 128], BF16)
        k.din("wmaskb", [128, 8, 128], BF16)
        k.din("cmaskb", [128, 8, 128], BF16)
        k.din("cmaskTb", [128, 8, 128], BF16)
        k.din("cforce", [NT, 128, 512], F32)
        k.din("nsa_w_out", [D, D], F32)
        k.din("final_g", [128, D], F32)
        declare_moe(k, 1)
        k.dint("O1", [TOK, D], BF16)
        k.dint("x1s1", [TOK, D], F32)
        k.dout("out", [TOK, D], F32)
        cst = emit_consts(k, es)
        mod1 = emit_mod(k, es, cst, 1, "m1")
        phase_B1(k, cst)
        phase_C(k, cst, mod1, 1, "O1", "xm0", "out", "nsa_w_out", True)
    return nc


_NC_CACHE = {}


def _get(name, fn):
    if name not in _NC_CACHE:
        _NC_CACHE[name] = fn()
    return _NC_CACHE[name]


def kernel(x, c, positions, ada_w, ada_b, norm_g, final_g, diff_w_in, diff_w_out, diff_lambda, diff_subln_g,
           nsa_w_in, nsa_w_out, nsa_cmp_pos, nsa_cmp_w1, nsa_cmp_b1, nsa_cmp_w2,
           moe_w_group, moe_b_group, moe_w_expert, moe_b_expert, moe_w_gate, moe_w_up, moe_w_down):
    A = lambda a: np.ascontiguousarray(np.asarray(a))
    x, c, positions, ada_w, ada_b, norm_g, final_g = map(A, (x, c, positions, ada_w, ada_b, norm_g, final_g))
    moe = tuple(map(A, (moe_w_group, moe_b_group, moe_w_expert, moe_b_expert, moe_w_gate, moe_w_up, moe_w_down)))
    cores = list(range(8))
    maps = []
    for core in cores:
        b, j = core // 4, core % 4
        m = common_inputs(core, x, c, positions, ada_w, ada_b, norm_g, [0])
        m["x"] = shard_rows(x[b], j)
        m["diff_w_in"] = A(diff_w_in)[0]
        maps.append(m)
    r1 = run_bass_kernel_spmd(_get("L1", build_L1), maps, core_ids=cores).results
    maps = []
    for core in cores:
        b, j = core // 4, core % 4
        m = common_inputs(core, x, c, positions, ada_w, ada_b, norm_g, [0, 1])
        m["x"] = shard_rows(x[b], j)
        m["QT0"] = r1[core]["QT0"]
        m["KT0g"] = np.stack([r1[4 * b + r]["KT0"] for r in range(4)])
        m["V0g"] = np.stack([r1[4 * b + r]["V0"] for r in range(4)])
        m["knm0g"] = np.concatenate([r1[4 * b + r]["knm0"] for r in range(4)], axis=1)
        m["lam"] = A(diff_lambda)[0].reshape(1, 256)
        m["subg"] = A(np.broadcast_to(A(diff_subln_g)[0][None, :], (128, 128)))
        m["dmask"] = make_dmask(j)
        m["diff_w_out"] = A(diff_w_out)[0]
        m["nsa_w_in"] = A(nsa_w_in)[0]
        moe_inputs(m, 0, *moe)
        maps.append(m)
    r2 = run_bass_kernel_spmd(_get("L2", build_L2), maps, core_ids=cores).results
    maps = []
    w1 = A(nsa_cmp_w1)[0]
    b1 = A(nsa_cmp_b1)[0]
    for core in cores:
        b, j = core // 4, core % 4
        m = common_inputs(core, x, c, positions, ada_w, ada_b, norm_g, [1])
        m["xm0"] = r2[core]["xm0"]
        m["QT1"] = r2[core]["QT1"]
        for nm in ("kcT1", "vcT1", "ksT1", "kwT1", "vs1", "vw1"):
            m[nm + "g"] = np.stack([r2[4 * b + r][nm] for r in range(4)])
        kn = np.stack([r2[4 * b + r]["knm1"] for r in range(4)], axis=2)
        m["knm1g"] = A(kn.reshape(128, 8))
        m["gates1"] = r2[core]["gates1"]
        m["cmp_w1"] = w1
        m["cmp_b1c"] = A(np.concatenate([col_layout(b1[0]), col_layout(b1[1])], axis=1))
        m["cmp_w2"] = A(nsa_cmp_w2)[0]
        m["cmp_posT"] = A(A(nsa_cmp_pos)[0].transpose(0, 2, 1))
        m.update(make_nsa_masks(j))
        m["nsa_w_out"] = A(nsa_w_out)[0]
        m["final_g"] = A(np.broadcast_to(final_g[None, :], (128, D)))
        moe_inputs(m, 1, *moe)
        maps.append(m)
    r3 = run_bass_kernel_spmd(_get("L3", build_L3), maps, core_ids=cores).results
    out = np.empty((2, SEQ // 128, 128, D), np.float32)
    for core in cores:
        b, j = core // 4, core % 4
        out[b, j::4] = np.asarray(r3[core]["out"]).reshape(NT, 128, D)
    return out.reshape(2, SEQ, D)
```
